# Optimizing a Trainium2 kernel written in Bass

```python
import jax
import jax.numpy as jnp
from jax import lax
import numpy as np

D_MODEL = 1024
BATCH = 1
SEQ = 16384
DEPTH = 2

GRID_W = 64
CTX_LEN = 256
HEAD_DIM = 64
N_Q_HEADS = 8
N_KV_HEADS = 2
Q_PER_KV = N_Q_HEADS // N_KV_HEADS
ATTN_W = N_Q_HEADS * HEAD_DIM
KV_W = N_KV_HEADS * HEAD_DIM
POOL_W = D_MODEL // 4
POOL_WINDOWS = (2, 4, 8, 16)
POOL_GROUPS = len(POOL_WINDOWS)
POOL_GW = POOL_W // POOL_GROUPS
CONV_W = D_MODEL // 4
CONV_K = 31
MIX_W = ATTN_W + POOL_W + CONV_W
IN_W = ATTN_W + 2 * KV_W + POOL_W + 2 * CONV_W
Q_BLOCK = 128
ROPE_THETA = 10000.0
N_GROUPS = 4
EXPERTS_PER_GROUP = 8
N_EXPERTS = N_GROUPS * EXPERTS_PER_GROUP
TOP_K_IN_GROUP = 2
D_EXPERT = D_MODEL // 2
MOE_BLOCK = 128
N_MOD = 6
ALPHA = (2 * DEPTH) ** 0.25
BETA = (8 * DEPTH) ** -0.25
EPS = 1e-6

kernel_name = 'hybrid_headgroup_prefix_hmoe_block'


def ln_plain(x):
    xf = x.astype(jnp.float32)
    mu = xf.mean(-1, keepdims=True)
    var = jnp.square(xf - mu).mean(-1, keepdims=True)
    return ((xf - mu) * lax.rsqrt(var + EPS)).astype(x.dtype)


def ln_affine(x, g, b):
    xf = x.astype(jnp.float32)
    mu = xf.mean(-1, keepdims=True)
    var = jnp.square(xf - mu).mean(-1, keepdims=True)
    return ((xf - mu) * lax.rsqrt(var + EPS) * g + b).astype(x.dtype)


def rms_heads(t, g):
    tf = t.astype(jnp.float32)
    return (tf * lax.rsqrt(jnp.square(tf).mean(-1, keepdims=True) + EPS) * g).astype(t.dtype)


def modulate(h, shift, scale):
    return h * (1 + scale) + shift


def rope_tables(n_tok):
    rows = n_tok // GRID_W
    row = jnp.broadcast_to(jnp.arange(rows)[:, None], (rows, GRID_W)).reshape(-1)
    col = jnp.broadcast_to(jnp.arange(GRID_W)[None, :], (rows, GRID_W)).reshape(-1)
    n_freq = HEAD_DIM // 4
    inv = ROPE_THETA ** (-jnp.arange(n_freq, dtype=jnp.float32) / n_freq)
    pos = jnp.stack([row, col], axis=-1).astype(jnp.float32)
    ang = pos[:, :, None] * inv
    return jnp.cos(ang), jnp.sin(ang)


def apply_rope(t, cos, sin):
    b, n, h, _ = t.shape
    tr = t.reshape(b, n, h, 2, 2, HEAD_DIM // 4)
    t1, t2 = tr[..., 0, :], tr[..., 1, :]
    cs = cos[None, :, None].astype(t.dtype)
    sn = sin[None, :, None].astype(t.dtype)
    out = jnp.stack([t1 * cs - t2 * sn, t2 * cs + t1 * sn], axis=-2)
    return out.reshape(t.shape)


def gqa(q, k, v):
    s = jnp.einsum('bqkgd,bskd->bkgqs', q, k).astype(jnp.float32) * (HEAD_DIM ** -0.5)
    p = jax.nn.softmax(s, axis=-1).astype(v.dtype)
    return jnp.einsum('bkgqs,bskd->bqkgd', p, v)


def latent_attention(q, k_all, v_all):
    b, n = q.shape[:2]
    nb = n // Q_BLOCK
    qb = q.reshape(b, nb, Q_BLOCK, N_KV_HEADS, Q_PER_KV, HEAD_DIM).swapaxes(0, 1)
    o = lax.map(lambda qblk: gqa(qblk, k_all, v_all), qb)
    return o.swapaxes(0, 1).reshape(b, n, ATTN_W)


def pool_mixer(u, w_pool, pool_scale):
    b, n, _ = u.shape
    ug = u.reshape(b, n, POOL_GROUPS, POOL_GW).astype(jnp.float32)
    cs = jnp.concatenate([jnp.zeros((b, 1, POOL_GROUPS, POOL_GW), jnp.float32), jnp.cumsum(ug, axis=1)], axis=1)
    t = jnp.arange(n)
    outs = []
    for gi, w in enumerate(POOL_WINDOWS):
        lo = jnp.clip(t - w // 2, 0, n)
        hi = jnp.clip(t + (w - w // 2), 0, n)
        cg = cs[:, :, gi]
        mean = (cg[:, hi] - cg[:, lo]) / (hi - lo).astype(jnp.float32)[None, :, None]
        outs.append(mean - ug[:, :, gi])
    pooled = jnp.stack(outs, axis=2).astype(u.dtype)
    y = jnp.einsum('blgc,gcd->blgd', pooled, w_pool)
    return y.reshape(b, n, POOL_W) * pool_scale


def conv_module(a, g, w_dw, b_dw, cv_g, cv_b, w_pw):
    u = a * jax.nn.sigmoid(g)
    y = lax.conv_general_dilated(u, w_dw[:, None, :], window_strides=(1,),
                                 padding=[(CONV_K // 2, CONV_K // 2)],
                                 dimension_numbers=('NWC', 'WIO', 'NWC'),
                                 feature_group_count=CONV_W) + b_dw
    return jax.nn.silu(ln_affine(y, cv_g, cv_b)) @ w_pw


def split_in(p):
    o = np.cumsum([0, ATTN_W, KV_W, KV_W, POOL_W, CONV_W, CONV_W])
    return tuple(p[..., int(o[i]):int(o[i + 1])] for i in range(6))


def heads(t, n):
    return t.reshape(*t.shape[:-1], n, HEAD_DIM)


def hier_moe(h, w_rg, b_rg, w_re, b_re, wg, wu, wd):
    T, D = h.shape
    A = T * TOP_K_IN_GROUP
    nblk = -(-A // MOE_BLOCK) + N_EXPERTS
    lg = (h @ w_rg).astype(jnp.float32) + b_rg
    pg = jax.nn.softmax(lg, axis=-1)
    gsel = jnp.argmax(lg, axis=-1)
    p_group = jnp.take_along_axis(pg, gsel[:, None], axis=1)
    le = ((h @ w_re).astype(jnp.float32) + b_re).reshape(T, N_GROUPS, EXPERTS_PER_GROUP)
    le_sel = jnp.take_along_axis(le, gsel[:, None, None], axis=1)[:, 0]
    top_v, top_i = lax.top_k(le_sel, TOP_K_IN_GROUP)
    gate = (p_group * jax.nn.softmax(top_v, axis=-1)).astype(h.dtype)
    eid = (gsel[:, None] * EXPERTS_PER_GROUP + top_i).reshape(-1)
    tok = jnp.repeat(jnp.arange(T), TOP_K_IN_GROUP)
    gw = gate.reshape(-1)
    order = jnp.argsort(eid)
    e_s, tok_s, w_s = eid[order], tok[order], gw[order]
    counts = jnp.bincount(eid, length=N_EXPERTS)
    starts = jnp.cumsum(counts) - counts
    padded = (counts + MOE_BLOCK - 1) // MOE_BLOCK * MOE_BLOCK
    pends = jnp.cumsum(padded)
    pstarts = pends - padded
    dest = pstarts[e_s] + jnp.arange(A) - starts[e_s]
    P = nblk * MOE_BLOCK
    tok_buf = jnp.zeros((P,), jnp.int32).at[dest].set(tok_s.astype(jnp.int32))
    w_buf = jnp.zeros((P,), h.dtype).at[dest].set(w_s)
    blk_e = jnp.minimum(jnp.searchsorted(pends, jnp.arange(nblk) * MOE_BLOCK, side='right'), N_EXPERTS - 1)

    def expert_block(args):
        tb, e = args
        xb = h[tb]
        return (jax.nn.silu(xb @ wg[e]) * (xb @ wu[e])) @ wd[e]

    ys = lax.map(expert_block, (tok_buf.reshape(nblk, MOE_BLOCK), blk_e))
    return jnp.zeros_like(h).at[tok_buf].add(ys.reshape(P, D) * w_buf[:, None])


def setup_inputs(seed: int = 0) -> dict:
    key = jax.random.key(seed)
    ks = jax.random.split(key, 28)

    def nrm(k, shape, s):
        return jax.random.normal(k, shape, jnp.float32) * s

    L = DEPTH
    w_in = nrm(ks[6], (L, D_MODEL, IN_W), D_MODEL ** -0.5)
    w_in = w_in.at[:, :, ATTN_W + KV_W:ATTN_W + 2 * KV_W].multiply(BETA)
    return {
        'x': nrm(ks[0], (BATCH, SEQ, D_MODEL), 1.0),
        'c': nrm(ks[1], (BATCH, D_MODEL), 1.0),
        'ctx': nrm(ks[2], (BATCH, CTX_LEN, D_MODEL), 1.0),
        'c_ctx': nrm(ks[3], (D_MODEL,), 1.0),
        'w_mod': nrm(ks[4], (L, D_MODEL, N_MOD * D_MODEL), D_MODEL ** -0.5),
        'b_mod': nrm(ks[5], (L, N_MOD * D_MODEL), 0.02),
        'w_in': w_in,
        'q_gain': 1.0 + nrm(ks[7], (L, HEAD_DIM), 0.02),
        'k_gain': 1.0 + nrm(ks[8], (L, HEAD_DIM), 0.02),
        'w_pool': nrm(ks[9], (L, POOL_GROUPS, POOL_GW, POOL_GW), POOL_GW ** -0.5),
        'pool_scale': 1.0 + nrm(ks[10], (L, POOL_W), 0.02),
        'w_dw': nrm(ks[11], (L, CONV_K, CONV_W), CONV_K ** -0.5),
        'b_dw': nrm(ks[12], (L, CONV_W), 0.02),
        'cv_ln_g': 1.0 + nrm(ks[13], (L, CONV_W), 0.02),
        'cv_ln_b': nrm(ks[14], (L, CONV_W), 0.02),
        'w_cv_pw': nrm(ks[15], (L, CONV_W, CONV_W), CONV_W ** -0.5),
        'w_out': nrm(ks[16], (L, MIX_W, D_MODEL), BETA * MIX_W ** -0.5),
        'ln1_g': 1.0 + nrm(ks[17], (L, D_MODEL), 0.02),
        'ln1_b': nrm(ks[18], (L, D_MODEL), 0.02),
        'ln2_g': 1.0 + nrm(ks[19], (L, D_MODEL), 0.02),
        'ln2_b': nrm(ks[20], (L, D_MODEL), 0.02),
        'w_rg': nrm(ks[21], (L, D_MODEL, N_GROUPS), D_MODEL ** -0.5),
        'b_rg': nrm(ks[22], (L, N_GROUPS), 0.01),
        'w_re': nrm(ks[23], (L, D_MODEL, N_EXPERTS), D_MODEL ** -0.5),
        'b_re': nrm(ks[24], (L, N_EXPERTS), 0.01),
        'w_e_gate': nrm(ks[25], (L, N_EXPERTS, D_MODEL, D_EXPERT), D_MODEL ** -0.5),
        'w_e_up': nrm(ks[26], (L, N_EXPERTS, D_MODEL, D_EXPERT), D_MODEL ** -0.5),
        'w_e_down': nrm(ks[27], (L, N_EXPERTS, D_EXPERT, D_MODEL), BETA * D_EXPERT ** -0.5),
    }


def reference(x, c, ctx, c_ctx, w_mod, b_mod, w_in, q_gain, k_gain, w_pool, pool_scale, w_dw, b_dw,
              cv_ln_g, cv_ln_b, w_cv_pw, w_out, ln1_g, ln1_b, ln2_g, ln2_b, w_rg, b_rg, w_re, b_re,
              w_e_gate, w_e_up, w_e_down):
    B, L, D = x.shape
    n_ctx = ctx.shape[1]
    cos, sin = rope_tables(L)
    xl, xc = x, ctx
    for l in range(DEPTH):
        last = l == DEPTH - 1
        mod_l = (jax.nn.silu(c) @ w_mod[l] + b_mod[l])[:, None, :]
        mod_c = jax.nn.silu(c_ctx) @ w_mod[l] + b_mod[l]
        sh1, sc1, g1, sh2, sc2, g2 = jnp.split(mod_l, N_MOD, axis=-1)
        csh1, csc1, cg1, csh2, csc2, cg2 = jnp.split(mod_c, N_MOD, axis=-1)

        hl = modulate(ln_plain(xl), sh1, sc1)
        hc = modulate(ln_plain(xc), csh1, csc1)
        ql, kl, vl, ul, al, gl = split_in(hl @ w_in[l])
        if last:
            kc, vc = jnp.split(hc @ w_in[l][:, ATTN_W:ATTN_W + 2 * KV_W], 2, axis=-1)
        else:
            qc, kc, vc, uc, ac, gc = split_in(hc @ w_in[l])
        kc = rms_heads(heads(kc, N_KV_HEADS), k_gain[l])
        vc = heads(vc, N_KV_HEADS)
        ql = apply_rope(rms_heads(heads(ql, N_Q_HEADS), q_gain[l]), cos, sin)
        kl = apply_rope(rms_heads(heads(kl, N_KV_HEADS), k_gain[l]), cos, sin)
        vl = heads(vl, N_KV_HEADS)
        k_all = jnp.concatenate([kc, kl], axis=1)
        v_all = jnp.concatenate([vc, vl], axis=1)
        attn_l = latent_attention(ql, k_all, v_all)
        yl = jnp.concatenate([attn_l,
                              pool_mixer(ul, w_pool[l], pool_scale[l]),
                              conv_module(al, gl, w_dw[l], b_dw[l], cv_ln_g[l], cv_ln_b[l], w_cv_pw[l])],
                             axis=-1) @ w_out[l]
        if not last:
            qc = rms_heads(heads(qc, N_Q_HEADS), q_gain[l]).reshape(B, n_ctx, N_KV_HEADS, Q_PER_KV, HEAD_DIM)
            attn_c = gqa(qc, kc, vc).reshape(B, n_ctx, ATTN_W)
            yc = jnp.concatenate([attn_c,
                                  pool_mixer(uc, w_pool[l], pool_scale[l]),
                                  conv_module(ac, gc, w_dw[l], b_dw[l], cv_ln_g[l], cv_ln_b[l], w_cv_pw[l])],
                                 axis=-1) @ w_out[l]
            xc = ln_affine(ALPHA * xc + cg1 * yc, ln1_g[l], ln1_b[l])
        xl = ln_affine(ALPHA * xl + g1 * yl, ln1_g[l], ln1_b[l])

        hl = modulate(ln_plain(xl), sh2, sc2).reshape(B * L, D)
        if last:
            ol = hier_moe(hl, w_rg[l], b_rg[l], w_re[l], b_re[l], w_e_gate[l], w_e_up[l], w_e_down[l])
        else:
            hc = modulate(ln_plain(xc), csh2, csc2).reshape(B * n_ctx, D)
            o = hier_moe(jnp.concatenate([hc, hl], axis=0), w_rg[l], b_rg[l], w_re[l], b_re[l],
                         w_e_gate[l], w_e_up[l], w_e_down[l])
            oc, ol = o[:B * n_ctx], o[B * n_ctx:]
            xc = ln_affine(ALPHA * xc + cg2 * oc.reshape(xc.shape), ln2_g[l], ln2_b[l])
        xl = ln_affine(ALPHA * xl + g2 * ol.reshape(xl.shape), ln2_g[l], ln2_b[l])
    return xl
```

```python
import numpy as np
import ml_dtypes
from contextlib import ExitStack
import concourse.bass as bass
import concourse.mybir as mybir
from concourse.bass_utils import run_bass_kernel_spmd

F32 = mybir.dt.float32
BF = mybir.dt.bfloat16
ALU = mybir.AluOpType
AF = mybir.ActivationFunctionType
AX = mybir.AxisListType

D = 1024
SEQ = 16384
NCORE = 8
NTOK = SEQ // NCORE
NTI = NTOK // 128
NCTX = 256
NKC = (SEQ + NCTX) // 128
KEYS = SEQ + NCTX
HTW = NTOK + NCTX + 32
HAL0 = NTOK + NCTX
NEXP = 32
DEPTH = 2
ALPHA = (2 * DEPTH) ** 0.25
EPS = 1e-6
GRID_W = 64


class Sched:
    def __init__(self, nc, es):
        self.nc = nc
        self.E = {'pe': nc.tensor, 'act': nc.scalar, 'dve': nc.vector, 'pool': nc.gpsimd, 'sp': nc.sync}
        self.sem = {k: es.enter_context(nc.semaphore('c_' + k)) for k in self.E}
        self.cnt = {k: 0 for k in self.E}
        self.seen = {k: {} for k in self.E}
        self.W = {}
        self.R = {}
        self.ND = 32
        self.dsem = [es.enter_context(nc.semaphore('d%d' % i)) for i in range(self.ND)]
        self.dval = [0] * self.ND
        self.di = 0
        self.nwait = 0

    def _wait(self, eng, dep):
        sem, val, key = dep
        if self.seen[eng].get(key, 0) >= val:
            return
        self.seen[eng][key] = val
        self.E[eng].wait_ge(sem, val)
        self.nwait += 1

    def _deps(self, eng, reads, writes):
        for k in reads:
            d = self.W.get(k)
            if d is not None and not (eng == 'pe' and d[2] == 'pe'):
                self._wait(eng, d)
        for k in writes:
            d = self.W.get(k)
            if d is not None and not (eng == 'pe' and d[2] == 'pe'):
                self._wait(eng, d)
            for d in self.R.get(k, {}).values():
                if not (eng == 'pe' and d[2] == 'pe'):
                    self._wait(eng, d)

    def _reg(self, dep, reads, writes):
        for k in writes:
            self.W[k] = dep
            self.R[k] = {}
        for k in reads:
            self.R.setdefault(k, {})[dep[2]] = dep

    def op(self, eng, fn, reads=(), writes=(), inc=True):
        self._deps(eng, reads, writes)
        ins = fn(self.E[eng])
        if inc:
            self.cnt[eng] += 1
            ins.then_inc(self.sem[eng], 1)
            dep = (self.sem[eng], self.cnt[eng], eng)
        else:
            dep = (self.sem[eng], self.cnt[eng] + 1, eng)
        self._reg(dep, reads, writes)

    def dma(self, q, out, in_, reads=(), writes=()):
        i = self.di
        self.di = (self.di + 1) % self.ND
        if self.dval[i] > 0:
            self._wait(q, (self.dsem[i], self.dval[i], ('d', i)))
        self._deps(q, reads, writes)
        self.dval[i] += 16
        self.E[q].dma_start(out=out, in_=in_).then_inc(self.dsem[i], 16)
        dep = (self.dsem[i], self.dval[i], ('d', i))
        self._reg(dep, reads, writes)

    def barrier(self, engs=None):
        engs = engs or list(self.E)
        for e in engs:
            for f in self.E:
                if f != e and self.cnt[f] > 0:
                    self._wait(e, (self.sem[f], self.cnt[f], f))
            for i in range(self.ND):
                if self.dval[i] > 0:
                    self._wait(e, (self.dsem[i], self.dval[i], ('d', i)))


INPUTS = [
    ('x_all', [SEQ, D], F32), ('x_own', [NTOK, D], F32), ('x_halo', [32, D], F32), ('ctx', [NCTX, D], F32),
    ('cT', [128, 16], F32), ('w_mod', [D, 6 * D], F32), ('b_mod', [1, 6 * D], F32), ('w_in', [D, 1536], F32),
    ('qk_gain', [1, 128], F32), ('w_pool', [256, 64], F32), ('cvp', [128, 8], F32), ('w_dwT', [128, 62], F32),
    ('w_pw', [256, 256], F32), ('w_out', [D, D], F32), ('lnv', [1, 4 * D], F32), ('w_r', [D, 36], F32),
    ('b_r', [1, 36], F32), ('w_eg', [NEXP, D, 512], F32), ('w_eu', [NEXP, D, 512], F32),
    ('w_ed', [NEXP, 512, D], F32), ('cs_all', [SEQ, 64], F32), ('cs_own', [NTOK, 64], F32),
    ('invcnt', [128, 2 * (NTOK + NCTX)], F32), ('hmask', [128, 32], F32), ('sel', [32, NEXP * 128], F32),
    ('selrow', [64, 256], F32), ('ident_f', [128, 128], F32), ('ident_b', [128, 128], BF),
]


def build(stop_after=99, dbg=False):
    nc = bass.Bass("TRN2", target_bir_lowering=False)
    I = {n: nc.dram_tensor(n, list(s), dt, kind="ExternalInput").ap() for n, s, dt in INPUTS}
    x_out = nc.dram_tensor('x_out', [NTOK, D], F32, kind="ExternalOutput").ap()
    ctx_out = nc.dram_tensor('ctx_out', [NCTX, D], F32, kind="ExternalOutput").ap()
    DBG = {}
    if dbg:
        for n, s, dt in [('d_modT', [128, 64], F32), ('d_gbc', [128, 4096], F32), ('d_KT', [128, KEYS], BF),
                         ('d_V', [128, NKC * 192], BF), ('d_QT', [128, 4, NTOK + NCTX], BF),
                         ('d_yT', [128, 4, NTOK + NCTX], BF), ('d_hT', [128, 8 * HTW], BF),
                         ('d_xres', [128, 18 * D], F32), ('d_GT', [32, NTOK + NCTX], F32)]:
            DBG[n] = nc.dram_tensor(n, list(s), dt, kind="ExternalOutput").ap()

    es = ExitStack()
    S = Sched(nc, es)

    def sb(name, shape, dt=F32, st=None):
        return (st or es).enter_context(nc.sbuf_tensor('s_' + name, list(shape), dt))

    def ps(name, shape, dt=F32, st=None):
        return (st or es).enter_context(nc.psum_tensor('p_' + name, list(shape), dt))

    def dbg_dump(name, ap, key):
        if dbg:
            S.barrier()
            S.dma('sp', DBG[name], ap, reads=[key], writes=[('dbgout', name)])
            S.barrier()

    ident_f = sb('ident_f', [128, 128]); ident_b = sb('ident_b', [128, 128], BF)
    ones_f = sb('ones_f', [128, 128]); epst = sb('epst', [128, 1])
    modT = sb('modT', [128, 64])
    gbc = sb('gbc', [128, 4, D])
    qkg = sb('qkg', [128, 128]); cvp = sb('cvp', [128, 8]); wdwT = sb('wdwT', [128, 62])
    negm = sb('negm', [128, 1]); hmask = sb('hmask', [128, 32])
    QY = sb('QY', [128, 8, NTOK + NCTX], BF)
    QT = QY[:, 0:4, :]
    yT = QY[:, 4:8, :]
    sm = sb('sm', [128, 8, 64])
    smi = [0]

    def smslot():
        smi[0] = (smi[0] + 1) % 8
        return smi[0]

    S.dma('sp', ident_f[:], I['ident_f'], writes=['ident_f'])
    S.dma('sp', ident_b[:], I['ident_b'], writes=['ident_b'])
    S.dma('sp', qkg[:], I['qk_gain'].to_broadcast([128, 128]), writes=['qkg'])
    S.dma('sp', cvp[:], I['cvp'], writes=['cvp'])
    S.dma('sp', wdwT[:], I['w_dwT'], writes=['wdwT'])
    S.dma('sp', hmask[:], I['hmask'], writes=['hmask'])
    S.op('dve', lambda e: e.memset(ones_f[:], 1.0), writes=['ones_f'])
    S.op('dve', lambda e: e.memset(epst[:], EPS), writes=['epst'])
    sl = smslot()
    S.op('dve', lambda e: e.tensor_tensor(out=sm[:, sl, 0:64], in0=qkg[:, 0:64], in1=qkg[:, 64:128], op=ALU.mult),
         reads=['qkg'], writes=[('sm', sl)])
    S.op('dve', lambda e: e.tensor_reduce(out=negm[:], in_=sm[:, sl, 0:64], axis=AX.X, op=ALU.max,
                                          apply_absolute_value=True), reads=[('sm', sl)], writes=['negm'])
    S.op('dve', lambda e: e.tensor_scalar(out=negm[:], in0=negm[:], scalar1=-8.0, scalar2=None, op0=ALU.mult),
         reads=['negm'], writes=['negm'])

    def ln_stats(src, srckey):
        s = smslot()
        k = ('sm', s)
        S.op('dve', lambda e: e.bn_stats(out=sm[:, s, 0:6], in_=src[:, 0:512]), reads=[srckey], writes=[k])
        S.op('dve', lambda e: e.bn_stats(out=sm[:, s, 6:12], in_=src[:, 512:1024]), reads=[srckey, k], writes=[k])
        S.op('dve', lambda e: e.bn_aggr(out=sm[:, s, 12:14], in_=sm[:, s, 0:12]), reads=[k], writes=[k])
        S.op('act', lambda e: e.activation(out=sm[:, s, 14:15], in_=sm[:, s, 13:14], func=AF.Sqrt,
                                           bias=epst[:, 0:1], scale=1.0), reads=[k, 'epst'], writes=[k])
        S.op('dve', lambda e: e.reciprocal(out=sm[:, s, 14:15], in_=sm[:, s, 14:15]), reads=[k], writes=[k])
        S.op('dve', lambda e: e.tensor_scalar(out=sm[:, s, 15:16], in0=sm[:, s, 12:13], scalar1=sm[:, s, 14:15],
                                              scalar2=-1.0, op0=ALU.mult, op1=ALU.mult), reads=[k], writes=[k])
        return sm[:, s, 14:15], sm[:, s, 15:16], k

    with ExitStack() as p0:
        cc = sb('cc', [128, 8, 64], st=p0); cTt = sb('cTt', [128, 16], st=p0)
        bmod = sb('bmod', [1, 6 * D], st=p0)
        wst = [sb('wst%d' % i, [128, 8, 512], st=p0) for i in range(2)]
        mrow = [sb('mrow%d' % i, [64, 512], st=p0) for i in range(2)]
        selrow = sb('selrow', [64, 256], st=p0)
        pm = [ps('pm%d' % i, [128, 512], st=p0) for i in range(2)]
        pt = [ps('ptm%d' % i, [128, 512], st=p0) for i in range(2)]
        S.dma('sp', cTt[:], I['cT'], writes=['cTt'])
        S.dma('sp', bmod[:], I['b_mod'], writes=['bmod'])
        S.dma('sp', selrow[:], I['selrow'], writes=['selrow'])
        S.op('dve', lambda e: e.memset(cc[:], 0.0), writes=['cc'])
        cTv = cTt[:].rearrange("p (k w) -> p k w", w=2)
        for w in range(2):
            S.op('act', lambda e: e.activation(out=cc[:, :, 32 * w:32 * w + 1], in_=cTv[:, :, w:w + 1], func=AF.Silu),
                 reads=['cTt', 'cc'], writes=['cc'])
        wmv = I['w_mod'].rearrange("(k p) n -> p k n", p=128)
        for nb in range(12):
            b = nb % 2
            S.dma('sp', wst[b][:], wmv[:, :, nb * 512:(nb + 1) * 512], writes=[('wst', b)])
            for k in range(8):
                S.op('pe', lambda e: e.matmul(out=pm[b][0:64, :], lhsT=cc[:, k, :], rhs=wst[b][:, k, :],
                                              start=(k == 0), stop=False),
                     reads=['cc', ('wst', b)], writes=[('pm', b)], inc=False)
            S.op('pe', lambda e: e.matmul(out=pm[b][0:64, :], lhsT=ones_f[0:1, 0:64],
                                          rhs=bmod[0:1, nb * 512:(nb + 1) * 512], start=False, stop=True),
                 reads=['ones_f', 'bmod'], writes=[('pm', b)])
            S.op('act', lambda e: e.copy(out=mrow[b][:], in_=pm[b][0:64, :]), reads=[('pm', b)], writes=[('mrow', b)])
            kind6 = nb // 2
            if kind6 in (2, 5):
                for w in range(2):
                    S.op('pe', lambda e: e.matmul(out=pt[w][:, :], lhsT=selrow[:, w * 128:(w + 1) * 128],
                                                  rhs=mrow[b][:], start=True, stop=True),
                         reads=['selrow', ('mrow', b)], writes=[('pt', w)])
                    gi = (0 if kind6 == 2 else 2) + w
                    S.op('act', lambda e: e.copy(out=gbc[:, gi, (nb % 2) * 512:(nb % 2) * 512 + 512], in_=pt[w][:, :]),
                         reads=[('pt', w)], writes=['gbc'])
            else:
                kind = {0: 0, 1: 1, 3: 2, 4: 3}[kind6]
                for j in range(4):
                    k = (nb % 2) * 4 + j
                    S.op('pe', lambda e: e.transpose(out=pt[0][:, j * 64:(j + 1) * 64],
                                                     in_=mrow[b][:, j * 128:(j + 1) * 128], identity=ident_f[0:64, 0:64]),
                         reads=[('mrow', b), 'ident_f'], writes=[('pt', 0)], inc=(j == 3))
                ptv = pt[0][:, 0:256].rearrange("p (j c) -> p j c", c=64)
                mv = modT[:, kind * 16 + (nb % 2) * 8: kind * 16 + (nb % 2) * 8 + 8].rearrange("p (j w) -> p j w", w=2)
                for w in range(2):
                    S.op('dve', lambda e: e.tensor_scalar(out=mv[:, :, w:w + 1], in0=ptv[:, :, 32 * w:32 * w + 1],
                                                          scalar1=(1.0 if kind in (1, 3) else 0.0), scalar2=None,
                                                          op0=ALU.add), reads=[('pt', 0)], writes=['modT'])
        dbg_dump('d_modT', modT[:], 'modT')
        dbg_dump('d_gbc', gbc[:].rearrange("p a b -> p (a b)"), 'gbc')
        S.barrier()
    if stop_after <= 0:
        return finish(nc, S, es, x_out, ctx_out)

    def SC(kind, k, w):
        i = kind * 16 + k * 2 + w
        return modT[:, i:i + 1]

    def ln_mod_T(src, srckey, ntok, kinds, w, dst_ap_fn, dstkey, xnb, xnbkey, ptr, ptrkey):
        ln_mod_a(src, srckey, ntok, xnb, xnbkey)
        ln_mod_b(ntok, kinds, w, dst_ap_fn, dstkey, xnb, xnbkey, ptr, ptrkey)

    def ln_mod_a(src, srckey, ntok, xnb, xnbkey):
        rstd, nmr, k = ln_stats(src, srckey)
        S.op('act', lambda e: e.activation(out=xnb[0:ntok, :], in_=src[0:ntok, :], func=AF.Identity, bias=nmr[0:ntok],
                                           scale=rstd[0:ntok]), reads=[srckey, k], writes=[xnbkey])

    def ln_mod_b(ntok, kinds, w, dst_ap_fn, dstkey, xnb, xnbkey, ptr, ptrkey):
        for kk in range(8):
            S.op('pe', lambda e: e.transpose(out=ptr[:, kk, 0:ntok], in_=xnb[0:ntok, kk * 128:(kk + 1) * 128],
                                             identity=ident_b[0:ntok, 0:ntok]),
                 reads=[xnbkey, 'ident_b'], writes=[ptrkey], inc=(kk == 7))
        for kk in range(8):
            if kk % 2 == 0:
                S.op('dve', lambda e: e.tensor_scalar(out=dst_ap_fn(kk), in0=ptr[:, kk, 0:ntok],
                                                      scalar1=SC(kinds[1], kk, w), scalar2=SC(kinds[0], kk, w),
                                                      op0=ALU.mult, op1=ALU.add),
                     reads=[ptrkey, 'modT'], writes=[dstkey])
            else:
                S.op('act', lambda e: e.activation(out=dst_ap_fn(kk), in_=ptr[:, kk, 0:ntok], func=AF.Identity,
                                                   bias=SC(kinds[0], kk, w), scale=SC(kinds[1], kk, w)),
                     reads=[ptrkey, 'modT'], writes=[dstkey])

    def rms_rope(src, srckey, nh, gain_ap, cs, cskey, dst, dstkey, tmp, tmpkey, rope):
        s = smslot(); k = ('sm', s)
        W_ = nh * 64
        src_ps = src
        src = tmp[:, 2 * W_:3 * W_]
        S.op('act', lambda e: e.copy(out=src, in_=src_ps), reads=[srckey], writes=[tmpkey])
        srckey = tmpkey
        S.op('dve', lambda e: e.tensor_tensor(out=tmp[:, 0:W_], in0=src, in1=src, op=ALU.mult),
             reads=[srckey], writes=[tmpkey])
        S.op('dve', lambda e: e.tensor_reduce(out=sm[:, s, 0:nh], in_=tmp[:, 0:W_].rearrange("p (h d) -> p h d", d=64),
                                              axis=AX.X, op=ALU.add), reads=[tmpkey], writes=[k])
        S.op('act', lambda e: e.activation(out=sm[:, s, 0:nh], in_=sm[:, s, 0:nh], func=AF.Sqrt, bias=epst[:, 0:1],
                                           scale=1.0 / 64), reads=[k, 'epst'], writes=[k])
        S.op('dve', lambda e: e.reciprocal(out=sm[:, s, 0:nh], in_=sm[:, s, 0:nh]), reads=[k], writes=[k])
        t3 = tmp[:, 0:W_].rearrange("p (h d) -> p h d", d=64)
        S.op('dve', lambda e: e.tensor_tensor(out=t3, in0=src.rearrange("p (h d) -> p h d", d=64),
                                              in1=sm[:, s, 0:nh].unsqueeze(2).to_broadcast([128, nh, 64]), op=ALU.mult),
             reads=[srckey, k], writes=[tmpkey])
        gdst = t3 if rope else dst.rearrange("p (h d) -> p h d", d=64)
        S.op('dve', lambda e: e.tensor_tensor(out=gdst, in0=t3, in1=gain_ap.unsqueeze(1).to_broadcast([128, nh, 64]),
                                              op=ALU.mult), reads=[tmpkey, 'qkg'], writes=[tmpkey if rope else dstkey])
        if not rope:
            return
        t5 = tmp[:, 0:W_].rearrange("p (h a b f) -> p h a b f", a=2, b=2, f=16)
        d5 = dst.rearrange("p (h a b f) -> p h a b f", a=2, b=2, f=16)
        u5 = tmp[:, W_:2 * W_].rearrange("p (h a b f) -> p h a b f", a=2, b=2, f=16)
        cosb = cs[:, 0:32].rearrange("p (a f) -> p a f", f=16).unsqueeze(1).to_broadcast([128, nh, 2, 16])
        sinb = cs[:, 32:64].rearrange("p (a f) -> p a f", f=16).unsqueeze(1).to_broadcast([128, nh, 2, 16])
        t1, t2 = t5[:, :, :, 0, :], t5[:, :, :, 1, :]
        ua, ub = u5[:, :, :, 0, :], u5[:, :, :, 1, :]
        rk = [tmpkey, cskey]
        S.op('dve', lambda e: e.tensor_tensor(out=ua, in0=t1, in1=cosb, op=ALU.mult), reads=rk, writes=[tmpkey])
        S.op('dve', lambda e: e.tensor_tensor(out=ub, in0=t2, in1=sinb, op=ALU.mult), reads=rk, writes=[tmpkey])
        S.op('dve', lambda e: e.tensor_tensor(out=d5[:, :, :, 0, :], in0=ua, in1=ub, op=ALU.subtract),
             reads=[tmpkey], writes=[dstkey])
        S.op('dve', lambda e: e.tensor_tensor(out=ua, in0=t2, in1=cosb, op=ALU.mult), reads=rk, writes=[tmpkey])
        S.op('dve', lambda e: e.tensor_tensor(out=ub, in0=t1, in1=sinb, op=ALU.mult), reads=rk, writes=[tmpkey])
        S.op('dve', lambda e: e.tensor_tensor(out=d5[:, :, :, 1, :], in0=ua, in1=ub, op=ALU.add),
             reads=[tmpkey], writes=[dstkey])

    LP = 16 + NTOK + 16
    LC = 16 + NCTX + 16
    with ExitStack() as pa:
        hT = sb('hT', [128, 8, HTW], BF, st=pa)
        uT = sb('uT', [128, 2, LP], st=pa); cuT = sb('cuT', [128, 2, LP], st=pa)
        uTc = sb('uTc', [128, 2, LC], st=pa); cuTc = sb('cuTc', [128, 2, LC], st=pa)
        with ExitStack() as pa1:
            winb = sb('winb', [128, 8, 1536], BF, st=pa1)
            xt = [sb('xt%d' % i, [128, D], st=pa1) for i in range(3)]
            xnb = [sb('xnb%d' % i, [128, D], BF, st=pa1) for i in range(2)]
            wst = [sb('wsta%d' % i, [128, 8, 512], st=pa1) for i in range(1)]
            qtmp = [sb('qtmp%d' % i, [128, 1536], st=pa1) for i in range(2)]
            qrb = [sb('qrb%d' % i, [128, 512], BF, st=pa1) for i in range(2)]
            cst = [sb('cst%d' % i, [128, 64], st=pa1) for i in range(2)]
            sig = [sb('sig%d' % i, [128, 512], st=pa1) for i in range(2)]
            ptr = [ps('ptr%d' % i, [128, 8, 128], BF, st=pa1) for i in range(2)]
            pq = [ps('pq%d' % i, [128, 512], st=pa1) for i in range(2)]
            pqt = [ps('pqt%d' % i, [128, 4, 128], BF, st=pa1) for i in range(2)]
            pf = [ps('pf%d' % i, [128, 512], st=pa1) for i in range(2)]
            wiv = I['w_in'].rearrange("(k p) n -> p k n", p=128)
            for nb in range(3):
                b = 0
                S.dma('sp', wst[b][:], wiv[:, :, nb * 512:(nb + 1) * 512], writes=[('wsta', b)])
                S.op('pool' if nb == 1 else 'dve', lambda e: e.tensor_copy(out=winb[:, :, nb * 512:(nb + 1) * 512], in_=wst[b][:]),
                     reads=[('wsta', b)], writes=['winb'])
            tiles = [('own', t) for t in range(NTI)] + [('ctx', t) for t in range(2)] + [('halo', 0)]
            for ti, (kind, t) in enumerate(tiles):
                b3 = ti % 3; b = ti % 2
                ntok = 32 if kind == 'halo' else 128
                src = {'own': I['x_own'], 'ctx': I['ctx'], 'halo': I['x_halo']}[kind]
                col0 = {'own': t * 128, 'ctx': NTOK + t * 128, 'halo': HAL0}[kind]
                w = 1 if kind == 'ctx' else 0
                S.dma('sp', xt[b3][0:ntok, :], src[t * 128:t * 128 + ntok, :], writes=[('xt', b3)])
                ln_mod_T(xt[b3], ('xt', b3), ntok, (0, 1), w,
                         lambda kk: hT[:, kk, col0:col0 + ntok], ('hT', ti), xnb[b], ('xnb', b), ptr[b], ('ptr', b))
                if kind == 'halo':
                    continue
                for kk in range(8):
                    S.op('pe', lambda e: e.matmul(out=pq[b][:, :], lhsT=hT[:, kk, col0:col0 + 128], rhs=winb[:, kk, 0:512],
                                                  start=(kk == 0), stop=(kk == 7)),
                         reads=[('hT', ti), 'winb'], writes=[('pq', b)], inc=(kk == 7))
                if kind == 'own':
                    S.dma('sp', cst[b][:], I['cs_own'][t * 128:(t + 1) * 128, :], writes=[('cst', b)])
                rms_rope(pq[b][:, :], ('pq', b), 8, qkg[:, 0:64], cst[b], ('cst', b), qrb[b][:, :], ('qrb', b),
                         qtmp[b], ('qtmp', b), rope=(kind == 'own'))
                for h_ in range(8):
                    g_, j_ = h_ // 4, h_ % 4
                    S.op('pe', lambda e: e.transpose(out=pqt[b][g_ * 64:(g_ + 1) * 64, j_, :], in_=qrb[b][:, h_ * 64:(h_ + 1) * 64],
                                                     identity=ident_b[:, :]),
                         reads=[('qrb', b), 'ident_b'], writes=[('pqt', b)], inc=(h_ == 7))
                S.op('act', lambda e: e.copy(out=QT[:, :, col0:col0 + 128], in_=pqt[b][:, :, :]),
                     reads=[('pqt', b)], writes=[('QT', col0 // 512)])
            blocks = [(i * 512, 512, 'own') for i in range(4)] + [(NTOK, 256, 'ctx'), (HAL0, 32, 'halo')]
            allhT = [('hT', ti) for ti in range(len(tiles))]
            pi = 0
            for (c0, n, kind) in blocks:
                def dsts(tn, ch):
                    if kind == 'own':
                        return tn[:, ch, 16 + c0:16 + c0 + n]
                    if kind == 'ctx':
                        return tn[:, ch, 16:16 + NCTX]
                    return tn[:, ch, :].rearrange("p (a b) -> p a b", a=2)[:, :, 0:16] if False else None
                for ch in range(2):
                    pu_, pa_, pg_ = None, None, None
                    res = {}
                    for which, cc0 in (('u', 768), ('g', 1280), ('a', 1024)):
                        b = pi % 2; pi += 1
                        for kk in range(8):
                            S.op('pe', lambda e: e.matmul(out=pf[b][:, 0:n], lhsT=winb[:, kk, cc0 + ch * 128:cc0 + ch * 128 + 128],
                                                          rhs=hT[:, kk, c0:c0 + n], start=(kk == 0), stop=(kk == 7)),
                                 reads=allhT + ['winb'], writes=[('pf', b)], inc=(kk == 7))
                        if kind == 'halo':
                            def hal(tn):
                                return [(tn[:, ch, 0:16], 0), (tn[:, ch, 16 + NTOK:32 + NTOK], 16)]
                        if which == 'u':
                            if kind == 'halo':
                                for (dap, o) in hal(uT):
                                    S.op('dve', lambda e: e.tensor_tensor(out=dap, in0=pf[b][:, o:o + 16], in1=hmask[:, o:o + 16],
                                                                          op=ALU.mult), reads=[('pf', b), 'hmask'], writes=['uT'])
                            else:
                                S.op('act', lambda e: e.copy(out=dsts(uT if kind == 'own' else uTc, ch), in_=pf[b][:, 0:n]),
                                     reads=[('pf', b)], writes=['uT'])
                        elif which == 'g':
                            sb_ = b
                            S.op('act', lambda e: e.activation(out=sig[sb_][:, 0:n], in_=pf[b][:, 0:n], func=AF.Sigmoid),
                                 reads=[('pf', b)], writes=[('sig', sb_)])
                        else:
                            if kind == 'halo':
                                S.op('dve', lambda e: e.tensor_tensor(out=sig[sb_][:, 0:32], in0=sig[sb_][:, 0:32], in1=hmask[:, :],
                                                                      op=ALU.mult), reads=[('sig', sb_), 'hmask'], writes=[('sig', sb_)])
                                for (dap, o) in hal(cuT):
                                    S.op('dve', lambda e: e.tensor_tensor(out=dap, in0=pf[b][:, o:o + 16], in1=sig[sb_][:, o:o + 16],
                                                                          op=ALU.mult), reads=[('pf', b), ('sig', sb_)], writes=['cuT'])
                            else:
                                S.op('dve', lambda e: e.tensor_tensor(out=dsts(cuT if kind == 'own' else cuTc, ch), in0=pf[b][:, 0:n],
                                                                      in1=sig[sb_][:, 0:n], op=ALU.mult),
                                     reads=[('pf', b), ('sig', sb_)], writes=['cuT'])
            for tn, kname in ((uTc, 'uT'), (cuTc, 'cuT')):
                S.op('pool', lambda e: e.memset(tn[:, :, 0:16], 0.0), writes=[kname])
                S.op('pool', lambda e: e.memset(tn[:, :, 16 + NCTX:32 + NCTX], 0.0), writes=[kname])
            S.barrier()
        dbg_dump('d_hT', hT[:].rearrange("p a b -> p (a b)"), ('hT', 0))

        with ExitStack() as pa2:
            pl = [sb('pl%d' % i, [128, LP], st=pa2) for i in range(2)]
            pooled = sb('pooled', [128, NTOK], BF, st=pa2)
            icn = sb('icn', [128, NTOK], st=pa2)
            wpbd = sb('wpbd', [128, 2, 128], BF, st=pa2); wpst = sb('wpst', [128, 2, 128], st=pa2)
            wpwb = sb('wpwb', [128, 2, 256], BF, st=pa2); wpws = sb('wpws', [128, 2, 256], st=pa2)
            acc = sb('acc', [128, 2, NTOK], st=pa2)
            sq = [sb('sq%d' % i, [128, 512], st=pa2) for i in range(2)]
            mean = sb('mean', [128, 512], st=pa2); rstd = sb('rstdc', [128, 512], st=pa2)
            zT = sb('zT', [128, 2, 512], BF, st=pa2)
            pp = [ps('pp%d' % i, [128, 512], st=pa2) for i in range(2)]
            ps1 = ps('ps1', [128, 512], st=pa2); ps2 = ps('ps2', [128, 512], st=pa2)
            pw = [ps('pw%d' % i, [128, 512], st=pa2) for i in range(2)]
            S.op('dve', lambda e: e.memset(wpst[:], 0.0), writes=['wpst'])
            for g in range(4):
                h = (g % 2) * 64
                S.dma('sp', wpst[h:h + 64, g // 2, h:h + 64], I['w_pool'][g * 64:(g + 1) * 64, :], reads=[], writes=['wpst'])
            S.op('dve', lambda e: e.tensor_copy(out=wpbd[:], in_=wpst[:]), reads=['wpst'], writes=['wpbd'])
            S.dma('sp', wpws[:], I['w_pw'].rearrange("(k p) n -> p k n", p=128), writes=['wpws'])
            S.op('dve', lambda e: e.tensor_copy(out=wpwb[:], in_=wpws[:]), reads=['wpws'], writes=['wpwb'])

            for (kind, U_, CU_, L, ycol0, ic0) in (('own', uT, cuT, NTOK, 0, 0), ('ctx', uTc, cuTc, NCTX, NTOK, 2 * NTOK)):
                LL = L + 32
                nblk = [(i * 512, 512) for i in range(L // 512)] if L >= 512 else [(0, L)]
                for ch in range(2):
                    U = U_[:, ch, :]
                    A, B = pl[0], pl[1]
                    S.dma('sp', icn[:, 0:L], I['invcnt'][:, ic0 + ch * L: ic0 + (ch + 1) * L], writes=['icn'])
                    S.op('dve', lambda e: e.tensor_tensor(out=A[:, 1:LL], in0=U[:, 0:LL - 1], in1=U[:, 1:LL], op=ALU.add),
                         reads=['uT'], writes=['plA'])
                    S.op('dve', lambda e: e.tensor_tensor(out=B[:, 2:LL - 1], in0=A[:, 1:LL - 2], in1=A[:, 3:LL], op=ALU.add),
                         reads=['plA'], writes=['plB'])
                    if ch == 0:
                        lo, hi = A, B
                    else:
                        S.op('dve', lambda e: e.tensor_tensor(out=A[:, 4:LL - 3], in0=B[:, 2:LL - 5], in1=B[:, 6:LL - 1], op=ALU.add),
                             reads=['plB'], writes=['plA'])
                        S.op('dve', lambda e: e.tensor_tensor(out=B[:, 8:LL - 7], in0=A[:, 4:LL - 11], in1=A[:, 12:LL - 3], op=ALU.add),
                             reads=['plA'], writes=['plB'])
                        lo, hi = A, B
                    for (h0, srcw, kk_) in ((0, lo, 'plA'), (64, hi, 'plB')):
                        S.op('dve', lambda e: e.tensor_tensor(out=srcw[h0:h0 + 64, 16:16 + L], in0=srcw[h0:h0 + 64, 16:16 + L],
                                                              in1=icn[h0:h0 + 64, 0:L], op=ALU.mult),
                             reads=[kk_, 'icn'], writes=[kk_])
                        S.op('dve', lambda e: e.tensor_tensor(out=pooled[h0:h0 + 64, 0:L], in0=srcw[h0:h0 + 64, 16:16 + L],
                                                              in1=U[h0:h0 + 64, 16:16 + L], op=ALU.subtract),
                             reads=[kk_, 'uT'], writes=['pooled'])
                    for bi, (c0, n) in enumerate(nblk):
                        b = bi % 2
                        S.op('pe', lambda e: e.matmul(out=pp[b][:, 0:n], lhsT=wpbd[:, ch, :], rhs=pooled[:, c0:c0 + n],
                                                      start=True, stop=True), reads=['wpbd', 'pooled'], writes=[('pp', b)])
                        S.op('act', lambda e: e.activation(out=yT[:, ch, ycol0 + c0:ycol0 + c0 + n], in_=pp[b][:, 0:n],
                                                           func=AF.Identity, bias=0.0, scale=cvp[:, ch * 4 + 3:ch * 4 + 4]),
                             reads=[('pp', b), 'cvp'], writes=['yT'])
                for ch in range(2):
                    CU = CU_[:, ch, :]
                    a_ = acc[:, ch, 0:L]
                    S.op('dve', lambda e: e.tensor_scalar(out=a_, in0=CU[:, 1:1 + L], scalar1=wdwT[:, ch * 31:ch * 31 + 1],
                                                          scalar2=cvp[:, ch * 4:ch * 4 + 1], op0=ALU.mult, op1=ALU.add),
                         reads=['cuT', 'wdwT', 'cvp'], writes=['acc'])
                    for j in range(1, 31):
                        S.op('dve', lambda e: e.scalar_tensor_tensor(out=a_, in0=CU[:, 1 + j:1 + j + L],
                                                                     scalar=wdwT[:, ch * 31 + j:ch * 31 + j + 1], in1=a_,
                                                                     op0=ALU.mult, op1=ALU.add),
                             reads=['cuT', 'acc'], writes=['acc'])
                for bi, (c0, n) in enumerate(nblk):
                    for ch in range(2):
                        S.op('pe', lambda e: e.matmul(out=ps1[:, 0:n], lhsT=ones_f[:, :], rhs=acc[:, ch, c0:c0 + n],
                                                      start=(ch == 0), stop=(ch == 1)), reads=['acc', 'ones_f'], writes=['ps1'], inc=(ch == 1))
                    for ch in range(2):
                        S.op('act', lambda e: e.activation(out=sq[ch][:, 0:n], in_=acc[:, ch, c0:c0 + n], func=AF.Square),
                             reads=['acc'], writes=[('sq', ch)])
                        S.op('pe', lambda e: e.matmul(out=ps2[:, 0:n], lhsT=ones_f[:, :], rhs=sq[ch][:, 0:n],
                                                      start=(ch == 0), stop=(ch == 1)), reads=[('sq', ch), 'ones_f'], writes=['ps2'], inc=(ch == 1))
                    S.op('act', lambda e: e.activation(out=mean[:, 0:n], in_=ps1[:, 0:n], func=AF.Copy, scale=1.0 / 256),
                         reads=['ps1'], writes=['mean'])
                    S.op('dve', lambda e: e.tensor_tensor(out=rstd[:, 0:n], in0=mean[:, 0:n], in1=mean[:, 0:n], op=ALU.mult),
                         reads=['mean'], writes=['rstd'])
                    S.op('dve', lambda e: e.scalar_tensor_tensor(out=rstd[:, 0:n], in0=ps2[:, 0:n], scalar=1.0 / 256, in1=rstd[:, 0:n],
                                                                 op0=ALU.mult, op1=ALU.subtract), reads=['ps2', 'rstd'], writes=['rstd'])
                    S.op('act', lambda e: e.activation(out=rstd[:, 0:n], in_=rstd[:, 0:n], func=AF.Sqrt, bias=epst[:, 0:1], scale=1.0),
                         reads=['rstd', 'epst'], writes=['rstd'])
                    S.op('dve', lambda e: e.reciprocal(out=rstd[:, 0:n], in_=rstd[:, 0:n]), reads=['rstd'], writes=['rstd'])
                    for ch in range(2):
                        S.op('dve', lambda e: e.tensor_tensor(out=sq[ch][:, 0:n], in0=acc[:, ch, c0:c0 + n], in1=mean[:, 0:n], op=ALU.subtract),
                             reads=['acc', 'mean'], writes=[('sq', ch)])
                        S.op('dve', lambda e: e.tensor_tensor(out=sq[ch][:, 0:n], in0=sq[ch][:, 0:n], in1=rstd[:, 0:n], op=ALU.mult),
                             reads=['rstd', ('sq', ch)], writes=[('sq', ch)])
                        S.op('act', lambda e: e.activation(out=zT[:, ch, 0:n], in_=sq[ch][:, 0:n], func=AF.Silu,
                                                           bias=cvp[:, ch * 4 + 2:ch * 4 + 3], scale=cvp[:, ch * 4 + 1:ch * 4 + 2]),
                             reads=[('sq', ch), 'cvp'], writes=['zT'])
                    for dch in range(2):
                        for cc_ in range(2):
                            S.op('pe', lambda e: e.matmul(out=pw[dch][:, 0:n], lhsT=wpwb[:, cc_, dch * 128:(dch + 1) * 128],
                                                          rhs=zT[:, cc_, 0:n], start=(cc_ == 0), stop=(cc_ == 1)),
                                 reads=['zT', 'wpwb'], writes=[('pw', dch)], inc=(cc_ == 1))
                        S.op('act', lambda e: e.copy(out=yT[:, 2 + dch, ycol0 + c0:ycol0 + c0 + n], in_=pw[dch][:, 0:n]),
                             reads=[('pw', dch)], writes=['yT'])
            S.barrier()
    dbg_dump('d_QT', QY[:, 0:4, :], ('QT', 0))
    dbg_dump('d_yT', QY[:, 4:8, :], 'yT')
    if stop_after <= 1:
        return finish(nc, S, es, x_out, ctx_out)

    with ExitStack() as pb:
        KTz = [sb('KTz%d' % i, [128, KEYS], BF, st=pb) for i in range(2)]
        KT = KTz[0]
        Vg = sb('Vg', [128, NKC, 192], BF, st=pb)
        with ExitStack() as pb1:
            wkvb = sb('wkvb', [128, 8, 256], BF, st=pb1)
            xt = [sb('xtb%d' % i, [128, D], st=pb1) for i in range(3)]
            xnb = [sb('xnbb%d' % i, [128, D], BF, st=pb1) for i in range(3)]
            hTt = [sb('hTt%d' % i, [128, 8, 128], BF, st=pb1) for i in range(2)]
            ktmp = [sb('ktmp%d' % i, [128, 384], st=pb1) for i in range(2)]
            krb = [sb('krb%d' % i, [128, 128], BF, st=pb1) for i in range(2)]
            cst = [sb('cstb%d' % i, [128, 64], st=pb1) for i in range(3)]
            ptr = [ps('ptrb%d' % i, [128, 8, 128], BF, st=pb1) for i in range(2)]
            pkv = [ps('pkv%d' % i, [128, 512], st=pb1) for i in range(3)]
            pkt = [ps('pkt%d' % i, [128, 128], BF, st=pb1) for i in range(2)]
            for hh in range(2):
                stv = xt[hh][:, :].rearrange("p (k n) -> p k n", n=256)
                S.dma('sp', stv, I['w_in'].rearrange("(k p) n -> p k n", p=128)[:, hh * 4:hh * 4 + 4, 512:768], writes=[('xtb', hh)])
                S.op('dve', lambda e: e.tensor_copy(out=wkvb[:, hh * 4:hh * 4 + 4, :], in_=stv), reads=[('xtb', hh)], writes=['wkvb'])
            S.op('pool', lambda e: e.memset(KTz[0][64:128, :], 0.0), writes=['KTm'])
            S.op('pool', lambda e: e.memset(KTz[1][0:64, :], 0.0), writes=['KTm'])
            S.op('pool', lambda e: e.memset(Vg[:, :, 64:128], 0.0), writes=['Vg'])
            S.op('pool', lambda e: e.memset(Vg[:, :, 64:65], 1.0), writes=['Vg'])
            def stB1(c):
                b3 = c % 3
                isctx = c < 2
                src = I['ctx'][c * 128:(c + 1) * 128, :] if isctx else I['x_all'][(c - 2) * 128:(c - 1) * 128, :]
                S.dma('sp', xt[b3][:], src, writes=[('xtb', b3)])
                ln_mod_a(xt[b3], ('xtb', b3), 128, xnb[b3], ('xnbb', b3))

            def stB2(c):
                b3 = c % 3; b = c % 2
                isctx = c < 2
                ln_mod_b(128, (0, 1), 1 if isctx else 0, lambda kk: hTt[b][:, kk, :], ('hTt', b),
                         xnb[b3], ('xnbb', b3), ptr[b], ('ptrb', b))
                for kk in range(8):
                    S.op('pe', lambda e: e.matmul(out=pkv[b3][:, 0:256], lhsT=hTt[b][:, kk, :], rhs=wkvb[:, kk, :],
                                                  start=(kk == 0), stop=(kk == 7)),
                         reads=[('hTt', b), 'wkvb'], writes=[('pkv', b3)], inc=(kk == 7))
                S.op('act', lambda e: e.copy(out=Vg[:, c, 0:64], in_=pkv[b3][:, 128:192]), reads=[('pkv', b3)], writes=[('Vg', c)])
                S.op('act', lambda e: e.copy(out=Vg[:, c, 128:192], in_=pkv[b3][:, 192:256]), reads=[('pkv', b3)], writes=[('Vg', c)])
                if not isctx:
                    S.dma('sp', cst[b3][:], I['cs_all'][(c - 2) * 128:(c - 1) * 128, :], writes=[('cstb', b3)])

            def stB3(c):
                b3 = c % 3; b = c % 2
                isctx = c < 2
                rms_rope(pkv[b3][:, 0:128], ('pkv', b3), 2, qkg[:, 64:128], cst[b3], ('cstb', b3), krb[b][:, :], ('krb', b),
                         ktmp[b], ('ktmp', b), rope=not isctx)
                S.op('pe', lambda e: e.transpose(out=pkt[b][:, :], in_=krb[b][:, :], identity=ident_b[:, :]),
                     reads=[('krb', b), 'ident_b'], writes=[('pkt', b)])
                S.op('act', lambda e: e.copy(out=KTz[0][0:64, c * 128:(c + 1) * 128], in_=pkt[b][0:64, :]),
                     reads=[('pkt', b)], writes=[('KT', c)])
                S.op('act', lambda e: e.copy(out=KTz[1][64:128, c * 128:(c + 1) * 128], in_=pkt[b][64:128, :]),
                     reads=[('pkt', b)], writes=[('KT', c)])

            for step in range(NKC + 2):
                if step < NKC:
                    stB1(step)
                if 0 <= step - 1 < NKC:
                    stB2(step - 1)
                if 0 <= step - 2 < NKC:
                    stB3(step - 2)
            S.barrier()
        dbg_dump('d_KT', KT[:], ('KT', 0))
        dbg_dump('d_V', Vg[:].rearrange("p a b -> p (a b)"), ('Vg', 0))
        if stop_after <= 2:
            S.barrier()
            return finish(nc, S, es, x_out, ctx_out)

        with ExitStack() as pb2:
            PT = [sb('PT%d' % i, [128, 2, 512], BF, st=pb2) for i in range(4)]
            rr = sb('rr', [128, 512], st=pb2); bcs = [sb('bcs%d' % i, [128, 512], st=pb2) for i in range(2)]
            O = [ps('O%d' % i, [128, 512], st=pb2) for i in range(4)]
            SP = [ps('SP%d' % i, [128, 2, 512], st=pb2) for i in range(2)]
            qblocks = [(i * 512, 512, list(range(NKC))) for i in range(4)] + [(NTOK, NCTX, [0, 1])]
            pti = 0
            for (q0, n, chunks) in qblocks:
                for g in range(2):
                    gp = slice(g * 64, (g + 1) * 64)
                    vsl = slice(0, 65) if g == 0 else slice(64, 192)
                    pend = []
                    steps = [(c, pr) for c in chunks for pr in range(2)]
                    for si, (c, pr) in enumerate(steps):
                        sp_ = SP[pr]
                        for jj in range(2):
                            j = pr * 2 + jj
                            S.op('pe', lambda e: e.matmul(out=sp_[:, jj, 0:n], lhsT=KTz[g][:, c * 128:(c + 1) * 128],
                                                          rhs=QT[:, j, q0:q0 + n], start=True, stop=True),
                                 reads=[('KT', c), ('QT', q0 // 512)], writes=[('SP', pr)], inc=(jj == 1))
                        pb_ = pti % 4; pti += 1
                        S.op('act', lambda e: e.activation(out=PT[pb_][:, :, 0:n], in_=sp_[:, :, 0:n], func=AF.Exp,
                                                           bias=negm[:, 0:1], scale=0.125),
                             reads=[('SP', pr), 'negm'], writes=[('PT', pb_)])
                        if pend:
                            pend.pop(0)()
                        def pv(c=c, pr=pr, pb_=pb_):
                            for jj in range(2):
                                j = pr * 2 + jj
                                S.op('pe', lambda e: e.matmul(out=O[j][0:(65 if g == 0 else 128), 0:n], lhsT=Vg[:, c, vsl],
                                                              rhs=PT[pb_][:, jj, 0:n], start=(c == chunks[0]), stop=(c == chunks[-1])),
                                     reads=[('Vg', c), ('PT', pb_)], writes=[('O', j)], inc=(jj == 1))
                        pend.append(pv)
                    while pend:
                        pend.pop(0)()
                    srow = 64 if g == 0 else 0
                    for j in range(4):
                        bb = j % 2
                        S.op('dve', lambda e: e.reciprocal(out=rr[srow:srow + 1, 0:n], in_=O[j][srow:srow + 1, 0:n]),
                             reads=[('O', j)], writes=['rr'])
                        S.op('pe', lambda e: e.matmul(out=SP[bb][:, 0, 0:n], lhsT=ones_f[srow:srow + 1, :], rhs=rr[srow:srow + 1, 0:n],
                                                      start=True, stop=True), reads=['rr', 'ones_f'], writes=[('SP', bb)])
                        S.op('act', lambda e: e.copy(out=bcs[bb][:, 0:n], in_=SP[bb][:, 0, 0:n]), reads=[('SP', bb)], writes=[('bcs', bb)])
                        S.op('dve', lambda e: e.tensor_tensor(out=QT[gp, j, q0:q0 + n], in0=O[j][gp, 0:n], in1=bcs[bb][gp, 0:n], op=ALU.mult),
                             reads=[('O', j), ('bcs', bb)], writes=[('QT', q0 // 512)])
            S.barrier()
    if dbg:
        dbg_dump('d_QT', QY[:, 0:4, :], ('QT', 0))
    if stop_after <= 3:
        S.barrier()
        return finish(nc, S, es, x_out, ctx_out)

    x_res = sb('x_res', [128, 18, D])
    GT = sb('GT', [32, NTOK + NCTX])
    tilesC = [('own', t, t * 128) for t in range(NTI)] + [('ctx', t, NTOK + t * 128) for t in range(2)]

    LN_ = {}

    def post_ln(tI, pre, prekey):
        lnbc = LN_['t']
        rstd_, nmr_, k = ln_stats(pre, prekey)
        S.op('act', lambda e: e.activation(out=pre[:, :], in_=pre[:, :], func=AF.Identity, bias=nmr_, scale=rstd_),
             reads=[prekey, k], writes=[prekey])
        S.op('dve', lambda e: e.tensor_tensor(out=pre[:, :], in0=pre[:, :], in1=lnbc[:, 0, :], op=ALU.mult),
             reads=[prekey, 'lnbc'], writes=[prekey])
        S.op('pool', lambda e: e.tensor_tensor(out=x_res[:, tI, :], in0=pre[:, :], in1=lnbc[:, 1, :], op=ALU.add),
             reads=[prekey, 'lnbc'], writes=[('xres', tI)])

    with ExitStack() as pc:
        lnbc = sb('lnbc', [128, 2, D], st=pc)
        LN_['t'] = lnbc
        woutb = sb('woutb', [128, 8, D], BF, st=pc)
        wst = [sb('wstc%d' % i, [128, 2, D], st=pc) for i in range(2)]
        xt = [sb('xtc%d' % i, [128, D], st=pc) for i in range(2)]
        pre = [sb('pre%d' % i, [128, D], st=pc) for i in range(2)]
        py = [ps('py%d' % i, [128, 2, 512], st=pc) for i in range(2)]
        S.dma('sp', lnbc[:, 0, :], I['lnv'][:, 0:D].to_broadcast([128, D]), writes=['lnbc'])
        S.dma('sp', lnbc[:, 1, :], I['lnv'][:, D:2 * D].to_broadcast([128, D]), writes=['lnbc'])
        for j in range(4):
            b = j % 2
            for g in range(2):
                r0 = g * 256 + j * 64
                S.dma('sp', wst[b][g * 64:(g + 1) * 64, 0, :], I['w_out'][r0:r0 + 64, :], writes=[('wstc', b)])
            S.dma('sp', wst[b][:, 1, :], I['w_out'][512 + j * 128:512 + (j + 1) * 128, :], writes=[('wstc', b)])
            S.op('dve', lambda e: e.tensor_copy(out=woutb[:, j, :], in_=wst[b][:, 0, :]), reads=[('wstc', b)], writes=['woutb'])
            S.op('pool', lambda e: e.tensor_copy(out=woutb[:, 4 + j, :], in_=wst[b][:, 1, :]), reads=[('wstc', b)], writes=['woutb'])
        for tI, (kind, t, col0) in enumerate(tilesC):
            b = tI % 2
            src = I['x_own'] if kind == 'own' else I['ctx']
            S.dma('sp', xt[b][:], src[t * 128:(t + 1) * 128, :], writes=[('xtc', b)])
            for nh in range(2):
                for kk in range(8):
                    lh = QT[:, kk, col0:col0 + 128] if kk < 4 else yT[:, kk - 4, col0:col0 + 128]
                    S.op('pe', lambda e: e.matmul(out=py[b][:, nh, :], lhsT=lh, rhs=woutb[:, kk, nh * 512:(nh + 1) * 512],
                                                  start=(kk == 0), stop=(kk == 7)),
                         reads=[('QT', col0 // 512), 'yT', 'woutb'], writes=[('py', b)], inc=(kk == 7 and nh == 1))
            gi = 1 if kind == 'ctx' else 0
            S.op('dve', lambda e: e.tensor_tensor(out=pre[b][:, :], in0=py[b][:, :, :].rearrange("p a b -> p (a b)"), in1=gbc[:, gi, :], op=ALU.mult),
                 reads=[('py', b), 'gbc'], writes=[('pre', b)])
            S.op('dve', lambda e: e.scalar_tensor_tensor(out=pre[b][:, :], in0=xt[b][:, :], scalar=ALPHA, in1=pre[b][:, :],
                                                         op0=ALU.mult, op1=ALU.add), reads=[('xtc', b), ('pre', b)], writes=[('pre', b)])
            post_ln(tI, pre[b], ('pre', b))
        S.barrier()
    dbg_dump('d_xres', x_res[:].rearrange("p a b -> p (a b)"), ('xres', 0))
    if stop_after <= 4:
        S.barrier()
        return finish(nc, S, es, x_out, ctx_out)

    hT = QY
    with ExitStack() as pd:
        wrb = sb('wrb', [128, 8, 36], BF, st=pd); wrs = sb('wrs', [128, 8, 36], st=pd)
        brb = sb('brb', [1, 36], BF, st=pd); brs = sb('brs', [1, 36], st=pd)
        ones_b = sb('ones_b', [1, 128], BF, st=pd)
        xnb = [sb('xnbd%d' % i, [128, D], BF, st=pd) for i in range(2)]
        rt = sb('rt', [128, 4, 64], st=pd)
        ptr = [ps('ptrd%d' % i, [128, 8, 128], BF, st=pd) for i in range(2)]
        prr = [ps('prr%d' % i, [128, 64], st=pd) for i in range(2)]
        pgt = [ps('pgt%d' % i, [32, 128], st=pd) for i in range(2)]
        S.dma('sp', wrs[:], I['w_r'].rearrange("(k p) n -> p k n", p=128), writes=['wrs'])
        S.op('dve', lambda e: e.tensor_copy(out=wrb[:], in_=wrs[:]), reads=['wrs'], writes=['wrb'])
        S.dma('sp', brs[:], I['b_r'], writes=['brs'])
        S.op('dve', lambda e: e.tensor_copy(out=brb[:], in_=brs[:]), reads=['brs'], writes=['brb'])
        S.op('dve', lambda e: e.memset(ones_b[:], 1.0), writes=['ones_b'])
        for tI, (kind, t, col0) in enumerate(tilesC):
            b = tI % 2
            w = 1 if kind == 'ctx' else 0
            xr = x_res[:, tI, :]
            ln_mod_T(xr, ('xres', tI), 128, (2, 3), w, lambda kk: hT[:, kk, col0:col0 + 128], ('hT2', tI),
                     xnb[b], ('xnbd', b), ptr[b], ('ptrd', b))
            S.op('pool', lambda e: e.tensor_scalar(out=xr, in0=xr, scalar1=ALPHA, scalar2=None, op0=ALU.mult),
                 reads=[('xres', tI), ('xnbd', b)], writes=[('xres', tI)])
            for kk in range(8):
                S.op('pe', lambda e: e.matmul(out=prr[b][:, 0:36], lhsT=hT[:, kk, col0:col0 + 128], rhs=wrb[:, kk, :],
                                              start=(kk == 0), stop=False), reads=[('hT2', tI), 'wrb'], writes=[('prr', b)], inc=False)
            S.op('pe', lambda e: e.matmul(out=prr[b][:, 0:36], lhsT=ones_b[0:1, :], rhs=brb[0:1, :], start=False, stop=True),
                 reads=['ones_b', 'brb'], writes=[('prr', b)])
            r = rt[:, tI % 4, :]; rk = ('rt', tI % 4)
            lg = r[:, 0:36]
            S.op('act', lambda e: e.copy(out=lg, in_=prr[b][:, 0:36]), reads=[('prr', b)], writes=[rk])
            S.op('dve', lambda e: e.tensor_reduce(out=r[:, 36:37], in_=r[:, 0:4], axis=AX.X, op=ALU.max), reads=[rk], writes=[rk])
            S.op('dve', lambda e: e.tensor_scalar(out=r[:, 40:44], in0=r[:, 0:4], scalar1=r[:, 36:37], scalar2=None, op0=ALU.is_ge),
                 reads=[rk], writes=[rk])
            S.op('dve', lambda e: e.tensor_scalar(out=r[:, 37:38], in0=r[:, 36:37], scalar1=-1.0, scalar2=None, op0=ALU.mult),
                 reads=[rk], writes=[rk])
            S.op('act', lambda e: e.activation(out=r[:, 44:48], in_=r[:, 0:4], func=AF.Exp, bias=r[:, 37:38], scale=1.0,
                                               accum_out=r[:, 38:39]), reads=[rk], writes=[rk])
            S.op('dve', lambda e: e.tensor_scalar(out=r[:, 40:44], in0=r[:, 40:44], scalar1=-1.0, scalar2=1.0e4, op0=ALU.add, op1=ALU.mult),
                 reads=[rk], writes=[rk])
            le = r[:, 4:36].rearrange("p (g x) -> p g x", x=8)
            S.op('dve', lambda e: e.tensor_tensor(out=le, in0=le, in1=r[:, 40:44].unsqueeze(2).to_broadcast([128, 4, 8]), op=ALU.add),
                 reads=[rk], writes=[rk])
            S.op('dve', lambda e: e.max(out=r[:, 48:56], in_=r[:, 4:36]), reads=[rk], writes=[rk])
            S.op('dve', lambda e: e.tensor_scalar(out=r[:, 39:40], in0=r[:, 48:49], scalar1=-1.0, scalar2=None, op0=ALU.mult),
                 reads=[rk], writes=[rk])
            S.op('act', lambda e: e.activation(out=r[:, 4:36], in_=r[:, 4:36], func=AF.Exp, bias=r[:, 39:40], scale=1.0),
                 reads=[rk], writes=[rk])
            S.op('act', lambda e: e.activation(out=r[:, 56:57], in_=r[:, 49:50], func=AF.Exp, bias=r[:, 39:40], scale=1.0),
                 reads=[rk], writes=[rk])
            S.op('dve', lambda e: e.tensor_scalar(out=r[:, 57:58], in0=r[:, 56:57], scalar1=1.0, scalar2=r[:, 38:39], op0=ALU.add, op1=ALU.mult),
                 reads=[rk], writes=[rk])
            S.op('dve', lambda e: e.reciprocal(out=r[:, 57:58], in_=r[:, 57:58]), reads=[rk], writes=[rk])
            S.op('dve', lambda e: e.scalar_tensor_tensor(out=r[:, 4:36], in0=r[:, 4:36], scalar=r[:, 56:57], in1=r[:, 4:36],
                                                         op0=ALU.is_ge, op1=ALU.mult), reads=[rk], writes=[rk])
            S.op('dve', lambda e: e.tensor_scalar(out=r[:, 4:36], in0=r[:, 4:36], scalar1=r[:, 57:58], scalar2=None, op0=ALU.mult),
                 reads=[rk], writes=[rk])
            S.op('pe', lambda e: e.transpose(out=pgt[b][:, :], in_=r[:, 4:36], identity=ident_f[:, :]),
                 reads=[rk, 'ident_f'], writes=[('pgt', b)])
            S.op('act', lambda e: e.copy(out=GT[:, col0:col0 + 128], in_=pgt[b][:, :]), reads=[('pgt', b)], writes=['GT'])
        S.barrier()
    dbg_dump('d_GT', GT[:], 'GT')
    if stop_after <= 5:
        S.barrier()
        return finish(nc, S, es, x_out, ctx_out)

    allhT2 = [('hT2', i) for i in range(18)]
    with ExitStack() as pe_:
        wgb = [sb('wgb%d' % i, [128, 8, 256], BF, st=pe_) for i in range(2)]
        wub = [sb('wub%d' % i, [128, 8, 256], BF, st=pe_) for i in range(2)]
        wdb = [sb('wdb%d' % i, [128, 2, D], BF, st=pe_) for i in range(2)]
        wdc = [sb('wdc%d' % i, [128, 2, D], BF, st=pe_) for i in range(2)]
        stg = [sb('stg%d' % i, [128, 2048], st=pe_) for i in range(2)]
        Gs = [sb('Gs%d' % i, [128, 512], st=pe_) for i in range(2)]
        st_ = [sb('st_s%d' % i, [128, 512], st=pe_) for i in range(2)]
        tt_ = [sb('tt_s%d' % i, [128, 512], st=pe_) for i in range(2)]
        aT = [sb('aT%d' % i, [128, 2, 512], BF, st=pe_) for i in range(2)]
        pg = [ps('pg%d' % i, [128, 512], st=pe_) for i in range(2)]
        pu = [ps('pu%d' % i, [128, 512], st=pe_) for i in range(2)]
        pgb = ps('pgb', [128, 512], st=pe_)
        pyd = [ps('pyd%d' % i, [128, 512], st=pe_) for i in range(3)]
        si = 0; YD = [0]; cnt2 = 0; pend_dn = []
        tblocks = [(i * 512, 512, 'own') for i in range(4)] + [(NTOK, NCTX, 'ctx')]
        for e_ in range(NEXP):
            for fh in range(2):
                wb = (e_ * 2 + fh) % 2
                for which in range(3):
                    s_ = si % 2; si += 1
                    if which < 2:
                        srcw = (I['w_eg'] if which == 0 else I['w_eu'])[e_].rearrange("(k p) f -> p k f", p=128)[:, :, fh * 256:(fh + 1) * 256]
                        S.dma('sp', stg[s_][:, :].rearrange("p (k f) -> p k f", f=256), srcw, writes=[('stg', s_)])
                        dst = (wgb if which == 0 else wub)[wb]
                        S.op('pool' if which == 0 else 'act',
                             (lambda e: e.tensor_copy(out=dst[:], in_=stg[s_][:, :].rearrange("p (k f) -> p k f", f=256))) if which == 0 else
                             (lambda e: e.copy(out=dst[:], in_=stg[s_][:, :].rearrange("p (k f) -> p k f", f=256))),
                             reads=[('stg', s_)], writes=[('wg' if which == 0 else 'wu', wb)])
                    else:
                        srcw = I['w_ed'][e_].rearrange("(c p) n -> p c n", p=128)[:, fh * 2:fh * 2 + 2, :]
                        S.dma('sp', stg[s_][:, :].rearrange("p (c n) -> p c n", n=D), srcw, writes=[('stg', s_)])
                        sv = stg[s_][:, :].rearrange("p (c n) -> p c n", n=D)
                        S.op('pool', lambda e: e.tensor_tensor(out=wdb[wb][:], in0=sv, in1=gbc[:, 2:3, :].to_broadcast([128, 2, D]), op=ALU.mult),
                             reads=[('stg', s_), 'gbc'], writes=[('wd', wb)])
                        S.op('dve', lambda e: e.tensor_tensor(out=wdc[wb][:], in0=sv, in1=gbc[:, 3:4, :].to_broadcast([128, 2, D]), op=ALU.mult),
                             reads=[('stg', s_), 'gbc'], writes=[('wdc', wb)])
                for (c0, n, kind) in tblocks:
                    gb = cnt2 % 2; cnt2 += 1
                    S.op('pe', lambda e: e.matmul(out=pgb[:, 0:n], lhsT=ident_f[0:32, e_:e_ + 1].to_broadcast([32, 128]), rhs=GT[:, c0:c0 + n], start=True, stop=True),
                         reads=['ident_f', 'GT'], writes=['pgb'])
                    S.op('act', lambda e: e.copy(out=Gs[gb][:, 0:n], in_=pgb[:, 0:n]), reads=['pgb'], writes=[('Gs', gb)])
                    for fc in range(2):
                        for kk in range(8):
                            S.op('pe', lambda e: e.matmul(out=pg[fc][:, 0:n], lhsT=wgb[wb][:, kk, fc * 128:(fc + 1) * 128], rhs=hT[:, kk, c0:c0 + n],
                                                          start=(kk == 0), stop=(kk == 7)), reads=allhT2 + [('wg', wb)], writes=[('pg', fc)], inc=(kk == 7))
                        for kk in range(8):
                            S.op('pe', lambda e: e.matmul(out=pu[fc][:, 0:n], lhsT=wub[wb][:, kk, fc * 128:(fc + 1) * 128], rhs=hT[:, kk, c0:c0 + n],
                                                          start=(kk == 0), stop=(kk == 7)), reads=allhT2 + [('wu', wb)], writes=[('pu', fc)], inc=(kk == 7))
                        S.op('act', lambda e: e.activation(out=st_[fc][:, 0:n], in_=pg[fc][:, 0:n], func=AF.Silu), reads=[('pg', fc)], writes=[('st', fc)])
                        S.op('dve', lambda e: e.tensor_tensor(out=tt_[fc][:, 0:n], in0=pu[fc][:, 0:n], in1=Gs[gb][:, 0:n], op=ALU.mult),
                             reads=[('pu', fc), ('Gs', gb)], writes=[('tt', fc)])
                        S.op('pool', lambda e: e.tensor_tensor(out=aT[gb][:, fc, 0:n], in0=st_[fc][:, 0:n], in1=tt_[fc][:, 0:n], op=ALU.mult),
                             reads=[('st', fc), ('tt', fc)], writes=[('aT', gb)])
                    def down(gb=gb, wb=wb, kind=kind, c0=c0, n=n):
                        wdsel = wdc if kind == 'ctx' else wdb
                        for tt in range(n // 128):
                            tI = (c0 + tt * 128) // 128
                            for nh in range(2):
                                y_ = YD[0] % 3; YD[0] += 1
                                for fc in range(2):
                                    S.op('pe', lambda e: e.matmul(out=pyd[y_][:, :], lhsT=aT[gb][:, fc, tt * 128:(tt + 1) * 128],
                                                                  rhs=wdsel[wb][:, fc, nh * 512:(nh + 1) * 512], start=(fc == 0), stop=(fc == 1)),
                                         reads=[('aT', gb), ('wdc' if kind == 'ctx' else 'wd', wb)], writes=[('pyd', y_)], inc=(fc == 1))
                                xs = x_res[:, tI, nh * 512:(nh + 1) * 512]
                                S.op('dve', lambda e: e.tensor_tensor(out=xs, in0=pyd[y_][:, :], in1=xs, op=ALU.add),
                                     reads=[('pyd', y_), ('xres', tI)], writes=[('xres', tI)])
                    if pend_dn:
                        pend_dn.pop(0)()
                    pend_dn.append(down)
        while pend_dn:
            pend_dn.pop(0)()
        S.barrier()

    with ExitStack() as pf_:
        lnbc = sb('lnbc2', [128, 2, D], st=pf_)
        LN_['t'] = lnbc
        S.dma('sp', lnbc[:, 0, :], I['lnv'][:, 2 * D:3 * D].to_broadcast([128, D]), writes=['lnbc'])
        S.dma('sp', lnbc[:, 1, :], I['lnv'][:, 3 * D:4 * D].to_broadcast([128, D]), writes=['lnbc'])
        pre = [sb('pree%d' % i, [128, D], st=pf_) for i in range(2)]
        for tI, (kind, t, col0) in enumerate(tilesC):
            b = tI % 2
            S.op('act', lambda e: e.copy(out=pre[b][:, :], in_=x_res[:, tI, :]), reads=[('xres', tI)], writes=[('pree', b)])
            post_ln(tI, pre[b], ('pree', b))
            dst = x_out[t * 128:(t + 1) * 128, :] if kind == 'own' else ctx_out[t * 128:(t + 1) * 128, :]
            S.dma('sp', dst, x_res[:, tI, :], reads=[('xres', tI)], writes=[('out', tI)])
        S.barrier()
    return finish(nc, S, es, x_out, ctx_out)


def finish(nc, S, es, x_out, ctx_out):
    S.barrier()
    es.close()
    return nc


def _rope_cs(n_tok):
    t = np.arange(n_tok)
    row = (t // GRID_W).astype(np.float32); col = (t % GRID_W).astype(np.float32)
    inv = (10000.0 ** (-np.arange(16, dtype=np.float32) / 16)).astype(np.float32)
    ang = np.stack([row[:, None] * inv, col[:, None] * inv], axis=1).astype(np.float32)
    return np.concatenate([np.cos(ang).reshape(n_tok, 32), np.sin(ang).reshape(n_tok, 32)], axis=1).astype(np.float32)


def _invcnt(n, t0, L):
    t = np.arange(t0, t0 + L)
    out = np.zeros((128, 2 * L), np.float32)
    for ch, (wa, wb) in enumerate(((2, 4), (8, 16))):
        for h, w in enumerate((wa, wb)):
            lo = np.clip(t - w // 2, 0, n); hi = np.clip(t + (w - w // 2), 0, n)
            out[h * 64:(h + 1) * 64, ch * L:(ch + 1) * L] = (1.0 / (hi - lo).astype(np.float32))[None, :]
    return out


_NC_CACHE = {}


def _layer_inputs(l, x, ctx_x, P):
    f = np.float32
    common = {
        'x_all': np.ascontiguousarray(x, f), 'ctx': np.ascontiguousarray(ctx_x, f),
        'cT': np.ascontiguousarray(np.stack([P['c'].reshape(8, 128).T, P['c_ctx'].reshape(8, 128).T], axis=2).reshape(128, 16), f),
        'w_mod': np.ascontiguousarray(P['w_mod'][l], f), 'b_mod': np.ascontiguousarray(P['b_mod'][l][None, :], f),
        'w_in': np.ascontiguousarray(P['w_in'][l], f),
        'qk_gain': np.ascontiguousarray(np.concatenate([P['q_gain'][l], P['k_gain'][l]])[None, :], f),
        'w_pool': np.ascontiguousarray(P['w_pool'][l].reshape(256, 64), f),
        'cvp': np.ascontiguousarray(np.stack([P['b_dw'][l].reshape(2, 128).T, P['cv_ln_g'][l].reshape(2, 128).T,
                                              P['cv_ln_b'][l].reshape(2, 128).T, P['pool_scale'][l].reshape(2, 128).T],
                                             axis=2).reshape(128, 8), f),
        'w_dwT': np.ascontiguousarray(P['w_dw'][l].reshape(31, 2, 128).transpose(2, 1, 0).reshape(128, 62), f),
        'w_pw': np.ascontiguousarray(P['w_cv_pw'][l], f), 'w_out': np.ascontiguousarray(P['w_out'][l], f),
        'lnv': np.ascontiguousarray(np.concatenate([P['ln1_g'][l], P['ln1_b'][l], P['ln2_g'][l], P['ln2_b'][l]])[None, :], f),
        'w_r': np.ascontiguousarray(np.concatenate([P['w_rg'][l], P['w_re'][l]], axis=1), f),
        'b_r': np.ascontiguousarray(np.concatenate([P['b_rg'][l], P['b_re'][l]])[None, :], f),
        'w_eg': np.ascontiguousarray(P['w_e_gate'][l], f), 'w_eu': np.ascontiguousarray(P['w_e_up'][l], f),
        'w_ed': np.ascontiguousarray(P['w_e_down'][l], f),
        'cs_all': _rope_cs(SEQ),
        'sel': np.ascontiguousarray(np.repeat(np.eye(32, dtype=f), 128, axis=1)),
        'selrow': np.ascontiguousarray(np.concatenate([np.repeat(np.eye(64, dtype=f)[:, 0:1], 128, 1),
                                                       np.repeat(np.eye(64, dtype=f)[:, 32:33], 128, 1)], axis=1)),
        'ident_f': np.eye(128, dtype=f), 'ident_b': np.eye(128, dtype=f).astype(ml_dtypes.bfloat16),
    }
    ic_ctx = _invcnt(NCTX, 0, NCTX)
    maps = []
    for r in range(NCORE):
        m = dict(common)
        t0 = r * NTOK
        m['x_own'] = np.ascontiguousarray(x[t0:t0 + NTOK], f)
        xh = np.zeros((32, D), f); hm = np.zeros((128, 32), f)
        if r > 0:
            xh[0:16] = x[t0 - 16:t0]; hm[:, 0:16] = 1.0
        if r < NCORE - 1:
            xh[16:32] = x[t0 + NTOK:t0 + NTOK + 16]; hm[:, 16:32] = 1.0
        m['x_halo'] = xh; m['hmask'] = hm
        m['cs_own'] = np.ascontiguousarray(common['cs_all'][t0:t0 + NTOK])
        m['invcnt'] = np.ascontiguousarray(np.concatenate([_invcnt(SEQ, t0, NTOK), ic_ctx], axis=1))
        maps.append(m)
    return maps


def kernel(**inputs):
    P = {k: np.asarray(v) for k, v in inputs.items()}
    x = P['x'][0]
    ctx_x = P['ctx'][0]
    P['c'] = P['c'].reshape(-1)
    if 'nc' not in _NC_CACHE:
        _NC_CACHE['nc'] = build()
    nc = _NC_CACHE['nc']
    for l in range(DEPTH):
        maps = _layer_inputs(l, x, ctx_x, P)
        res = run_bass_kernel_spmd(nc, maps, core_ids=list(range(NCORE)))
        x = np.concatenate([np.asarray(res.results[r]['x_out']) for r in range(NCORE)], axis=0)
        ctx_x = np.asarray(res.results[0]['ctx_out'])
    return x[None].astype(np.float32)
```

```python
import numpy as np
import ml_dtypes
from contextlib import ExitStack
import concourse.bass as bass
import concourse.mybir as mybir
from concourse.bass_utils import run_bass_kernel_spmd

F32 = mybir.dt.float32
BF = mybir.dt.bfloat16
ALU = mybir.AluOpType
AF = mybir.ActivationFunctionType
AX = mybir.AxisListType

D = 1024
SEQ = 16384
NCORE = 8
NTOK = SEQ // NCORE
NTI = NTOK // 128
NCTX = 256
NKC = (SEQ + NCTX) // 128
KEYS = SEQ + NCTX
HTW = NTOK + NCTX + 32
HAL0 = NTOK + NCTX
NEXP = 32
DEPTH = 2
ALPHA = (2 * DEPTH) ** 0.25
EPS = 1e-6
GRID_W = 64


class Sched:
    def __init__(self, nc, es):
        self.nc = nc
        self.E = {'pe': nc.tensor, 'act': nc.scalar, 'dve': nc.vector, 'pool': nc.gpsimd, 'sp': nc.sync}
        self.sem = {k: es.enter_context(nc.semaphore('c_' + k)) for k in self.E}
        self.cnt = {k: 0 for k in self.E}
        self.seen = {k: {} for k in self.E}
        self.W = {}
        self.R = {}
        self.ND = 32
        self.dsem = [es.enter_context(nc.semaphore('d%d' % i)) for i in range(self.ND)]
        self.dval = [0] * self.ND
        self.di = 0
        self.nwait = 0

    def _wait(self, eng, dep):
        sem, val, key = dep
        if self.seen[eng].get(key, 0) >= val:
            return
        self.seen[eng][key] = val
        self.E[eng].wait_ge(sem, val)
        self.nwait += 1

    def _deps(self, eng, reads, writes):
        for k in reads:
            d = self.W.get(k)
            if d is not None and not (eng == 'pe' and d[2] == 'pe'):
                self._wait(eng, d)
        for k in writes:
            d = self.W.get(k)
            if d is not None and not (eng == 'pe' and d[2] == 'pe'):
                self._wait(eng, d)
            for d in self.R.get(k, {}).values():
                if not (eng == 'pe' and d[2] == 'pe'):
                    self._wait(eng, d)

    def _reg(self, dep, reads, writes):
        for k in writes:
            self.W[k] = dep
            self.R[k] = {}
        for k in reads:
            self.R.setdefault(k, {})[dep[2]] = dep

    def op(self, eng, fn, reads=(), writes=(), inc=True):
        self._deps(eng, reads, writes)
        ins = fn(self.E[eng])
        if inc:
            self.cnt[eng] += 1
            ins.then_inc(self.sem[eng], 1)
            dep = (self.sem[eng], self.cnt[eng], eng)
        else:
            dep = (self.sem[eng], self.cnt[eng] + 1, eng)
        self._reg(dep, reads, writes)

    def dma(self, q, out, in_, reads=(), writes=()):
        i = self.di
        self.di = (self.di + 1) % self.ND
        if self.dval[i] > 0:
            self._wait(q, (self.dsem[i], self.dval[i], ('d', i)))
        self._deps(q, reads, writes)
        self.dval[i] += 16
        self.E[q].dma_start(out=out, in_=in_).then_inc(self.dsem[i], 16)
        dep = (self.dsem[i], self.dval[i], ('d', i))
        self._reg(dep, reads, writes)

    def barrier(self, engs=None):
        engs = engs or list(self.E)
        for e in engs:
            for f in self.E:
                if f != e and self.cnt[f] > 0:
                    self._wait(e, (self.sem[f], self.cnt[f], f))
            for i in range(self.ND):
                if self.dval[i] > 0:
                    self._wait(e, (self.dsem[i], self.dval[i], ('d', i)))


INPUTS = [
    ('x_all', [SEQ, D], F32), ('x_own', [NTOK, D], F32), ('x_halo', [32, D], F32), ('ctx', [NCTX, D], F32),
    ('cT', [128, 16], F32), ('w_mod', [D, 6 * D], F32), ('b_mod', [1, 6 * D], F32), ('w_in', [D, 1536], F32),
    ('qk_gain', [1, 128], F32), ('w_pool', [256, 64], F32), ('cvp', [128, 8], F32), ('w_dwT', [128, 62], F32),
    ('w_pw', [256, 256], F32), ('w_out', [D, D], F32), ('lnv', [1, 4 * D], F32), ('w_r', [D, 36], F32),
    ('b_r', [1, 36], F32), ('w_eg', [NEXP, D, 512], F32), ('w_eu', [NEXP, D, 512], F32),
    ('w_ed', [NEXP, 512, D], F32), ('cs_all', [SEQ, 64], F32), ('cs_own', [NTOK, 64], F32),
    ('invcnt', [128, 2 * (NTOK + NCTX)], F32), ('hmask', [128, 32], F32), ('sel', [32, NEXP * 128], F32),
    ('selrow', [64, 256], F32), ('ident_f', [128, 128], F32), ('ident_b', [128, 128], BF),
]


def build(stop_after=99, dbg=False):
    nc = bass.Bass("TRN2", target_bir_lowering=False)
    I = {n: nc.dram_tensor(n, list(s), dt, kind="ExternalInput").ap() for n, s, dt in INPUTS}
    x_out = nc.dram_tensor('x_out', [NTOK, D], F32, kind="ExternalOutput").ap()
    ctx_out = nc.dram_tensor('ctx_out', [NCTX, D], F32, kind="ExternalOutput").ap()
    DBG = {}
    if dbg:
        for n, s, dt in [('d_modT', [128, 64], F32), ('d_gbc', [128, 4096], F32), ('d_KT', [128, KEYS], BF),
                         ('d_V', [128, NKC * 192], BF), ('d_QT', [128, 4, NTOK + NCTX], BF),
                         ('d_yT', [128, 4, NTOK + NCTX], BF), ('d_hT', [128, 8 * HTW], BF),
                         ('d_xres', [128, 18 * D], F32), ('d_GT', [32, NTOK + NCTX], F32)]:
            DBG[n] = nc.dram_tensor(n, list(s), dt, kind="ExternalOutput").ap()

    es = ExitStack()
    S = Sched(nc, es)

    def sb(name, shape, dt=F32, st=None):
        return (st or es).enter_context(nc.sbuf_tensor('s_' + name, list(shape), dt))

    def ps(name, shape, dt=F32, st=None):
        return (st or es).enter_context(nc.psum_tensor('p_' + name, list(shape), dt))

    def dbg_dump(name, ap, key):
        if dbg:
            S.barrier()
            S.dma('sp', DBG[name], ap, reads=[key], writes=[('dbgout', name)])
            S.barrier()

    ident_f = sb('ident_f', [128, 128]); ident_b = sb('ident_b', [128, 128], BF)
    ones_f = sb('ones_f', [128, 128]); epst = sb('epst', [128, 1])
    modT = sb('modT', [128, 64])
    gbc = sb('gbc', [128, 4, D])
    qkg = sb('qkg', [128, 128]); cvp = sb('cvp', [128, 8]); wdwT = sb('wdwT', [128, 62])
    negm = sb('negm', [128, 1]); hmask = sb('hmask', [128, 32])
    QY = sb('QY', [128, 8, NTOK + NCTX], BF)
    QT = QY[:, 0:4, :]
    yT = QY[:, 4:8, :]
    sm = sb('sm', [128, 8, 64])
    smi = [0]

    def smslot():
        smi[0] = (smi[0] + 1) % 8
        return smi[0]

    S.dma('sp', ident_f[:], I['ident_f'], writes=['ident_f'])
    S.dma('sp', ident_b[:], I['ident_b'], writes=['ident_b'])
    S.dma('sp', qkg[:], I['qk_gain'].to_broadcast([128, 128]), writes=['qkg'])
    S.dma('sp', cvp[:], I['cvp'], writes=['cvp'])
    S.dma('sp', wdwT[:], I['w_dwT'], writes=['wdwT'])
    S.dma('sp', hmask[:], I['hmask'], writes=['hmask'])
    S.op('dve', lambda e: e.memset(ones_f[:], 1.0), writes=['ones_f'])
    S.op('dve', lambda e: e.memset(epst[:], EPS), writes=['epst'])
    sl = smslot()
    S.op('dve', lambda e: e.tensor_tensor(out=sm[:, sl, 0:64], in0=qkg[:, 0:64], in1=qkg[:, 64:128], op=ALU.mult),
         reads=['qkg'], writes=[('sm', sl)])
    S.op('dve', lambda e: e.tensor_reduce(out=negm[:], in_=sm[:, sl, 0:64], axis=AX.X, op=ALU.max,
                                          apply_absolute_value=True), reads=[('sm', sl)], writes=['negm'])
    S.op('dve', lambda e: e.tensor_scalar(out=negm[:], in0=negm[:], scalar1=-8.0, scalar2=None, op0=ALU.mult),
         reads=['negm'], writes=['negm'])

    def ln_stats(src, srckey):
        return ln_stats_b(ln_stats_a(src, srckey))

    def ln_stats_a(src, srckey):
        s = smslot()
        k = ('sm', s)
        S.op('dve', lambda e: e.bn_stats(out=sm[:, s, 0:6], in_=src[:, 0:512]), reads=[srckey], writes=[k])
        S.op('dve', lambda e: e.bn_stats(out=sm[:, s, 6:12], in_=src[:, 512:1024]), reads=[srckey, k], writes=[k])
        S.op('dve', lambda e: e.bn_aggr(out=sm[:, s, 12:14], in_=sm[:, s, 0:12]), reads=[k], writes=[k])
        S.op('act', lambda e: e.activation(out=sm[:, s, 14:15], in_=sm[:, s, 13:14], func=AF.Sqrt,
                                           bias=epst[:, 0:1], scale=1.0), reads=[k, 'epst'], writes=[k])
        return s

    def ln_stats_b(s):
        k = ('sm', s)
        S.op('dve', lambda e: e.reciprocal(out=sm[:, s, 14:15], in_=sm[:, s, 14:15]), reads=[k], writes=[k])
        S.op('dve', lambda e: e.tensor_scalar(out=sm[:, s, 15:16], in0=sm[:, s, 12:13], scalar1=sm[:, s, 14:15],
                                              scalar2=-1.0, op0=ALU.mult, op1=ALU.mult), reads=[k], writes=[k])
        return sm[:, s, 14:15], sm[:, s, 15:16], k

    with ExitStack() as p0:
        cc = sb('cc', [128, 8, 64], st=p0); cTt = sb('cTt', [128, 16], st=p0)
        bmod = sb('bmod', [1, 6 * D], st=p0)
        wst = [sb('wst%d' % i, [128, 8, 512], st=p0) for i in range(2)]
        mrow = [sb('mrow%d' % i, [64, 512], st=p0) for i in range(2)]
        selrow = sb('selrow', [64, 256], st=p0)
        pm = [ps('pm%d' % i, [128, 512], st=p0) for i in range(2)]
        pt = [ps('ptm%d' % i, [128, 512], st=p0) for i in range(2)]
        S.dma('sp', cTt[:], I['cT'], writes=['cTt'])
        S.dma('sp', bmod[:], I['b_mod'], writes=['bmod'])
        S.dma('sp', selrow[:], I['selrow'], writes=['selrow'])
        S.op('dve', lambda e: e.memset(cc[:], 0.0), writes=['cc'])
        cTv = cTt[:].rearrange("p (k w) -> p k w", w=2)
        for w in range(2):
            S.op('act', lambda e: e.activation(out=cc[:, :, 32 * w:32 * w + 1], in_=cTv[:, :, w:w + 1], func=AF.Silu),
                 reads=['cTt', 'cc'], writes=['cc'])
        wmv = I['w_mod'].rearrange("(k p) n -> p k n", p=128)
        for nb in range(12):
            b = nb % 2
            S.dma('sp', wst[b][:], wmv[:, :, nb * 512:(nb + 1) * 512], writes=[('wst', b)])
            for k in range(8):
                S.op('pe', lambda e: e.matmul(out=pm[b][0:64, :], lhsT=cc[:, k, :], rhs=wst[b][:, k, :],
                                              start=(k == 0), stop=False),
                     reads=['cc', ('wst', b)], writes=[('pm', b)], inc=False)
            S.op('pe', lambda e: e.matmul(out=pm[b][0:64, :], lhsT=ones_f[0:1, 0:64],
                                          rhs=bmod[0:1, nb * 512:(nb + 1) * 512], start=False, stop=True),
                 reads=['ones_f', 'bmod'], writes=[('pm', b)])
            S.op('act', lambda e: e.copy(out=mrow[b][:], in_=pm[b][0:64, :]), reads=[('pm', b)], writes=[('mrow', b)])
            kind6 = nb // 2
            if kind6 in (2, 5):
                for w in range(2):
                    S.op('pe', lambda e: e.matmul(out=pt[w][:, :], lhsT=selrow[:, w * 128:(w + 1) * 128],
                                                  rhs=mrow[b][:], start=True, stop=True),
                         reads=['selrow', ('mrow', b)], writes=[('pt', w)])
                    gi = (0 if kind6 == 2 else 2) + w
                    S.op('act', lambda e: e.copy(out=gbc[:, gi, (nb % 2) * 512:(nb % 2) * 512 + 512], in_=pt[w][:, :]),
                         reads=[('pt', w)], writes=['gbc'])
            else:
                kind = {0: 0, 1: 1, 3: 2, 4: 3}[kind6]
                for j in range(4):
                    k = (nb % 2) * 4 + j
                    S.op('pe', lambda e: e.transpose(out=pt[0][:, j * 64:(j + 1) * 64],
                                                     in_=mrow[b][:, j * 128:(j + 1) * 128], identity=ident_f[0:64, 0:64]),
                         reads=[('mrow', b), 'ident_f'], writes=[('pt', 0)], inc=(j == 3))
                ptv = pt[0][:, 0:256].rearrange("p (j c) -> p j c", c=64)
                mv = modT[:, kind * 16 + (nb % 2) * 8: kind * 16 + (nb % 2) * 8 + 8].rearrange("p (j w) -> p j w", w=2)
                for w in range(2):
                    S.op('dve', lambda e: e.tensor_scalar(out=mv[:, :, w:w + 1], in0=ptv[:, :, 32 * w:32 * w + 1],
                                                          scalar1=(1.0 if kind in (1, 3) else 0.0), scalar2=None,
                                                          op0=ALU.add), reads=[('pt', 0)], writes=['modT'])
        dbg_dump('d_modT', modT[:], 'modT')
        dbg_dump('d_gbc', gbc[:].rearrange("p a b -> p (a b)"), 'gbc')
        S.barrier()
    if stop_after <= 0:
        return finish(nc, S, es, x_out, ctx_out)

    def SC(kind, k, w):
        i = kind * 16 + k * 2 + w
        return modT[:, i:i + 1]

    def ln_mod_T(src, srckey, ntok, kinds, w, dst_ap_fn, dstkey, xnb, xnbkey, ptr, ptrkey):
        ln_mod_a(src, srckey, ntok, xnb, xnbkey)
        ln_mod_b(ntok, kinds, w, dst_ap_fn, dstkey, xnb, xnbkey, ptr, ptrkey)

    def ln_mod_a(src, srckey, ntok, xnb, xnbkey, s_=None):
        rstd, nmr, k = ln_stats(src, srckey) if s_ is None else ln_stats_b(s_)
        S.op('act', lambda e: e.activation(out=xnb[0:ntok, :], in_=src[0:ntok, :], func=AF.Identity, bias=nmr[0:ntok],
                                           scale=rstd[0:ntok]), reads=[srckey, k], writes=[xnbkey])

    def ln_mod_b(ntok, kinds, w, dst_ap_fn, dstkey, xnb, xnbkey, ptr, ptrkey):
        for kk in range(8):
            S.op('pe', lambda e: e.transpose(out=ptr[:, kk, 0:ntok], in_=xnb[0:ntok, kk * 128:(kk + 1) * 128],
                                             identity=ident_b[0:ntok, 0:ntok]),
                 reads=[xnbkey, 'ident_b'], writes=[ptrkey], inc=(kk == 7))
        for kk in range(8):
            if kk % 2 == 0:
                S.op('dve', lambda e: e.tensor_scalar(out=dst_ap_fn(kk), in0=ptr[:, kk, 0:ntok],
                                                      scalar1=SC(kinds[1], kk, w), scalar2=SC(kinds[0], kk, w),
                                                      op0=ALU.mult, op1=ALU.add),
                     reads=[ptrkey, 'modT'], writes=[dstkey])
            else:
                S.op('act', lambda e: e.activation(out=dst_ap_fn(kk), in_=ptr[:, kk, 0:ntok], func=AF.Identity,
                                                   bias=SC(kinds[0], kk, w), scale=SC(kinds[1], kk, w)),
                     reads=[ptrkey, 'modT'], writes=[dstkey])

    def hk(key):
        return [key]

    def rms_rope(src, srckey, nh, gain_ap, cs, cskey, dst, dstkey, tmp, tmpkey, rope, part=None, st=None):
        if part != 'b':
            s = smslot()
        else:
            s = st
        k = ('sm', s)
        W_ = nh * 64
        if part == 'b':
            src = tmp[:, 2 * W_:3 * W_]
            srckey = tmpkey
        else:
            _rms_a(src, srckey, nh, tmp, tmpkey, s)
            src = tmp[:, 2 * W_:3 * W_]
            srckey = tmpkey
            if part == 'a':
                return s
        _rms_b(src, srckey, nh, gain_ap, cs, cskey, dst, dstkey, tmp, tmpkey, rope, s)

    def _rms_a(src, srckey, nh, tmp, tmpkey, s):
        k = ('sm', s)
        W_ = nh * 64
        src_ps = src
        src = tmp[:, 2 * W_:3 * W_]
        S.op('act', lambda e: e.copy(out=src, in_=src_ps), reads=[srckey], writes=[tmpkey])
        srckey = tmpkey
        S.op('dve', lambda e: e.tensor_tensor(out=tmp[:, 0:W_], in0=src, in1=src, op=ALU.mult),
             reads=[srckey], writes=[tmpkey])
        S.op('dve', lambda e: e.tensor_reduce(out=sm[:, s, 0:nh], in_=tmp[:, 0:W_].rearrange("p (h d) -> p h d", d=64),
                                              axis=AX.X, op=ALU.add), reads=[tmpkey], writes=[k])
        S.op('act', lambda e: e.activation(out=sm[:, s, 0:nh], in_=sm[:, s, 0:nh], func=AF.Sqrt, bias=epst[:, 0:1],
                                           scale=1.0 / 64), reads=[k, 'epst'], writes=[k])

    def _rms_b(src, srckey, nh, gain_ap, cs, cskey, dst, dstkey, tmp, tmpkey, rope, s):
        k = ('sm', s)
        W_ = nh * 64
        S.op('dve', lambda e: e.reciprocal(out=sm[:, s, 0:nh], in_=sm[:, s, 0:nh]), reads=[k], writes=[k])
        t3 = tmp[:, 0:W_].rearrange("p (h d) -> p h d", d=64)
        S.op('dve', lambda e: e.tensor_tensor(out=t3, in0=src.rearrange("p (h d) -> p h d", d=64),
                                              in1=sm[:, s, 0:nh].unsqueeze(2).to_broadcast([128, nh, 64]), op=ALU.mult),
             reads=[srckey, k], writes=[tmpkey])
        gdst = t3 if rope else dst.rearrange("p (h d) -> p h d", d=64)
        S.op('dve', lambda e: e.tensor_tensor(out=gdst, in0=t3, in1=gain_ap.unsqueeze(1).to_broadcast([128, nh, 64]),
                                              op=ALU.mult), reads=[tmpkey, 'qkg'], writes=[tmpkey if rope else dstkey])
        if not rope:
            return
        t5 = tmp[:, 0:W_].rearrange("p (h a b f) -> p h a b f", a=2, b=2, f=16)
        d5 = dst.rearrange("p (h a b f) -> p h a b f", a=2, b=2, f=16)
        u5 = tmp[:, W_:2 * W_].rearrange("p (h a b f) -> p h a b f", a=2, b=2, f=16)
        cosb = cs[:, 0:32].rearrange("p (a f) -> p a f", f=16).unsqueeze(1).to_broadcast([128, nh, 2, 16])
        sinb = cs[:, 32:64].rearrange("p (a f) -> p a f", f=16).unsqueeze(1).to_broadcast([128, nh, 2, 16])
        t1, t2 = t5[:, :, :, 0, :], t5[:, :, :, 1, :]
        ua, ub = u5[:, :, :, 0, :], u5[:, :, :, 1, :]
        rk = [tmpkey, cskey]
        S.op('dve', lambda e: e.tensor_tensor(out=ua, in0=t1, in1=cosb, op=ALU.mult), reads=rk, writes=[tmpkey])
        S.op('dve', lambda e: e.tensor_tensor(out=ub, in0=t2, in1=sinb, op=ALU.mult), reads=rk, writes=[tmpkey])
        S.op('dve', lambda e: e.tensor_tensor(out=d5[:, :, :, 0, :], in0=ua, in1=ub, op=ALU.subtract),
             reads=[tmpkey], writes=[dstkey])
        S.op('dve', lambda e: e.tensor_tensor(out=ua, in0=t2, in1=cosb, op=ALU.mult), reads=rk, writes=[tmpkey])
        S.op('dve', lambda e: e.tensor_tensor(out=ub, in0=t1, in1=sinb, op=ALU.mult), reads=rk, writes=[tmpkey])
        S.op('dve', lambda e: e.tensor_tensor(out=d5[:, :, :, 1, :], in0=ua, in1=ub, op=ALU.add),
             reads=[tmpkey], writes=[dstkey])

    LP = 16 + NTOK + 16
    LC = 16 + NCTX + 16
    with ExitStack() as pa:
        hT = sb('hT', [128, 8, HTW], BF, st=pa)
        uT = sb('uT', [128, 2, LP], st=pa); cuT = sb('cuT', [128, 2, LP], st=pa)
        uTc = sb('uTc', [128, 2, LC], st=pa); cuTc = sb('cuTc', [128, 2, LC], st=pa)
        with ExitStack() as pa1:
            winb = sb('winb', [128, 8, 1536], BF, st=pa1)
            xt = [sb('xt%d' % i, [128, D], st=pa1) for i in range(3)]
            xnb = [sb('xnb%d' % i, [128, D], BF, st=pa1) for i in range(2)]
            wst = [sb('wsta%d' % i, [128, 8, 512], st=pa1) for i in range(1)]
            qtmp = [sb('qtmp%d' % i, [128, 1536], st=pa1) for i in range(2)]
            qrb = [sb('qrb%d' % i, [128, 512], BF, st=pa1) for i in range(2)]
            cst = [sb('cst%d' % i, [128, 64], st=pa1) for i in range(2)]
            sig = [sb('sig%d' % i, [128, 512], st=pa1) for i in range(2)]
            ptr = [ps('ptr%d' % i, [128, 8, 128], BF, st=pa1) for i in range(2)]
            pq = [ps('pq%d' % i, [128, 512], st=pa1) for i in range(2)]
            pqt = [ps('pqt%d' % i, [128, 4, 128], BF, st=pa1) for i in range(2)]
            pf = [ps('pf%d' % i, [128, 512], st=pa1) for i in range(2)]
            wiv = I['w_in'].rearrange("(k p) n -> p k n", p=128)
            for nb in range(3):
                b = 0
                S.dma('sp', wst[b][:], wiv[:, :, nb * 512:(nb + 1) * 512], writes=[('wsta', b)])
                S.op('pool' if nb == 1 else 'dve', lambda e: e.tensor_copy(out=winb[:, :, nb * 512:(nb + 1) * 512], in_=wst[b][:]),
                     reads=[('wsta', b)], writes=['winb'])
            tiles = [('own', t) for t in range(NTI)] + [('ctx', t) for t in range(2)] + [('halo', 0)]
            for ti, (kind, t) in enumerate(tiles):
                b3 = ti % 3; b = ti % 2
                ntok = 32 if kind == 'halo' else 128
                src = {'own': I['x_own'], 'ctx': I['ctx'], 'halo': I['x_halo']}[kind]
                col0 = {'own': t * 128, 'ctx': NTOK + t * 128, 'halo': HAL0}[kind]
                w = 1 if kind == 'ctx' else 0
                S.dma('sp', xt[b3][0:ntok, :], src[t * 128:t * 128 + ntok, :], writes=[('xt', b3)])
                ln_mod_T(xt[b3], ('xt', b3), ntok, (0, 1), w,
                         lambda kk: hT[:, kk, col0:col0 + ntok], ('hT', ti), xnb[b], ('xnb', b), ptr[b], ('ptr', b))
                if kind == 'halo':
                    continue
                for kk in range(8):
                    S.op('pe', lambda e: e.matmul(out=pq[b][:, :], lhsT=hT[:, kk, col0:col0 + 128], rhs=winb[:, kk, 0:512],
                                                  start=(kk == 0), stop=(kk == 7)),
                         reads=hk(('hT', ti)) + ['winb'], writes=[('pq', b)], inc=(kk == 7))
                if kind == 'own':
                    S.dma('sp', cst[b][:], I['cs_own'][t * 128:(t + 1) * 128, :], writes=[('cst', b)])
                rms_rope(pq[b][:, :], ('pq', b), 8, qkg[:, 0:64], cst[b], ('cst', b), qrb[b][:, :], ('qrb', b),
                         qtmp[b], ('qtmp', b), rope=(kind == 'own'))
                for h_ in range(8):
                    g_, j_ = h_ // 4, h_ % 4
                    S.op('pe', lambda e: e.transpose(out=pqt[b][g_ * 64:(g_ + 1) * 64, j_, :], in_=qrb[b][:, h_ * 64:(h_ + 1) * 64],
                                                     identity=ident_b[:, :]),
                         reads=[('qrb', b), 'ident_b'], writes=[('pqt', b)], inc=(h_ == 7))
                S.op('act', lambda e: e.copy(out=QT[:, :, col0:col0 + 128], in_=pqt[b][:, :, :]),
                     reads=[('pqt', b)], writes=[('QT', col0 // 512)])
            blocks = [(i * 512, 512, 'own') for i in range(4)] + [(NTOK, 256, 'ctx'), (HAL0, 32, 'halo')]
            allhT = [k_ for ti in range(len(tiles)) for k_ in hk(('hT', ti))]
            pi = 0
            for (c0, n, kind) in blocks:
                def dsts(tn, ch):
                    if kind == 'own':
                        return tn[:, ch, 16 + c0:16 + c0 + n]
                    if kind == 'ctx':
                        return tn[:, ch, 16:16 + NCTX]
                    return tn[:, ch, :].rearrange("p (a b) -> p a b", a=2)[:, :, 0:16] if False else None
                for ch in range(2):
                    pu_, pa_, pg_ = None, None, None
                    res = {}
                    for which, cc0 in (('u', 768), ('g', 1280), ('a', 1024)):
                        b = pi % 2; pi += 1
                        for kk in range(8):
                            S.op('pe', lambda e: e.matmul(out=pf[b][:, 0:n], lhsT=winb[:, kk, cc0 + ch * 128:cc0 + ch * 128 + 128],
                                                          rhs=hT[:, kk, c0:c0 + n], start=(kk == 0), stop=(kk == 7)),
                                 reads=allhT + ['winb'], writes=[('pf', b)], inc=(kk == 7))
                        if kind == 'halo':
                            def hal(tn):
                                return [(tn[:, ch, 0:16], 0), (tn[:, ch, 16 + NTOK:32 + NTOK], 16)]
                        if which == 'u':
                            if kind == 'halo':
                                for (dap, o) in hal(uT):
                                    S.op('dve', lambda e: e.tensor_tensor(out=dap, in0=pf[b][:, o:o + 16], in1=hmask[:, o:o + 16],
                                                                          op=ALU.mult), reads=[('pf', b), 'hmask'], writes=['uT'])
                            else:
                                S.op('act', lambda e: e.copy(out=dsts(uT if kind == 'own' else uTc, ch), in_=pf[b][:, 0:n]),
                                     reads=[('pf', b)], writes=['uT'])
                        elif which == 'g':
                            sb_ = b
                            S.op('act', lambda e: e.activation(out=sig[sb_][:, 0:n], in_=pf[b][:, 0:n], func=AF.Sigmoid),
                                 reads=[('pf', b)], writes=[('sig', sb_)])
                        else:
                            if kind == 'halo':
                                S.op('dve', lambda e: e.tensor_tensor(out=sig[sb_][:, 0:32], in0=sig[sb_][:, 0:32], in1=hmask[:, :],
                                                                      op=ALU.mult), reads=[('sig', sb_), 'hmask'], writes=[('sig', sb_)])
                                for (dap, o) in hal(cuT):
                                    S.op('dve', lambda e: e.tensor_tensor(out=dap, in0=pf[b][:, o:o + 16], in1=sig[sb_][:, o:o + 16],
                                                                          op=ALU.mult), reads=[('pf', b), ('sig', sb_)], writes=['cuT'])
                            else:
                                S.op('dve', lambda e: e.tensor_tensor(out=dsts(cuT if kind == 'own' else cuTc, ch), in0=pf[b][:, 0:n],
                                                                      in1=sig[sb_][:, 0:n], op=ALU.mult),
                                     reads=[('pf', b), ('sig', sb_)], writes=['cuT'])
            for tn, kname in ((uTc, 'uT'), (cuTc, 'cuT')):
                S.op('pool', lambda e: e.memset(tn[:, :, 0:16], 0.0), writes=[kname])
                S.op('pool', lambda e: e.memset(tn[:, :, 16 + NCTX:32 + NCTX], 0.0), writes=[kname])
            S.barrier()
        dbg_dump('d_hT', hT[:].rearrange("p a b -> p (a b)"), ('hT', 0))

        with ExitStack() as pa2:
            pl = [sb('pl%d' % i, [128, LP], st=pa2) for i in range(2)]
            pooled = sb('pooled', [128, NTOK], BF, st=pa2)
            icn = sb('icn', [128, NTOK], st=pa2)
            wpbd = sb('wpbd', [128, 2, 128], BF, st=pa2); wpst = sb('wpst', [128, 2, 128], st=pa2)
            wpwb = sb('wpwb', [128, 2, 256], BF, st=pa2); wpws = sb('wpws', [128, 2, 256], st=pa2)
            acc = sb('acc', [128, 2, NTOK], st=pa2)
            sq = [sb('sq%d' % i, [128, 512], st=pa2) for i in range(2)]
            mean = sb('mean', [128, 512], st=pa2); rstd = sb('rstdc', [128, 512], st=pa2)
            zT = sb('zT', [128, 2, 512], BF, st=pa2)
            pp = [ps('pp%d' % i, [128, 512], st=pa2) for i in range(2)]
            ps1 = ps('ps1', [128, 512], st=pa2); ps2 = ps('ps2', [128, 512], st=pa2)
            pw = [ps('pw%d' % i, [128, 512], st=pa2) for i in range(2)]
            S.op('dve', lambda e: e.memset(wpst[:], 0.0), writes=['wpst'])
            for g in range(4):
                h = (g % 2) * 64
                S.dma('sp', wpst[h:h + 64, g // 2, h:h + 64], I['w_pool'][g * 64:(g + 1) * 64, :], reads=[], writes=['wpst'])
            S.op('dve', lambda e: e.tensor_copy(out=wpbd[:], in_=wpst[:]), reads=['wpst'], writes=['wpbd'])
            S.dma('sp', wpws[:], I['w_pw'].rearrange("(k p) n -> p k n", p=128), writes=['wpws'])
            S.op('dve', lambda e: e.tensor_copy(out=wpwb[:], in_=wpws[:]), reads=['wpws'], writes=['wpwb'])

            for (kind, U_, CU_, L, ycol0, ic0) in (('own', uT, cuT, NTOK, 0, 0), ('ctx', uTc, cuTc, NCTX, NTOK, 2 * NTOK)):
                LL = L + 32
                nblk = [(i * 512, 512) for i in range(L // 512)] if L >= 512 else [(0, L)]
                for ch in range(2):
                    U = U_[:, ch, :]
                    A, B = pl[0], pl[1]
                    S.dma('sp', icn[:, 0:L], I['invcnt'][:, ic0 + ch * L: ic0 + (ch + 1) * L], writes=['icn'])
                    S.op('dve', lambda e: e.tensor_tensor(out=A[:, 1:LL], in0=U[:, 0:LL - 1], in1=U[:, 1:LL], op=ALU.add),
                         reads=['uT'], writes=['plA'])
                    S.op('dve', lambda e: e.tensor_tensor(out=B[:, 2:LL - 1], in0=A[:, 1:LL - 2], in1=A[:, 3:LL], op=ALU.add),
                         reads=['plA'], writes=['plB'])
                    if ch == 0:
                        lo, hi = A, B
                    else:
                        S.op('dve', lambda e: e.tensor_tensor(out=A[:, 4:LL - 3], in0=B[:, 2:LL - 5], in1=B[:, 6:LL - 1], op=ALU.add),
                             reads=['plB'], writes=['plA'])
                        S.op('dve', lambda e: e.tensor_tensor(out=B[:, 8:LL - 7], in0=A[:, 4:LL - 11], in1=A[:, 12:LL - 3], op=ALU.add),
                             reads=['plA'], writes=['plB'])
                        lo, hi = A, B
                    for (h0, srcw, kk_) in ((0, lo, 'plA'), (64, hi, 'plB')):
                        S.op('dve', lambda e: e.tensor_tensor(out=srcw[h0:h0 + 64, 16:16 + L], in0=srcw[h0:h0 + 64, 16:16 + L],
                                                              in1=icn[h0:h0 + 64, 0:L], op=ALU.mult),
                             reads=[kk_, 'icn'], writes=[kk_])
                        S.op('dve', lambda e: e.tensor_tensor(out=pooled[h0:h0 + 64, 0:L], in0=srcw[h0:h0 + 64, 16:16 + L],
                                                              in1=U[h0:h0 + 64, 16:16 + L], op=ALU.subtract),
                             reads=[kk_, 'uT'], writes=['pooled'])
                    for bi, (c0, n) in enumerate(nblk):
                        b = bi % 2
                        S.op('pe', lambda e: e.matmul(out=pp[b][:, 0:n], lhsT=wpbd[:, ch, :], rhs=pooled[:, c0:c0 + n],
                                                      start=True, stop=True), reads=['wpbd', 'pooled'], writes=[('pp', b)])
                        S.op('act', lambda e: e.activation(out=yT[:, ch, ycol0 + c0:ycol0 + c0 + n], in_=pp[b][:, 0:n],
                                                           func=AF.Identity, bias=0.0, scale=cvp[:, ch * 4 + 3:ch * 4 + 4]),
                             reads=[('pp', b), 'cvp'], writes=['yT'])
                for ch in range(2):
                    CU = CU_[:, ch, :]
                    a_ = acc[:, ch, 0:L]
                    S.op('dve', lambda e: e.tensor_scalar(out=a_, in0=CU[:, 1:1 + L], scalar1=wdwT[:, ch * 31:ch * 31 + 1],
                                                          scalar2=cvp[:, ch * 4:ch * 4 + 1], op0=ALU.mult, op1=ALU.add),
                         reads=['cuT', 'wdwT', 'cvp'], writes=['acc'])
                    for j in range(1, 31):
                        S.op('dve', lambda e: e.scalar_tensor_tensor(out=a_, in0=CU[:, 1 + j:1 + j + L],
                                                                     scalar=wdwT[:, ch * 31 + j:ch * 31 + j + 1], in1=a_,
                                                                     op0=ALU.mult, op1=ALU.add),
                             reads=['cuT', 'acc'], writes=['acc'])
                for bi, (c0, n) in enumerate(nblk):
                    for ch in range(2):
                        S.op('pe', lambda e: e.matmul(out=ps1[:, 0:n], lhsT=ones_f[:, :], rhs=acc[:, ch, c0:c0 + n],
                                                      start=(ch == 0), stop=(ch == 1)), reads=['acc', 'ones_f'], writes=['ps1'], inc=(ch == 1))
                    for ch in range(2):
                        S.op('act', lambda e: e.activation(out=sq[ch][:, 0:n], in_=acc[:, ch, c0:c0 + n], func=AF.Square),
                             reads=['acc'], writes=[('sq', ch)])
                        S.op('pe', lambda e: e.matmul(out=ps2[:, 0:n], lhsT=ones_f[:, :], rhs=sq[ch][:, 0:n],
                                                      start=(ch == 0), stop=(ch == 1)), reads=[('sq', ch), 'ones_f'], writes=['ps2'], inc=(ch == 1))
                    S.op('act', lambda e: e.activation(out=mean[:, 0:n], in_=ps1[:, 0:n], func=AF.Copy, scale=1.0 / 256),
                         reads=['ps1'], writes=['mean'])
                    S.op('dve', lambda e: e.tensor_tensor(out=rstd[:, 0:n], in0=mean[:, 0:n], in1=mean[:, 0:n], op=ALU.mult),
                         reads=['mean'], writes=['rstd'])
                    S.op('dve', lambda e: e.scalar_tensor_tensor(out=rstd[:, 0:n], in0=ps2[:, 0:n], scalar=1.0 / 256, in1=rstd[:, 0:n],
                                                                 op0=ALU.mult, op1=ALU.subtract), reads=['ps2', 'rstd'], writes=['rstd'])
                    S.op('act', lambda e: e.activation(out=rstd[:, 0:n], in_=rstd[:, 0:n], func=AF.Sqrt, bias=epst[:, 0:1], scale=1.0),
                         reads=['rstd', 'epst'], writes=['rstd'])
                    S.op('dve', lambda e: e.reciprocal(out=rstd[:, 0:n], in_=rstd[:, 0:n]), reads=['rstd'], writes=['rstd'])
                    for ch in range(2):
                        S.op('dve', lambda e: e.tensor_tensor(out=sq[ch][:, 0:n], in0=acc[:, ch, c0:c0 + n], in1=mean[:, 0:n], op=ALU.subtract),
                             reads=['acc', 'mean'], writes=[('sq', ch)])
                        S.op('dve', lambda e: e.tensor_tensor(out=sq[ch][:, 0:n], in0=sq[ch][:, 0:n], in1=rstd[:, 0:n], op=ALU.mult),
                             reads=['rstd', ('sq', ch)], writes=[('sq', ch)])
                        S.op('act', lambda e: e.activation(out=zT[:, ch, 0:n], in_=sq[ch][:, 0:n], func=AF.Silu,
                                                           bias=cvp[:, ch * 4 + 2:ch * 4 + 3], scale=cvp[:, ch * 4 + 1:ch * 4 + 2]),
                             reads=[('sq', ch), 'cvp'], writes=['zT'])
                    for dch in range(2):
                        for cc_ in range(2):
                            S.op('pe', lambda e: e.matmul(out=pw[dch][:, 0:n], lhsT=wpwb[:, cc_, dch * 128:(dch + 1) * 128],
                                                          rhs=zT[:, cc_, 0:n], start=(cc_ == 0), stop=(cc_ == 1)),
                                 reads=['zT', 'wpwb'], writes=[('pw', dch)], inc=(cc_ == 1))
                        S.op('act', lambda e: e.copy(out=yT[:, 2 + dch, ycol0 + c0:ycol0 + c0 + n], in_=pw[dch][:, 0:n]),
                             reads=[('pw', dch)], writes=['yT'])
            S.barrier()
    dbg_dump('d_QT', QY[:, 0:4, :], ('QT', 0))
    dbg_dump('d_yT', QY[:, 4:8, :], 'yT')
    if stop_after <= 1:
        return finish(nc, S, es, x_out, ctx_out)

    with ExitStack() as pb:
        KTz = [sb('KTz%d' % i, [128, KEYS], BF, st=pb) for i in range(2)]
        KT = KTz[0]
        Vg = sb('Vg', [128, NKC, 192], BF, st=pb)
        with ExitStack() as pb1:
            wkvb = sb('wkvb', [128, 8, 256], BF, st=pb1)
            xt = [sb('xtb%d' % i, [128, D], st=pb1) for i in range(3)]
            xnb = [sb('xnbb%d' % i, [128, D], BF, st=pb1) for i in range(3)]
            hTt = [sb('hTt%d' % i, [128, 8, 128], BF, st=pb1) for i in range(2)]
            ktmp = [sb('ktmp%d' % i, [128, 384], st=pb1) for i in range(2)]
            krb = [sb('krb%d' % i, [128, 128], BF, st=pb1) for i in range(2)]
            cst = [sb('cstb%d' % i, [128, 64], st=pb1) for i in range(3)]
            ptr = [ps('ptrb%d' % i, [128, 8, 128], BF, st=pb1) for i in range(2)]
            pkv = [ps('pkv%d' % i, [128, 512], st=pb1) for i in range(3)]
            pkt = [ps('pkt%d' % i, [128, 128], BF, st=pb1) for i in range(2)]
            for hh in range(2):
                stv = xt[hh][:, :].rearrange("p (k n) -> p k n", n=256)
                S.dma('sp', stv, I['w_in'].rearrange("(k p) n -> p k n", p=128)[:, hh * 4:hh * 4 + 4, 512:768], writes=[('xtb', hh)])
                S.op('dve', lambda e: e.tensor_copy(out=wkvb[:, hh * 4:hh * 4 + 4, :], in_=stv), reads=[('xtb', hh)], writes=['wkvb'])
            S.op('pool', lambda e: e.memset(KTz[0][64:128, :], 0.0), writes=['KTm'])
            S.op('pool', lambda e: e.memset(KTz[1][0:64, :], 0.0), writes=['KTm'])
            S.op('pool', lambda e: e.memset(Vg[:, :, 64:128], 0.0), writes=['Vg'])
            S.op('pool', lambda e: e.memset(Vg[:, :, 64:65], 1.0), writes=['Vg'])
            SB_ = {}; SR_ = {}

            def stB1(c):
                b3 = c % 3
                isctx = c < 2
                src = I['ctx'][c * 128:(c + 1) * 128, :] if isctx else I['x_all'][(c - 2) * 128:(c - 1) * 128, :]
                S.dma('sp', xt[b3][:], src, writes=[('xtb', b3)])
                SB_[c] = ln_stats_a(xt[b3], ('xtb', b3))

            def stB1b(c):
                b3 = c % 3
                ln_mod_a(xt[b3], ('xtb', b3), 128, xnb[b3], ('xnbb', b3), s_=SB_.pop(c))

            def stB2(c):
                b3 = c % 3; b = c % 2
                isctx = c < 2
                ln_mod_b(128, (0, 1), 1 if isctx else 0, lambda kk: hTt[b][:, kk, :], ('hTt', b),
                         xnb[b3], ('xnbb', b3), ptr[b], ('ptrb', b))
                for kk in range(8):
                    S.op('pe', lambda e: e.matmul(out=pkv[b3][:, 0:256], lhsT=hTt[b][:, kk, :], rhs=wkvb[:, kk, :],
                                                  start=(kk == 0), stop=(kk == 7)),
                         reads=hk(('hTt', b)) + ['wkvb'], writes=[('pkv', b3)], inc=(kk == 7))
                S.op('act', lambda e: e.copy(out=Vg[:, c, 0:64], in_=pkv[b3][:, 128:192]), reads=[('pkv', b3)], writes=[('Vg', c)])
                S.op('act', lambda e: e.copy(out=Vg[:, c, 128:192], in_=pkv[b3][:, 192:256]), reads=[('pkv', b3)], writes=[('Vg', c)])
                if not isctx:
                    S.dma('sp', cst[b3][:], I['cs_all'][(c - 2) * 128:(c - 1) * 128, :], writes=[('cstb', b3)])

            def stB3a(c):
                b3 = c % 3; b = c % 2
                SR_[c] = rms_rope(pkv[b3][:, 0:128], ('pkv', b3), 2, qkg[:, 64:128], cst[b3], ('cstb', b3), krb[b][:, :], ('krb', b),
                                  ktmp[b], ('ktmp', b), rope=(c >= 2), part='a')

            def stB3(c):
                b3 = c % 3; b = c % 2
                isctx = c < 2
                rms_rope(pkv[b3][:, 0:128], ('pkv', b3), 2, qkg[:, 64:128], cst[b3], ('cstb', b3), krb[b][:, :], ('krb', b),
                         ktmp[b], ('ktmp', b), rope=not isctx, part='b', st=SR_.pop(c))
                S.op('pe', lambda e: e.transpose(out=pkt[b][:, :], in_=krb[b][:, :], identity=ident_b[:, :]),
                     reads=[('krb', b), 'ident_b'], writes=[('pkt', b)])
                S.op('act', lambda e: e.copy(out=KTz[0][0:64, c * 128:(c + 1) * 128], in_=pkt[b][0:64, :]),
                     reads=[('pkt', b)], writes=[('KT', c)])
                S.op('act', lambda e: e.copy(out=KTz[1][64:128, c * 128:(c + 1) * 128], in_=pkt[b][64:128, :]),
                     reads=[('pkt', b)], writes=[('KT', c)])

            for step in range(NKC + 2):
                if step < NKC:
                    stB1(step)
                if 0 <= step - 2 < NKC:
                    stB3a(step - 2)
                if step < NKC:
                    stB1b(step)
                if 0 <= step - 2 < NKC:
                    stB3(step - 2)
                if 0 <= step - 1 < NKC:
                    stB2(step - 1)
            S.barrier()
        dbg_dump('d_KT', KT[:], ('KT', 0))
        dbg_dump('d_V', Vg[:].rearrange("p a b -> p (a b)"), ('Vg', 0))
        if stop_after <= 2:
            S.barrier()
            return finish(nc, S, es, x_out, ctx_out)

        with ExitStack() as pb2:
            PT = [sb('PT%d' % i, [128, 2, 512], BF, st=pb2) for i in range(4)]
            rr = sb('rr', [128, 512], st=pb2); bcs = [sb('bcs%d' % i, [128, 512], st=pb2) for i in range(2)]
            O = [ps('O%d' % i, [128, 512], st=pb2) for i in range(4)]
            SP = [ps('SP%d' % i, [128, 2, 512], st=pb2) for i in range(2)]
            qblocks = [(i * 512, 512, list(range(NKC))) for i in range(4)] + [(NTOK, NCTX, [0, 1])]
            pti = 0
            for (q0, n, chunks) in qblocks:
                for g in range(2):
                    gp = slice(g * 64, (g + 1) * 64)
                    vsl = slice(0, 65) if g == 0 else slice(64, 192)
                    pend = []
                    steps = [(c, pr) for c in chunks for pr in range(2)]
                    for si, (c, pr) in enumerate(steps):
                        sp_ = SP[pr]
                        for jj in range(2):
                            j = pr * 2 + jj
                            S.op('pe', lambda e: e.matmul(out=sp_[:, jj, 0:n], lhsT=KTz[g][:, c * 128:(c + 1) * 128],
                                                          rhs=QT[:, j, q0:q0 + n], start=True, stop=True),
                                 reads=[('KT', c), ('QT', q0 // 512)], writes=[('SP', pr)], inc=(jj == 1))
                        pb_ = pti % 4; pti += 1
                        S.op('act', lambda e: e.activation(out=PT[pb_][:, :, 0:n], in_=sp_[:, :, 0:n], func=AF.Exp,
                                                           bias=negm[:, 0:1], scale=0.125),
                             reads=[('SP', pr), 'negm'], writes=[('PT', pb_)])
                        if pend:
                            pend.pop(0)()
                        def pv(c=c, pr=pr, pb_=pb_):
                            for jj in range(2):
                                j = pr * 2 + jj
                                S.op('pe', lambda e: e.matmul(out=O[j][0:(65 if g == 0 else 128), 0:n], lhsT=Vg[:, c, vsl],
                                                              rhs=PT[pb_][:, jj, 0:n], start=(c == chunks[0]), stop=(c == chunks[-1])),
                                     reads=[('Vg', c), ('PT', pb_)], writes=[('O', j)], inc=(jj == 1))
                        pend.append(pv)
                    while pend:
                        pend.pop(0)()
                    srow = 64 if g == 0 else 0
                    for j in range(4):
                        bb = j % 2
                        S.op('dve', lambda e: e.reciprocal(out=rr[srow:srow + 1, 0:n], in_=O[j][srow:srow + 1, 0:n]),
                             reads=[('O', j)], writes=['rr'])
                        S.op('pe', lambda e: e.matmul(out=SP[bb][:, 0, 0:n], lhsT=ones_f[srow:srow + 1, :], rhs=rr[srow:srow + 1, 0:n],
                                                      start=True, stop=True), reads=['rr', 'ones_f'], writes=[('SP', bb)])
                        S.op('act', lambda e: e.copy(out=bcs[bb][:, 0:n], in_=SP[bb][:, 0, 0:n]), reads=[('SP', bb)], writes=[('bcs', bb)])
                        S.op('dve', lambda e: e.tensor_tensor(out=QT[gp, j, q0:q0 + n], in0=O[j][gp, 0:n], in1=bcs[bb][gp, 0:n], op=ALU.mult),
                             reads=[('O', j), ('bcs', bb)], writes=[('QT', q0 // 512)])
            S.barrier()
    if dbg:
        dbg_dump('d_QT', QY[:, 0:4, :], ('QT', 0))
    if stop_after <= 3:
        S.barrier()
        return finish(nc, S, es, x_out, ctx_out)

    x_res = sb('x_res', [128, 18, D])
    GT = sb('GT', [32, NTOK + NCTX])
    tilesC = [('own', t, t * 128) for t in range(NTI)] + [('ctx', t, NTOK + t * 128) for t in range(2)]

    LN_ = {}

    def post_ln(tI, pre, prekey):
        lnbc = LN_['t']
        rstd_, nmr_, k = ln_stats(pre, prekey)
        S.op('act', lambda e: e.activation(out=pre[:, :], in_=pre[:, :], func=AF.Identity, bias=nmr_, scale=rstd_),
             reads=[prekey, k], writes=[prekey])
        S.op('dve', lambda e: e.tensor_tensor(out=pre[:, :], in0=pre[:, :], in1=lnbc[:, 0, :], op=ALU.mult),
             reads=[prekey, 'lnbc'], writes=[prekey])
        S.op('pool', lambda e: e.tensor_tensor(out=x_res[:, tI, :], in0=pre[:, :], in1=lnbc[:, 1, :], op=ALU.add),
             reads=[prekey, 'lnbc'], writes=[('xres', tI)])

    with ExitStack() as pc:
        lnbc = sb('lnbc', [128, 2, D], st=pc)
        LN_['t'] = lnbc
        woutb = sb('woutb', [128, 8, D], BF, st=pc)
        wst = [sb('wstc%d' % i, [128, 2, D], st=pc) for i in range(2)]
        xt = [sb('xtc%d' % i, [128, D], st=pc) for i in range(2)]
        pre = [sb('pre%d' % i, [128, D], st=pc) for i in range(2)]
        py = [ps('py%d' % i, [128, 2, 512], st=pc) for i in range(2)]
        S.dma('sp', lnbc[:, 0, :], I['lnv'][:, 0:D].to_broadcast([128, D]), writes=['lnbc'])
        S.dma('sp', lnbc[:, 1, :], I['lnv'][:, D:2 * D].to_broadcast([128, D]), writes=['lnbc'])
        for j in range(4):
            b = j % 2
            for g in range(2):
                r0 = g * 256 + j * 64
                S.dma('sp', wst[b][g * 64:(g + 1) * 64, 0, :], I['w_out'][r0:r0 + 64, :], writes=[('wstc', b)])
            S.dma('sp', wst[b][:, 1, :], I['w_out'][512 + j * 128:512 + (j + 1) * 128, :], writes=[('wstc', b)])
            S.op('dve', lambda e: e.tensor_copy(out=woutb[:, j, :], in_=wst[b][:, 0, :]), reads=[('wstc', b)], writes=['woutb'])
            S.op('pool', lambda e: e.tensor_copy(out=woutb[:, 4 + j, :], in_=wst[b][:, 1, :]), reads=[('wstc', b)], writes=['woutb'])
        for tI, (kind, t, col0) in enumerate(tilesC):
            b = tI % 2
            src = I['x_own'] if kind == 'own' else I['ctx']
            S.dma('sp', xt[b][:], src[t * 128:(t + 1) * 128, :], writes=[('xtc', b)])
            for nh in range(2):
                for kk in range(8):
                    lh = QT[:, kk, col0:col0 + 128] if kk < 4 else yT[:, kk - 4, col0:col0 + 128]
                    S.op('pe', lambda e: e.matmul(out=py[b][:, nh, :], lhsT=lh, rhs=woutb[:, kk, nh * 512:(nh + 1) * 512],
                                                  start=(kk == 0), stop=(kk == 7)),
                         reads=[('QT', col0 // 512), 'yT', 'woutb'], writes=[('py', b)], inc=(kk == 7 and nh == 1))
            gi = 1 if kind == 'ctx' else 0
            S.op('dve', lambda e: e.tensor_tensor(out=pre[b][:, :], in0=py[b][:, :, :].rearrange("p a b -> p (a b)"), in1=gbc[:, gi, :], op=ALU.mult),
                 reads=[('py', b), 'gbc'], writes=[('pre', b)])
            S.op('dve', lambda e: e.scalar_tensor_tensor(out=pre[b][:, :], in0=xt[b][:, :], scalar=ALPHA, in1=pre[b][:, :],
                                                         op0=ALU.mult, op1=ALU.add), reads=[('xtc', b), ('pre', b)], writes=[('pre', b)])
            post_ln(tI, pre[b], ('pre', b))
        S.barrier()
    dbg_dump('d_xres', x_res[:].rearrange("p a b -> p (a b)"), ('xres', 0))
    if stop_after <= 4:
        S.barrier()
        return finish(nc, S, es, x_out, ctx_out)

    hT = QY
    with ExitStack() as pd:
        wrb = sb('wrb', [128, 8, 36], BF, st=pd); wrs = sb('wrs', [128, 8, 36], st=pd)
        brb = sb('brb', [1, 36], BF, st=pd); brs = sb('brs', [1, 36], st=pd)
        ones_b = sb('ones_b', [1, 128], BF, st=pd)
        xnb = [sb('xnbd%d' % i, [128, D], BF, st=pd) for i in range(2)]
        rt = sb('rt', [128, 4, 64], st=pd)
        ptr = [ps('ptrd%d' % i, [128, 8, 128], BF, st=pd) for i in range(2)]
        prr = [ps('prr%d' % i, [128, 64], st=pd) for i in range(2)]
        pgt = [ps('pgt%d' % i, [32, 128], st=pd) for i in range(2)]
        S.dma('sp', wrs[:], I['w_r'].rearrange("(k p) n -> p k n", p=128), writes=['wrs'])
        S.op('dve', lambda e: e.tensor_copy(out=wrb[:], in_=wrs[:]), reads=['wrs'], writes=['wrb'])
        S.dma('sp', brs[:], I['b_r'], writes=['brs'])
        S.op('dve', lambda e: e.tensor_copy(out=brb[:], in_=brs[:]), reads=['brs'], writes=['brb'])
        S.op('dve', lambda e: e.memset(ones_b[:], 1.0), writes=['ones_b'])
        for tI, (kind, t, col0) in enumerate(tilesC):
            b = tI % 2
            w = 1 if kind == 'ctx' else 0
            xr = x_res[:, tI, :]
            ln_mod_T(xr, ('xres', tI), 128, (2, 3), w, lambda kk: hT[:, kk, col0:col0 + 128], ('hT2', tI),
                     xnb[b], ('xnbd', b), ptr[b], ('ptrd', b))
            S.op('pool', lambda e: e.tensor_scalar(out=xr, in0=xr, scalar1=ALPHA, scalar2=None, op0=ALU.mult),
                 reads=[('xres', tI), ('xnbd', b)], writes=[('xres', tI)])
            for kk in range(8):
                S.op('pe', lambda e: e.matmul(out=prr[b][:, 0:36], lhsT=hT[:, kk, col0:col0 + 128], rhs=wrb[:, kk, :],
                                              start=(kk == 0), stop=False), reads=hk(('hT2', tI)) + ['wrb'], writes=[('prr', b)], inc=False)
            S.op('pe', lambda e: e.matmul(out=prr[b][:, 0:36], lhsT=ones_b[0:1, :], rhs=brb[0:1, :], start=False, stop=True),
                 reads=['ones_b', 'brb'], writes=[('prr', b)])
            r = rt[:, tI % 4, :]; rk = ('rt', tI % 4)
            lg = r[:, 0:36]
            S.op('act', lambda e: e.copy(out=lg, in_=prr[b][:, 0:36]), reads=[('prr', b)], writes=[rk])
            S.op('dve', lambda e: e.tensor_reduce(out=r[:, 36:37], in_=r[:, 0:4], axis=AX.X, op=ALU.max), reads=[rk], writes=[rk])
            S.op('dve', lambda e: e.tensor_scalar(out=r[:, 40:44], in0=r[:, 0:4], scalar1=r[:, 36:37], scalar2=None, op0=ALU.is_ge),
                 reads=[rk], writes=[rk])
            S.op('dve', lambda e: e.tensor_scalar(out=r[:, 37:38], in0=r[:, 36:37], scalar1=-1.0, scalar2=None, op0=ALU.mult),
                 reads=[rk], writes=[rk])
            S.op('act', lambda e: e.activation(out=r[:, 44:48], in_=r[:, 0:4], func=AF.Exp, bias=r[:, 37:38], scale=1.0,
                                               accum_out=r[:, 38:39]), reads=[rk], writes=[rk])
            S.op('dve', lambda e: e.tensor_scalar(out=r[:, 40:44], in0=r[:, 40:44], scalar1=-1.0, scalar2=1.0e4, op0=ALU.add, op1=ALU.mult),
                 reads=[rk], writes=[rk])
            le = r[:, 4:36].rearrange("p (g x) -> p g x", x=8)
            S.op('dve', lambda e: e.tensor_tensor(out=le, in0=le, in1=r[:, 40:44].unsqueeze(2).to_broadcast([128, 4, 8]), op=ALU.add),
                 reads=[rk], writes=[rk])
            S.op('dve', lambda e: e.max(out=r[:, 48:56], in_=r[:, 4:36]), reads=[rk], writes=[rk])
            S.op('dve', lambda e: e.tensor_scalar(out=r[:, 39:40], in0=r[:, 48:49], scalar1=-1.0, scalar2=None, op0=ALU.mult),
                 reads=[rk], writes=[rk])
            S.op('act', lambda e: e.activation(out=r[:, 4:36], in_=r[:, 4:36], func=AF.Exp, bias=r[:, 39:40], scale=1.0),
                 reads=[rk], writes=[rk])
            S.op('act', lambda e: e.activation(out=r[:, 56:57], in_=r[:, 49:50], func=AF.Exp, bias=r[:, 39:40], scale=1.0),
                 reads=[rk], writes=[rk])
            S.op('dve', lambda e: e.tensor_scalar(out=r[:, 57:58], in0=r[:, 56:57], scalar1=1.0, scalar2=r[:, 38:39], op0=ALU.add, op1=ALU.mult),
                 reads=[rk], writes=[rk])
            S.op('dve', lambda e: e.reciprocal(out=r[:, 57:58], in_=r[:, 57:58]), reads=[rk], writes=[rk])
            S.op('dve', lambda e: e.scalar_tensor_tensor(out=r[:, 4:36], in0=r[:, 4:36], scalar=r[:, 56:57], in1=r[:, 4:36],
                                                         op0=ALU.is_ge, op1=ALU.mult), reads=[rk], writes=[rk])
            S.op('dve', lambda e: e.tensor_scalar(out=r[:, 4:36], in0=r[:, 4:36], scalar1=r[:, 57:58], scalar2=None, op0=ALU.mult),
                 reads=[rk], writes=[rk])
            S.op('pe', lambda e: e.transpose(out=pgt[b][:, :], in_=r[:, 4:36], identity=ident_f[:, :]),
                 reads=[rk, 'ident_f'], writes=[('pgt', b)])
            S.op('act', lambda e: e.copy(out=GT[:, col0:col0 + 128], in_=pgt[b][:, :]), reads=[('pgt', b)], writes=['GT'])
        S.barrier()
    dbg_dump('d_GT', GT[:], 'GT')
    if stop_after <= 5:
        S.barrier()
        return finish(nc, S, es, x_out, ctx_out)

    allhT2 = [k_ for i in range(18) for k_ in hk(('hT2', i))]
    with ExitStack() as pe_:
        wgb = [sb('wgb%d' % i, [128, 8, 256], BF, st=pe_) for i in range(2)]
        wub = [sb('wub%d' % i, [128, 8, 256], BF, st=pe_) for i in range(2)]
        wdb = [sb('wdb%d' % i, [128, 2, D], BF, st=pe_) for i in range(2)]
        wdc = [sb('wdc%d' % i, [128, 2, D], BF, st=pe_) for i in range(2)]
        stg = [sb('stg%d' % i, [128, 2048], st=pe_) for i in range(2)]
        Gs = [sb('Gs%d' % i, [128, 512], st=pe_) for i in range(2)]
        st_ = [sb('st_s%d' % i, [128, 512], st=pe_) for i in range(2)]
        tt_ = [sb('tt_s%d' % i, [128, 512], st=pe_) for i in range(2)]
        aT = [sb('aT%d' % i, [128, 2, 512], BF, st=pe_) for i in range(2)]
        pg = [ps('pg%d' % i, [128, 512], st=pe_) for i in range(2)]
        pu = [ps('pu%d' % i, [128, 512], st=pe_) for i in range(2)]
        pgb = ps('pgb', [128, 512], st=pe_)
        pyd = [ps('pyd%d' % i, [128, 512], st=pe_) for i in range(3)]
        si = 0; YD = [0]; cnt2 = 0; pend_dn = []
        tblocks = [(i * 512, 512, 'own') for i in range(4)] + [(NTOK, NCTX, 'ctx')]
        for e_ in range(NEXP):
            for fh in range(2):
                wb = (e_ * 2 + fh) % 2
                for which in range(3):
                    s_ = si % 2; si += 1
                    if which < 2:
                        srcw = (I['w_eg'] if which == 0 else I['w_eu'])[e_].rearrange("(k p) f -> p k f", p=128)[:, :, fh * 256:(fh + 1) * 256]
                        S.dma('sp', stg[s_][:, :].rearrange("p (k f) -> p k f", f=256), srcw, writes=[('stg', s_)])
                        dst = (wgb if which == 0 else wub)[wb]
                        S.op('pool' if which == 0 else 'act',
                             (lambda e: e.tensor_copy(out=dst[:], in_=stg[s_][:, :].rearrange("p (k f) -> p k f", f=256))) if which == 0 else
                             (lambda e: e.copy(out=dst[:], in_=stg[s_][:, :].rearrange("p (k f) -> p k f", f=256))),
                             reads=[('stg', s_)], writes=[('wg' if which == 0 else 'wu', wb)])
                    else:
                        srcw = I['w_ed'][e_].rearrange("(c p) n -> p c n", p=128)[:, fh * 2:fh * 2 + 2, :]
                        S.dma('sp', stg[s_][:, :].rearrange("p (c n) -> p c n", n=D), srcw, writes=[('stg', s_)])
                        sv = stg[s_][:, :].rearrange("p (c n) -> p c n", n=D)
                        S.op('pool', lambda e: e.tensor_tensor(out=wdb[wb][:], in0=sv, in1=gbc[:, 2:3, :].to_broadcast([128, 2, D]), op=ALU.mult),
                             reads=[('stg', s_), 'gbc'], writes=[('wd', wb)])
                        S.op('dve', lambda e: e.tensor_tensor(out=wdc[wb][:], in0=sv, in1=gbc[:, 3:4, :].to_broadcast([128, 2, D]), op=ALU.mult),
                             reads=[('stg', s_), 'gbc'], writes=[('wdc', wb)])
                for (c0, n, kind) in tblocks:
                    gb = cnt2 % 2; cnt2 += 1
                    S.op('pe', lambda e: e.matmul(out=pgb[:, 0:n], lhsT=ident_f[0:32, e_:e_ + 1].to_broadcast([32, 128]), rhs=GT[:, c0:c0 + n], start=True, stop=True),
                         reads=['ident_f', 'GT'], writes=['pgb'])
                    S.op('act', lambda e: e.copy(out=Gs[gb][:, 0:n], in_=pgb[:, 0:n]), reads=['pgb'], writes=[('Gs', gb)])
                    for fc in range(2):
                        for kk in range(8):
                            S.op('pe', lambda e: e.matmul(out=pg[fc][:, 0:n], lhsT=wgb[wb][:, kk, fc * 128:(fc + 1) * 128], rhs=hT[:, kk, c0:c0 + n],
                                                          start=(kk == 0), stop=(kk == 7)), reads=allhT2 + [('wg', wb)], writes=[('pg', fc)], inc=(kk == 7))
                        for kk in range(8):
                            S.op('pe', lambda e: e.matmul(out=pu[fc][:, 0:n], lhsT=wub[wb][:, kk, fc * 128:(fc + 1) * 128], rhs=hT[:, kk, c0:c0 + n],
                                                          start=(kk == 0), stop=(kk == 7)), reads=allhT2 + [('wu', wb)], writes=[('pu', fc)], inc=(kk == 7))
                        S.op('act', lambda e: e.activation(out=st_[fc][:, 0:n], in_=pg[fc][:, 0:n], func=AF.Silu), reads=[('pg', fc)], writes=[('st', fc)])
                        S.op('dve', lambda e: e.tensor_tensor(out=tt_[fc][:, 0:n], in0=pu[fc][:, 0:n], in1=Gs[gb][:, 0:n], op=ALU.mult),
                             reads=[('pu', fc), ('Gs', gb)], writes=[('tt', fc)])
                        S.op('pool', lambda e: e.tensor_tensor(out=aT[gb][:, fc, 0:n], in0=st_[fc][:, 0:n], in1=tt_[fc][:, 0:n], op=ALU.mult),
                             reads=[('st', fc), ('tt', fc)], writes=[('aT', gb)])
                    def down(gb=gb, wb=wb, kind=kind, c0=c0, n=n):
                        wdsel = wdc if kind == 'ctx' else wdb
                        for tt in range(n // 128):
                            tI = (c0 + tt * 128) // 128
                            for nh in range(2):
                                y_ = YD[0] % 3; YD[0] += 1
                                for fc in range(2):
                                    S.op('pe', lambda e: e.matmul(out=pyd[y_][:, :], lhsT=aT[gb][:, fc, tt * 128:(tt + 1) * 128],
                                                                  rhs=wdsel[wb][:, fc, nh * 512:(nh + 1) * 512], start=(fc == 0), stop=(fc == 1)),
                                         reads=[('aT', gb), ('wdc' if kind == 'ctx' else 'wd', wb)], writes=[('pyd', y_)], inc=(fc == 1))
                                xs = x_res[:, tI, nh * 512:(nh + 1) * 512]
                                S.op('dve', lambda e: e.tensor_tensor(out=xs, in0=pyd[y_][:, :], in1=xs, op=ALU.add),
                                     reads=[('pyd', y_), ('xres', tI)], writes=[('xres', tI)])
                    if pend_dn:
                        pend_dn.pop(0)()
                    pend_dn.append(down)
        while pend_dn:
            pend_dn.pop(0)()
        S.barrier()

    with ExitStack() as pf_:
        lnbc = sb('lnbc2', [128, 2, D], st=pf_)
        LN_['t'] = lnbc
        S.dma('sp', lnbc[:, 0, :], I['lnv'][:, 2 * D:3 * D].to_broadcast([128, D]), writes=['lnbc'])
        S.dma('sp', lnbc[:, 1, :], I['lnv'][:, 3 * D:4 * D].to_broadcast([128, D]), writes=['lnbc'])
        pre = [sb('pree%d' % i, [128, D], st=pf_) for i in range(2)]
        for tI, (kind, t, col0) in enumerate(tilesC):
            b = tI % 2
            S.op('act', lambda e: e.copy(out=pre[b][:, :], in_=x_res[:, tI, :]), reads=[('xres', tI)], writes=[('pree', b)])
            post_ln(tI, pre[b], ('pree', b))
            dst = x_out[t * 128:(t + 1) * 128, :] if kind == 'own' else ctx_out[t * 128:(t + 1) * 128, :]
            S.dma('sp', dst, x_res[:, tI, :], reads=[('xres', tI)], writes=[('out', tI)])
        S.barrier()
    return finish(nc, S, es, x_out, ctx_out)


def finish(nc, S, es, x_out, ctx_out):
    S.barrier()
    es.close()
    return nc


def _rope_cs(n_tok):
    t = np.arange(n_tok)
    row = (t // GRID_W).astype(np.float32); col = (t % GRID_W).astype(np.float32)
    inv = (10000.0 ** (-np.arange(16, dtype=np.float32) / 16)).astype(np.float32)
    ang = np.stack([row[:, None] * inv, col[:, None] * inv], axis=1).astype(np.float32)
    return np.concatenate([np.cos(ang).reshape(n_tok, 32), np.sin(ang).reshape(n_tok, 32)], axis=1).astype(np.float32)


def _invcnt(n, t0, L):
    t = np.arange(t0, t0 + L)
    out = np.zeros((128, 2 * L), np.float32)
    for ch, (wa, wb) in enumerate(((2, 4), (8, 16))):
        for h, w in enumerate((wa, wb)):
            lo = np.clip(t - w // 2, 0, n); hi = np.clip(t + (w - w // 2), 0, n)
            out[h * 64:(h + 1) * 64, ch * L:(ch + 1) * L] = (1.0 / (hi - lo).astype(np.float32))[None, :]
    return out


_NC_CACHE = {}


def _layer_inputs(l, x, ctx_x, P):
    f = np.float32
    common = {
        'x_all': np.ascontiguousarray(x, f), 'ctx': np.ascontiguousarray(ctx_x, f),
        'cT': np.ascontiguousarray(np.stack([P['c'].reshape(8, 128).T, P['c_ctx'].reshape(8, 128).T], axis=2).reshape(128, 16), f),
        'w_mod': np.ascontiguousarray(P['w_mod'][l], f), 'b_mod': np.ascontiguousarray(P['b_mod'][l][None, :], f),
        'w_in': np.ascontiguousarray(P['w_in'][l], f),
        'qk_gain': np.ascontiguousarray(np.concatenate([P['q_gain'][l], P['k_gain'][l]])[None, :], f),
        'w_pool': np.ascontiguousarray(P['w_pool'][l].reshape(256, 64), f),
        'cvp': np.ascontiguousarray(np.stack([P['b_dw'][l].reshape(2, 128).T, P['cv_ln_g'][l].reshape(2, 128).T,
                                              P['cv_ln_b'][l].reshape(2, 128).T, P['pool_scale'][l].reshape(2, 128).T],
                                             axis=2).reshape(128, 8), f),
        'w_dwT': np.ascontiguousarray(P['w_dw'][l].reshape(31, 2, 128).transpose(2, 1, 0).reshape(128, 62), f),
        'w_pw': np.ascontiguousarray(P['w_cv_pw'][l], f), 'w_out': np.ascontiguousarray(P['w_out'][l], f),
        'lnv': np.ascontiguousarray(np.concatenate([P['ln1_g'][l], P['ln1_b'][l], P['ln2_g'][l], P['ln2_b'][l]])[None, :], f),
        'w_r': np.ascontiguousarray(np.concatenate([P['w_rg'][l], P['w_re'][l]], axis=1), f),
        'b_r': np.ascontiguousarray(np.concatenate([P['b_rg'][l], P['b_re'][l]])[None, :], f),
        'w_eg': np.ascontiguousarray(P['w_e_gate'][l], f), 'w_eu': np.ascontiguousarray(P['w_e_up'][l], f),
        'w_ed': np.ascontiguousarray(P['w_e_down'][l], f),
        'cs_all': _rope_cs(SEQ),
        'sel': np.ascontiguousarray(np.repeat(np.eye(32, dtype=f), 128, axis=1)),
        'selrow': np.ascontiguousarray(np.concatenate([np.repeat(np.eye(64, dtype=f)[:, 0:1], 128, 1),
                                                       np.repeat(np.eye(64, dtype=f)[:, 32:33], 128, 1)], axis=1)),
        'ident_f': np.eye(128, dtype=f), 'ident_b': np.eye(128, dtype=f).astype(ml_dtypes.bfloat16),
    }
    ic_ctx = _invcnt(NCTX, 0, NCTX)
    maps = []
    for r in range(NCORE):
        m = dict(common)
        t0 = r * NTOK
        m['x_own'] = np.ascontiguousarray(x[t0:t0 + NTOK], f)
        xh = np.zeros((32, D), f); hm = np.zeros((128, 32), f)
        if r > 0:
            xh[0:16] = x[t0 - 16:t0]; hm[:, 0:16] = 1.0
        if r < NCORE - 1:
            xh[16:32] = x[t0 + NTOK:t0 + NTOK + 16]; hm[:, 16:32] = 1.0
        m['x_halo'] = xh; m['hmask'] = hm
        m['cs_own'] = np.ascontiguousarray(common['cs_all'][t0:t0 + NTOK])
        m['invcnt'] = np.ascontiguousarray(np.concatenate([_invcnt(SEQ, t0, NTOK), ic_ctx], axis=1))
        maps.append(m)
    return maps


def kernel(**inputs):
    P = {k: np.asarray(v) for k, v in inputs.items()}
    x = P['x'][0]
    ctx_x = P['ctx'][0]
    P['c'] = P['c'].reshape(-1)
    if 'nc' not in _NC_CACHE:
        _NC_CACHE['nc'] = build()
    nc = _NC_CACHE['nc']
    for l in range(DEPTH):
        maps = _layer_inputs(l, x, ctx_x, P)
        res = run_bass_kernel_spmd(nc, maps, core_ids=list(range(NCORE)))
        x = np.concatenate([np.asarray(res.results[r]['x_out']) for r in range(NCORE)], axis=0)
        ctx_x = np.asarray(res.results[0]['ctx_out'])
    return x[None].astype(np.float32)
```

```python
import numpy as np
import ml_dtypes
from contextlib import ExitStack
import concourse.bass as bass
import concourse.mybir as mybir
from concourse.bass_utils import run_bass_kernel_spmd

F32 = mybir.dt.float32
BF = mybir.dt.bfloat16
ALU = mybir.AluOpType
AF = mybir.ActivationFunctionType
AX = mybir.AxisListType

D = 1024
SEQ = 16384
NCORE = 8
NTOK = SEQ // NCORE
NTI = NTOK // 128
NCTX = 256
NKC = (SEQ + NCTX) // 128
KEYS = SEQ + NCTX
HTW = NTOK + NCTX + 32
HAL0 = NTOK + NCTX
NEXP = 32
DEPTH = 2
ALPHA = (2 * DEPTH) ** 0.25
EPS = 1e-6
GRID_W = 64


class Sched:
    def __init__(self, nc, es):
        self.nc = nc
        self.E = {'pe': nc.tensor, 'act': nc.scalar, 'dve': nc.vector, 'pool': nc.gpsimd, 'sp': nc.sync}
        self.sem = {k: es.enter_context(nc.semaphore('c_' + k)) for k in self.E}
        self.cnt = {k: 0 for k in self.E}
        self.seen = {k: {} for k in self.E}
        self.W = {}
        self.R = {}
        self.ND = 32
        self.dsem = [es.enter_context(nc.semaphore('d%d' % i)) for i in range(self.ND)]
        self.dval = [0] * self.ND
        self.di = 0
        self.nwait = 0

    def _wait(self, eng, dep):
        sem, val, key = dep
        if self.seen[eng].get(key, 0) >= val:
            return
        self.seen[eng][key] = val
        self.E[eng].wait_ge(sem, val)
        self.nwait += 1

    def _deps(self, eng, reads, writes):
        for k in reads:
            d = self.W.get(k)
            if d is not None and not (eng == 'pe' and d[2] == 'pe'):
                self._wait(eng, d)
        for k in writes:
            d = self.W.get(k)
            if d is not None and not (eng == 'pe' and d[2] == 'pe'):
                self._wait(eng, d)
            for d in self.R.get(k, {}).values():
                if not (eng == 'pe' and d[2] == 'pe'):
                    self._wait(eng, d)

    def _reg(self, dep, reads, writes):
        for k in writes:
            self.W[k] = dep
            self.R[k] = {}
        for k in reads:
            self.R.setdefault(k, {})[dep[2]] = dep

    def op(self, eng, fn, reads=(), writes=(), inc=True):
        self._deps(eng, reads, writes)
        ins = fn(self.E[eng])
        if inc:
            self.cnt[eng] += 1
            ins.then_inc(self.sem[eng], 1)
            dep = (self.sem[eng], self.cnt[eng], eng)
        else:
            dep = (self.sem[eng], self.cnt[eng] + 1, eng)
        self._reg(dep, reads, writes)

    def dma(self, q, out, in_, reads=(), writes=()):
        i = self.di
        self.di = (self.di + 1) % self.ND
        if self.dval[i] > 0:
            self._wait(q, (self.dsem[i], self.dval[i], ('d', i)))
        self._deps(q, reads, writes)
        self.dval[i] += 16
        self.E[q].dma_start(out=out, in_=in_).then_inc(self.dsem[i], 16)
        dep = (self.dsem[i], self.dval[i], ('d', i))
        self._reg(dep, reads, writes)

    def barrier(self, engs=None):
        engs = engs or list(self.E)
        for e in engs:
            for f in self.E:
                if f != e and self.cnt[f] > 0:
                    self._wait(e, (self.sem[f], self.cnt[f], f))
            for i in range(self.ND):
                if self.dval[i] > 0:
                    self._wait(e, (self.dsem[i], self.dval[i], ('d', i)))


INPUTS = [
    ('x_all', [SEQ, D], F32), ('x_own', [NTOK, D], F32), ('x_halo', [32, D], F32), ('ctx', [NCTX, D], F32),
    ('cT', [128, 16], F32), ('w_mod', [D, 6 * D], F32), ('b_mod', [1, 6 * D], F32), ('w_in', [D, 1536], F32),
    ('qk_gain', [1, 128], F32), ('w_pool', [256, 64], F32), ('cvp', [128, 8], F32), ('w_dwT', [128, 62], F32),
    ('w_pw', [256, 256], F32), ('w_out', [D, D], F32), ('lnv', [1, 4 * D], F32), ('w_r', [D, 36], F32),
    ('b_r', [1, 36], F32), ('w_eg', [NEXP, D, 512], F32), ('w_eu', [NEXP, D, 512], F32),
    ('w_ed', [NEXP, 512, D], F32), ('cs_all', [SEQ, 64], F32), ('cs_own', [NTOK, 64], F32),
    ('invcnt', [128, 2 * (NTOK + NCTX)], F32), ('hmask', [128, 32], F32), ('sel', [32, NEXP * 128], F32),
    ('selrow', [64, 256], F32), ('ident_f', [128, 128], F32), ('ident_b', [128, 128], BF),
]


def build(stop_after=99, dbg=False):
    nc = bass.Bass("TRN2", target_bir_lowering=False)
    I = {n: nc.dram_tensor(n, list(s), dt, kind="ExternalInput").ap() for n, s, dt in INPUTS}
    x_out = nc.dram_tensor('x_out', [NTOK, D], F32, kind="ExternalOutput").ap()
    ctx_out = nc.dram_tensor('ctx_out', [NCTX, D], F32, kind="ExternalOutput").ap()
    DBG = {}
    if dbg:
        for n, s, dt in [('d_modT', [128, 64], F32), ('d_gbc', [128, 4096], F32), ('d_KT', [128, KEYS], BF),
                         ('d_V', [128, NKC * 192], BF), ('d_QT', [128, 4, NTOK + NCTX], BF),
                         ('d_yT', [128, 4, NTOK + NCTX], BF), ('d_hT', [128, 8 * HTW], BF),
                         ('d_xres', [128, 18 * D], F32), ('d_GT', [32, NTOK + NCTX], F32)]:
            DBG[n] = nc.dram_tensor(n, list(s), dt, kind="ExternalOutput").ap()

    es = ExitStack()
    S = Sched(nc, es)

    def sb(name, shape, dt=F32, st=None):
        return (st or es).enter_context(nc.sbuf_tensor('s_' + name, list(shape), dt))

    def ps(name, shape, dt=F32, st=None):
        return (st or es).enter_context(nc.psum_tensor('p_' + name, list(shape), dt))

    def dbg_dump(name, ap, key):
        if dbg:
            S.barrier()
            S.dma('sp', DBG[name], ap, reads=[key], writes=[('dbgout', name)])
            S.barrier()

    ident_f = sb('ident_f', [128, 128]); ident_b = sb('ident_b', [128, 128], BF)
    ones_f = sb('ones_f', [128, 128]); epst = sb('epst', [128, 1])
    modT = sb('modT', [128, 64])
    gbc = sb('gbc', [128, 4, D])
    qkg = sb('qkg', [128, 128]); cvp = sb('cvp', [128, 8]); wdwT = sb('wdwT', [128, 62])
    negm = sb('negm', [128, 1]); hmask = sb('hmask', [128, 32])
    QY = sb('QY', [128, 8, NTOK + NCTX], BF)
    QT = QY[:, 0:4, :]
    yT = QY[:, 4:8, :]
    sm = sb('sm', [128, 8, 64])
    smi = [0]

    def smslot():
        smi[0] = (smi[0] + 1) % 8
        return smi[0]

    S.dma('sp', ident_f[:], I['ident_f'], writes=['ident_f'])
    S.dma('sp', ident_b[:], I['ident_b'], writes=['ident_b'])
    S.dma('sp', qkg[:], I['qk_gain'].to_broadcast([128, 128]), writes=['qkg'])
    S.dma('sp', cvp[:], I['cvp'], writes=['cvp'])
    S.dma('sp', wdwT[:], I['w_dwT'], writes=['wdwT'])
    S.dma('sp', hmask[:], I['hmask'], writes=['hmask'])
    S.op('dve', lambda e: e.memset(ones_f[:], 1.0), writes=['ones_f'])
    S.op('dve', lambda e: e.memset(epst[:], EPS), writes=['epst'])
    sl = smslot()
    S.op('dve', lambda e: e.tensor_tensor(out=sm[:, sl, 0:64], in0=qkg[:, 0:64], in1=qkg[:, 64:128], op=ALU.mult),
         reads=['qkg'], writes=[('sm', sl)])
    S.op('dve', lambda e: e.tensor_reduce(out=negm[:], in_=sm[:, sl, 0:64], axis=AX.X, op=ALU.max,
                                          apply_absolute_value=True), reads=[('sm', sl)], writes=['negm'])
    S.op('dve', lambda e: e.tensor_scalar(out=negm[:], in0=negm[:], scalar1=-8.0, scalar2=None, op0=ALU.mult),
         reads=['negm'], writes=['negm'])

    def ln_stats(src, srckey):
        return ln_stats_b(ln_stats_a(src, srckey))

    def ln_stats_a(src, srckey):
        s = smslot()
        k = ('sm', s)
        S.op('dve', lambda e: e.bn_stats(out=sm[:, s, 0:6], in_=src[:, 0:512]), reads=[srckey], writes=[k])
        S.op('dve', lambda e: e.bn_stats(out=sm[:, s, 6:12], in_=src[:, 512:1024]), reads=[srckey, k], writes=[k])
        S.op('dve', lambda e: e.bn_aggr(out=sm[:, s, 12:14], in_=sm[:, s, 0:12]), reads=[k], writes=[k])
        S.op('act', lambda e: e.activation(out=sm[:, s, 14:15], in_=sm[:, s, 13:14], func=AF.Sqrt,
                                           bias=epst[:, 0:1], scale=1.0), reads=[k, 'epst'], writes=[k])
        return s

    def ln_stats_b(s):
        k = ('sm', s)
        S.op('dve', lambda e: e.reciprocal(out=sm[:, s, 14:15], in_=sm[:, s, 14:15]), reads=[k], writes=[k])
        S.op('dve', lambda e: e.tensor_scalar(out=sm[:, s, 15:16], in0=sm[:, s, 12:13], scalar1=sm[:, s, 14:15],
                                              scalar2=-1.0, op0=ALU.mult, op1=ALU.mult), reads=[k], writes=[k])
        return sm[:, s, 14:15], sm[:, s, 15:16], k

    with ExitStack() as p0:
        cc = sb('cc', [128, 8, 64], st=p0); cTt = sb('cTt', [128, 16], st=p0)
        bmod = sb('bmod', [1, 6 * D], st=p0)
        wst = [sb('wst%d' % i, [128, 8, 512], st=p0) for i in range(2)]
        mrow = [sb('mrow%d' % i, [64, 512], st=p0) for i in range(2)]
        selrow = sb('selrow', [64, 256], st=p0)
        pm = [ps('pm%d' % i, [128, 512], st=p0) for i in range(2)]
        pt = [ps('ptm%d' % i, [128, 512], st=p0) for i in range(2)]
        S.dma('sp', cTt[:], I['cT'], writes=['cTt'])
        S.dma('sp', bmod[:], I['b_mod'], writes=['bmod'])
        S.dma('sp', selrow[:], I['selrow'], writes=['selrow'])
        S.op('dve', lambda e: e.memset(cc[:], 0.0), writes=['cc'])
        cTv = cTt[:].rearrange("p (k w) -> p k w", w=2)
        for w in range(2):
            S.op('act', lambda e: e.activation(out=cc[:, :, 32 * w:32 * w + 1], in_=cTv[:, :, w:w + 1], func=AF.Silu),
                 reads=['cTt', 'cc'], writes=['cc'])
        wmv = I['w_mod'].rearrange("(k p) n -> p k n", p=128)
        for nb in range(12):
            b = nb % 2
            S.dma('sp', wst[b][:], wmv[:, :, nb * 512:(nb + 1) * 512], writes=[('wst', b)])
            for k in range(8):
                S.op('pe', lambda e: e.matmul(out=pm[b][0:64, :], lhsT=cc[:, k, :], rhs=wst[b][:, k, :],
                                              start=(k == 0), stop=False),
                     reads=['cc', ('wst', b)], writes=[('pm', b)], inc=False)
            S.op('pe', lambda e: e.matmul(out=pm[b][0:64, :], lhsT=ones_f[0:1, 0:64],
                                          rhs=bmod[0:1, nb * 512:(nb + 1) * 512], start=False, stop=True),
                 reads=['ones_f', 'bmod'], writes=[('pm', b)])
            S.op('act', lambda e: e.copy(out=mrow[b][:], in_=pm[b][0:64, :]), reads=[('pm', b)], writes=[('mrow', b)])
            kind6 = nb // 2
            if kind6 in (2, 5):
                for w in range(2):
                    S.op('pe', lambda e: e.matmul(out=pt[w][:, :], lhsT=selrow[:, w * 128:(w + 1) * 128],
                                                  rhs=mrow[b][:], start=True, stop=True),
                         reads=['selrow', ('mrow', b)], writes=[('pt', w)])
                    gi = (0 if kind6 == 2 else 2) + w
                    S.op('act', lambda e: e.copy(out=gbc[:, gi, (nb % 2) * 512:(nb % 2) * 512 + 512], in_=pt[w][:, :]),
                         reads=[('pt', w)], writes=['gbc'])
            else:
                kind = {0: 0, 1: 1, 3: 2, 4: 3}[kind6]
                for j in range(4):
                    k = (nb % 2) * 4 + j
                    S.op('pe', lambda e: e.transpose(out=pt[0][:, j * 64:(j + 1) * 64],
                                                     in_=mrow[b][:, j * 128:(j + 1) * 128], identity=ident_f[0:64, 0:64]),
                         reads=[('mrow', b), 'ident_f'], writes=[('pt', 0)], inc=(j == 3))
                ptv = pt[0][:, 0:256].rearrange("p (j c) -> p j c", c=64)
                mv = modT[:, kind * 16 + (nb % 2) * 8: kind * 16 + (nb % 2) * 8 + 8].rearrange("p (j w) -> p j w", w=2)
                for w in range(2):
                    S.op('dve', lambda e: e.tensor_scalar(out=mv[:, :, w:w + 1], in0=ptv[:, :, 32 * w:32 * w + 1],
                                                          scalar1=(1.0 if kind in (1, 3) else 0.0), scalar2=None,
                                                          op0=ALU.add), reads=[('pt', 0)], writes=['modT'])
        dbg_dump('d_modT', modT[:], 'modT')
        dbg_dump('d_gbc', gbc[:].rearrange("p a b -> p (a b)"), 'gbc')
        S.barrier()
    if stop_after <= 0:
        return finish(nc, S, es, x_out, ctx_out)

    def SC(kind, k, w):
        i = kind * 16 + k * 2 + w
        return modT[:, i:i + 1]

    def ln_mod_T(src, srckey, ntok, kinds, w, dst_ap_fn, dstkey, xnb, xnbkey, ptr, ptrkey):
        ln_mod_a(src, srckey, ntok, xnb, xnbkey)
        ln_mod_b(ntok, kinds, w, dst_ap_fn, dstkey, xnb, xnbkey, ptr, ptrkey)

    def ln_mod_a(src, srckey, ntok, xnb, xnbkey, s_=None):
        rstd, nmr, k = ln_stats(src, srckey) if s_ is None else ln_stats_b(s_)
        S.op('act', lambda e: e.activation(out=xnb[0:ntok, :], in_=src[0:ntok, :], func=AF.Identity, bias=nmr[0:ntok],
                                           scale=rstd[0:ntok]), reads=[srckey, k], writes=[xnbkey])

    def ln_mod_b(ntok, kinds, w, dst_ap_fn, dstkey, xnb, xnbkey, ptr, ptrkey):
        for kk in range(8):
            S.op('pe', lambda e: e.transpose(out=ptr[:, kk, 0:ntok], in_=xnb[0:ntok, kk * 128:(kk + 1) * 128],
                                             identity=ident_b[0:ntok, 0:ntok]),
                 reads=[xnbkey, 'ident_b'], writes=[ptrkey], inc=(kk == 7))
        for kk in range(8):
            if kk % 2 == 0:
                S.op('dve', lambda e: e.tensor_scalar(out=dst_ap_fn(kk), in0=ptr[:, kk, 0:ntok],
                                                      scalar1=SC(kinds[1], kk, w), scalar2=SC(kinds[0], kk, w),
                                                      op0=ALU.mult, op1=ALU.add),
                     reads=[ptrkey, 'modT'], writes=[dstkey])
            else:
                S.op('act', lambda e: e.activation(out=dst_ap_fn(kk), in_=ptr[:, kk, 0:ntok], func=AF.Identity,
                                                   bias=SC(kinds[0], kk, w), scale=SC(kinds[1], kk, w)),
                     reads=[ptrkey, 'modT'], writes=[dstkey])

    def hk(key):
        return [key]

    def rms_rope(src, srckey, nh, gain_ap, cs, cskey, dst, dstkey, tmp, tmpkey, rope, part=None, st=None):
        if part != 'b':
            s = smslot()
        else:
            s = st
        k = ('sm', s)
        W_ = nh * 64
        if part == 'b':
            src = tmp[:, 2 * W_:3 * W_]
            srckey = tmpkey
        else:
            _rms_a(src, srckey, nh, tmp, tmpkey, s)
            src = tmp[:, 2 * W_:3 * W_]
            srckey = tmpkey
            if part == 'a':
                return s
        _rms_b(src, srckey, nh, gain_ap, cs, cskey, dst, dstkey, tmp, tmpkey, rope, s)

    def _rms_a(src, srckey, nh, tmp, tmpkey, s):
        k = ('sm', s)
        W_ = nh * 64
        src_ps = src
        src = tmp[:, 2 * W_:3 * W_]
        S.op('act', lambda e: e.copy(out=src, in_=src_ps), reads=[srckey], writes=[tmpkey])
        srckey = tmpkey
        S.op('dve', lambda e: e.tensor_tensor(out=tmp[:, 0:W_], in0=src, in1=src, op=ALU.mult),
             reads=[srckey], writes=[tmpkey])
        S.op('dve', lambda e: e.tensor_reduce(out=sm[:, s, 0:nh], in_=tmp[:, 0:W_].rearrange("p (h d) -> p h d", d=64),
                                              axis=AX.X, op=ALU.add), reads=[tmpkey], writes=[k])
        S.op('act', lambda e: e.activation(out=sm[:, s, 0:nh], in_=sm[:, s, 0:nh], func=AF.Sqrt, bias=epst[:, 0:1],
                                           scale=1.0 / 64), reads=[k, 'epst'], writes=[k])

    def _rms_b(src, srckey, nh, gain_ap, cs, cskey, dst, dstkey, tmp, tmpkey, rope, s):
        k = ('sm', s)
        W_ = nh * 64
        S.op('dve', lambda e: e.reciprocal(out=sm[:, s, 0:nh], in_=sm[:, s, 0:nh]), reads=[k], writes=[k])
        EW = 'pool' if nh == 2 else 'dve'
        t3 = tmp[:, 0:W_].rearrange("p (h d) -> p h d", d=64)
        S.op(EW, lambda e: e.tensor_tensor(out=t3, in0=src.rearrange("p (h d) -> p h d", d=64),
                                              in1=sm[:, s, 0:nh].unsqueeze(2).to_broadcast([128, nh, 64]), op=ALU.mult),
             reads=[srckey, k], writes=[tmpkey])
        gdst = t3 if rope else dst.rearrange("p (h d) -> p h d", d=64)
        S.op(EW, lambda e: e.tensor_tensor(out=gdst, in0=t3, in1=gain_ap.unsqueeze(1).to_broadcast([128, nh, 64]),
                                              op=ALU.mult), reads=[tmpkey, 'qkg'], writes=[tmpkey if rope else dstkey])
        if not rope:
            return
        t5 = tmp[:, 0:W_].rearrange("p (h a b f) -> p h a b f", a=2, b=2, f=16)
        d5 = dst.rearrange("p (h a b f) -> p h a b f", a=2, b=2, f=16)
        u5 = tmp[:, W_:2 * W_].rearrange("p (h a b f) -> p h a b f", a=2, b=2, f=16)
        cosb = cs[:, 0:32].rearrange("p (a f) -> p a f", f=16).unsqueeze(1).to_broadcast([128, nh, 2, 16])
        sinb = cs[:, 32:64].rearrange("p (a f) -> p a f", f=16).unsqueeze(1).to_broadcast([128, nh, 2, 16])
        t1, t2 = t5[:, :, :, 0, :], t5[:, :, :, 1, :]
        ua, ub = u5[:, :, :, 0, :], u5[:, :, :, 1, :]
        rk = [tmpkey, cskey]
        S.op(EW, lambda e: e.tensor_tensor(out=ua, in0=t1, in1=cosb, op=ALU.mult), reads=rk, writes=[tmpkey])
        S.op(EW, lambda e: e.tensor_tensor(out=ub, in0=t2, in1=sinb, op=ALU.mult), reads=rk, writes=[tmpkey])
        S.op(EW, lambda e: e.tensor_tensor(out=d5[:, :, :, 0, :], in0=ua, in1=ub, op=ALU.subtract),
             reads=[tmpkey], writes=[dstkey])
        S.op(EW, lambda e: e.tensor_tensor(out=ua, in0=t2, in1=cosb, op=ALU.mult), reads=rk, writes=[tmpkey])
        S.op(EW, lambda e: e.tensor_tensor(out=ub, in0=t1, in1=sinb, op=ALU.mult), reads=rk, writes=[tmpkey])
        S.op(EW, lambda e: e.tensor_tensor(out=d5[:, :, :, 1, :], in0=ua, in1=ub, op=ALU.add),
             reads=[tmpkey], writes=[dstkey])

    LP = 16 + NTOK + 16
    LC = 16 + NCTX + 16
    with ExitStack() as pa:
        hT = sb('hT', [128, 8, HTW], BF, st=pa)
        uT = sb('uT', [128, 2, LP], st=pa); cuT = sb('cuT', [128, 2, LP], st=pa)
        uTc = sb('uTc', [128, 2, LC], st=pa); cuTc = sb('cuTc', [128, 2, LC], st=pa)
        with ExitStack() as pa1:
            winb = sb('winb', [128, 8, 1536], BF, st=pa1)
            xt = [sb('xt%d' % i, [128, D], st=pa1) for i in range(3)]
            xnb = [sb('xnb%d' % i, [128, D], BF, st=pa1) for i in range(2)]
            wst = [sb('wsta%d' % i, [128, 8, 512], st=pa1) for i in range(1)]
            qtmp = [sb('qtmp%d' % i, [128, 1536], st=pa1) for i in range(2)]
            qrb = [sb('qrb%d' % i, [128, 512], BF, st=pa1) for i in range(2)]
            cst = [sb('cst%d' % i, [128, 64], st=pa1) for i in range(2)]
            sig = [sb('sig%d' % i, [128, 512], st=pa1) for i in range(2)]
            ptr = [ps('ptr%d' % i, [128, 8, 128], BF, st=pa1) for i in range(2)]
            pq = [ps('pq%d' % i, [128, 512], st=pa1) for i in range(2)]
            pqt = [ps('pqt%d' % i, [128, 4, 128], BF, st=pa1) for i in range(2)]
            pf = [ps('pf%d' % i, [128, 512], st=pa1) for i in range(2)]
            wiv = I['w_in'].rearrange("(k p) n -> p k n", p=128)
            for nb in range(3):
                b = 0
                S.dma('sp', wst[b][:], wiv[:, :, nb * 512:(nb + 1) * 512], writes=[('wsta', b)])
                S.op('pool' if nb == 1 else 'dve', lambda e: e.tensor_copy(out=winb[:, :, nb * 512:(nb + 1) * 512], in_=wst[b][:]),
                     reads=[('wsta', b)], writes=['winb'])
            tiles = [('own', t) for t in range(NTI)] + [('ctx', t) for t in range(2)] + [('halo', 0)]
            for ti, (kind, t) in enumerate(tiles):
                b3 = ti % 3; b = ti % 2
                ntok = 32 if kind == 'halo' else 128
                src = {'own': I['x_own'], 'ctx': I['ctx'], 'halo': I['x_halo']}[kind]
                col0 = {'own': t * 128, 'ctx': NTOK + t * 128, 'halo': HAL0}[kind]
                w = 1 if kind == 'ctx' else 0
                S.dma('sp', xt[b3][0:ntok, :], src[t * 128:t * 128 + ntok, :], writes=[('xt', b3)])
                ln_mod_T(xt[b3], ('xt', b3), ntok, (0, 1), w,
                         lambda kk: hT[:, kk, col0:col0 + ntok], ('hT', ti), xnb[b], ('xnb', b), ptr[b], ('ptr', b))
                if kind == 'halo':
                    continue
                for kk in range(8):
                    S.op('pe', lambda e: e.matmul(out=pq[b][:, :], lhsT=hT[:, kk, col0:col0 + 128], rhs=winb[:, kk, 0:512],
                                                  start=(kk == 0), stop=(kk == 7)),
                         reads=hk(('hT', ti)) + ['winb'], writes=[('pq', b)], inc=(kk == 7))
                if kind == 'own':
                    S.dma('sp', cst[b][:], I['cs_own'][t * 128:(t + 1) * 128, :], writes=[('cst', b)])
                rms_rope(pq[b][:, :], ('pq', b), 8, qkg[:, 0:64], cst[b], ('cst', b), qrb[b][:, :], ('qrb', b),
                         qtmp[b], ('qtmp', b), rope=(kind == 'own'))
                for h_ in range(8):
                    g_, j_ = h_ // 4, h_ % 4
                    S.op('pe', lambda e: e.transpose(out=pqt[b][g_ * 64:(g_ + 1) * 64, j_, :], in_=qrb[b][:, h_ * 64:(h_ + 1) * 64],
                                                     identity=ident_b[:, :]),
                         reads=[('qrb', b), 'ident_b'], writes=[('pqt', b)], inc=(h_ == 7))
                S.op('act', lambda e: e.copy(out=QT[:, :, col0:col0 + 128], in_=pqt[b][:, :, :]),
                     reads=[('pqt', b)], writes=[('QT', col0 // 512)])
            blocks = [(i * 512, 512, 'own') for i in range(4)] + [(NTOK, 256, 'ctx'), (HAL0, 32, 'halo')]
            allhT = [k_ for ti in range(len(tiles)) for k_ in hk(('hT', ti))]
            pi = 0
            for (c0, n, kind) in blocks:
                def dsts(tn, ch):
                    if kind == 'own':
                        return tn[:, ch, 16 + c0:16 + c0 + n]
                    if kind == 'ctx':
                        return tn[:, ch, 16:16 + NCTX]
                    return tn[:, ch, :].rearrange("p (a b) -> p a b", a=2)[:, :, 0:16] if False else None
                for ch in range(2):
                    pu_, pa_, pg_ = None, None, None
                    res = {}
                    for which, cc0 in (('u', 768), ('g', 1280), ('a', 1024)):
                        b = pi % 2; pi += 1
                        for kk in range(8):
                            S.op('pe', lambda e: e.matmul(out=pf[b][:, 0:n], lhsT=winb[:, kk, cc0 + ch * 128:cc0 + ch * 128 + 128],
                                                          rhs=hT[:, kk, c0:c0 + n], start=(kk == 0), stop=(kk == 7)),
                                 reads=allhT + ['winb'], writes=[('pf', b)], inc=(kk == 7))
                        if kind == 'halo':
                            def hal(tn):
                                return [(tn[:, ch, 0:16], 0), (tn[:, ch, 16 + NTOK:32 + NTOK], 16)]
                        if which == 'u':
                            if kind == 'halo':
                                for (dap, o) in hal(uT):
                                    S.op('dve', lambda e: e.tensor_tensor(out=dap, in0=pf[b][:, o:o + 16], in1=hmask[:, o:o + 16],
                                                                          op=ALU.mult), reads=[('pf', b), 'hmask'], writes=['uT'])
                            else:
                                S.op('act', lambda e: e.copy(out=dsts(uT if kind == 'own' else uTc, ch), in_=pf[b][:, 0:n]),
                                     reads=[('pf', b)], writes=['uT'])
                        elif which == 'g':
                            sb_ = b
                            S.op('act', lambda e: e.activation(out=sig[sb_][:, 0:n], in_=pf[b][:, 0:n], func=AF.Sigmoid),
                                 reads=[('pf', b)], writes=[('sig', sb_)])
                        else:
                            if kind == 'halo':
                                S.op('dve', lambda e: e.tensor_tensor(out=sig[sb_][:, 0:32], in0=sig[sb_][:, 0:32], in1=hmask[:, :],
                                                                      op=ALU.mult), reads=[('sig', sb_), 'hmask'], writes=[('sig', sb_)])
                                for (dap, o) in hal(cuT):
                                    S.op('dve', lambda e: e.tensor_tensor(out=dap, in0=pf[b][:, o:o + 16], in1=sig[sb_][:, o:o + 16],
                                                                          op=ALU.mult), reads=[('pf', b), ('sig', sb_)], writes=['cuT'])
                            else:
                                S.op('dve', lambda e: e.tensor_tensor(out=dsts(cuT if kind == 'own' else cuTc, ch), in0=pf[b][:, 0:n],
                                                                      in1=sig[sb_][:, 0:n], op=ALU.mult),
                                     reads=[('pf', b), ('sig', sb_)], writes=['cuT'])
            for tn, kname in ((uTc, 'uT'), (cuTc, 'cuT')):
                S.op('pool', lambda e: e.memset(tn[:, :, 0:16], 0.0), writes=[kname])
                S.op('pool', lambda e: e.memset(tn[:, :, 16 + NCTX:32 + NCTX], 0.0), writes=[kname])
            S.barrier()
        dbg_dump('d_hT', hT[:].rearrange("p a b -> p (a b)"), ('hT', 0))

        with ExitStack() as pa2:
            pl = [sb('pl%d' % i, [128, LP], st=pa2) for i in range(2)]
            pooled = sb('pooled', [128, NTOK], BF, st=pa2)
            icn = sb('icn', [128, NTOK], st=pa2)
            wpbd = sb('wpbd', [128, 2, 128], BF, st=pa2); wpst = sb('wpst', [128, 2, 128], st=pa2)
            wpwb = sb('wpwb', [128, 2, 256], BF, st=pa2); wpws = sb('wpws', [128, 2, 256], st=pa2)
            acc = sb('acc', [128, 2, NTOK], st=pa2)
            sq = [sb('sq%d' % i, [128, 512], st=pa2) for i in range(2)]
            mean = sb('mean', [128, 512], st=pa2); rstd = sb('rstdc', [128, 512], st=pa2)
            zT = sb('zT', [128, 2, 512], BF, st=pa2)
            pp = [ps('pp%d' % i, [128, 512], st=pa2) for i in range(2)]
            ps1 = ps('ps1', [128, 512], st=pa2); ps2 = ps('ps2', [128, 512], st=pa2)
            pw = [ps('pw%d' % i, [128, 512], st=pa2) for i in range(2)]
            S.op('dve', lambda e: e.memset(wpst[:], 0.0), writes=['wpst'])
            for g in range(4):
                h = (g % 2) * 64
                S.dma('sp', wpst[h:h + 64, g // 2, h:h + 64], I['w_pool'][g * 64:(g + 1) * 64, :], reads=[], writes=['wpst'])
            S.op('dve', lambda e: e.tensor_copy(out=wpbd[:], in_=wpst[:]), reads=['wpst'], writes=['wpbd'])
            S.dma('sp', wpws[:], I['w_pw'].rearrange("(k p) n -> p k n", p=128), writes=['wpws'])
            S.op('dve', lambda e: e.tensor_copy(out=wpwb[:], in_=wpws[:]), reads=['wpws'], writes=['wpwb'])

            for (kind, U_, CU_, L, ycol0, ic0) in (('own', uT, cuT, NTOK, 0, 0), ('ctx', uTc, cuTc, NCTX, NTOK, 2 * NTOK)):
                LL = L + 32
                nblk = [(i * 512, 512) for i in range(L // 512)] if L >= 512 else [(0, L)]
                for ch in range(2):
                    U = U_[:, ch, :]
                    A, B = pl[0], pl[1]
                    S.dma('sp', icn[:, 0:L], I['invcnt'][:, ic0 + ch * L: ic0 + (ch + 1) * L], writes=['icn'])
                    S.op('dve', lambda e: e.tensor_tensor(out=A[:, 1:LL], in0=U[:, 0:LL - 1], in1=U[:, 1:LL], op=ALU.add),
                         reads=['uT'], writes=['plA'])
                    S.op('dve', lambda e: e.tensor_tensor(out=B[:, 2:LL - 1], in0=A[:, 1:LL - 2], in1=A[:, 3:LL], op=ALU.add),
                         reads=['plA'], writes=['plB'])
                    if ch == 0:
                        lo, hi = A, B
                    else:
                        S.op('dve', lambda e: e.tensor_tensor(out=A[:, 4:LL - 3], in0=B[:, 2:LL - 5], in1=B[:, 6:LL - 1], op=ALU.add),
                             reads=['plB'], writes=['plA'])
                        S.op('dve', lambda e: e.tensor_tensor(out=B[:, 8:LL - 7], in0=A[:, 4:LL - 11], in1=A[:, 12:LL - 3], op=ALU.add),
                             reads=['plA'], writes=['plB'])
                        lo, hi = A, B
                    for (h0, srcw, kk_) in ((0, lo, 'plA'), (64, hi, 'plB')):
                        S.op('dve', lambda e: e.tensor_tensor(out=srcw[h0:h0 + 64, 16:16 + L], in0=srcw[h0:h0 + 64, 16:16 + L],
                                                              in1=icn[h0:h0 + 64, 0:L], op=ALU.mult),
                             reads=[kk_, 'icn'], writes=[kk_])
                        S.op('dve', lambda e: e.tensor_tensor(out=pooled[h0:h0 + 64, 0:L], in0=srcw[h0:h0 + 64, 16:16 + L],
                                                              in1=U[h0:h0 + 64, 16:16 + L], op=ALU.subtract),
                             reads=[kk_, 'uT'], writes=['pooled'])
                    for bi, (c0, n) in enumerate(nblk):
                        b = bi % 2
                        S.op('pe', lambda e: e.matmul(out=pp[b][:, 0:n], lhsT=wpbd[:, ch, :], rhs=pooled[:, c0:c0 + n],
                                                      start=True, stop=True), reads=['wpbd', 'pooled'], writes=[('pp', b)])
                        S.op('act', lambda e: e.activation(out=yT[:, ch, ycol0 + c0:ycol0 + c0 + n], in_=pp[b][:, 0:n],
                                                           func=AF.Identity, bias=0.0, scale=cvp[:, ch * 4 + 3:ch * 4 + 4]),
                             reads=[('pp', b), 'cvp'], writes=['yT'])
                for ch in range(2):
                    CU = CU_[:, ch, :]
                    a_ = acc[:, ch, 0:L]
                    S.op('dve', lambda e: e.tensor_scalar(out=a_, in0=CU[:, 1:1 + L], scalar1=wdwT[:, ch * 31:ch * 31 + 1],
                                                          scalar2=cvp[:, ch * 4:ch * 4 + 1], op0=ALU.mult, op1=ALU.add),
                         reads=['cuT', 'wdwT', 'cvp'], writes=['acc'])
                    for j in range(1, 31):
                        S.op('dve', lambda e: e.scalar_tensor_tensor(out=a_, in0=CU[:, 1 + j:1 + j + L],
                                                                     scalar=wdwT[:, ch * 31 + j:ch * 31 + j + 1], in1=a_,
                                                                     op0=ALU.mult, op1=ALU.add),
                             reads=['cuT', 'acc'], writes=['acc'])
                for bi, (c0, n) in enumerate(nblk):
                    for ch in range(2):
                        S.op('pe', lambda e: e.matmul(out=ps1[:, 0:n], lhsT=ones_f[:, :], rhs=acc[:, ch, c0:c0 + n],
                                                      start=(ch == 0), stop=(ch == 1)), reads=['acc', 'ones_f'], writes=['ps1'], inc=(ch == 1))
                    for ch in range(2):
                        S.op('act', lambda e: e.activation(out=sq[ch][:, 0:n], in_=acc[:, ch, c0:c0 + n], func=AF.Square),
                             reads=['acc'], writes=[('sq', ch)])
                        S.op('pe', lambda e: e.matmul(out=ps2[:, 0:n], lhsT=ones_f[:, :], rhs=sq[ch][:, 0:n],
                                                      start=(ch == 0), stop=(ch == 1)), reads=[('sq', ch), 'ones_f'], writes=['ps2'], inc=(ch == 1))
                    S.op('act', lambda e: e.activation(out=mean[:, 0:n], in_=ps1[:, 0:n], func=AF.Copy, scale=1.0 / 256),
                         reads=['ps1'], writes=['mean'])
                    S.op('dve', lambda e: e.tensor_tensor(out=rstd[:, 0:n], in0=mean[:, 0:n], in1=mean[:, 0:n], op=ALU.mult),
                         reads=['mean'], writes=['rstd'])
                    S.op('dve', lambda e: e.scalar_tensor_tensor(out=rstd[:, 0:n], in0=ps2[:, 0:n], scalar=1.0 / 256, in1=rstd[:, 0:n],
                                                                 op0=ALU.mult, op1=ALU.subtract), reads=['ps2', 'rstd'], writes=['rstd'])
                    S.op('act', lambda e: e.activation(out=rstd[:, 0:n], in_=rstd[:, 0:n], func=AF.Sqrt, bias=epst[:, 0:1], scale=1.0),
                         reads=['rstd', 'epst'], writes=['rstd'])
                    S.op('dve', lambda e: e.reciprocal(out=rstd[:, 0:n], in_=rstd[:, 0:n]), reads=['rstd'], writes=['rstd'])
                    for ch in range(2):
                        S.op('dve', lambda e: e.tensor_tensor(out=sq[ch][:, 0:n], in0=acc[:, ch, c0:c0 + n], in1=mean[:, 0:n], op=ALU.subtract),
                             reads=['acc', 'mean'], writes=[('sq', ch)])
                        S.op('dve', lambda e: e.tensor_tensor(out=sq[ch][:, 0:n], in0=sq[ch][:, 0:n], in1=rstd[:, 0:n], op=ALU.mult),
                             reads=['rstd', ('sq', ch)], writes=[('sq', ch)])
                        S.op('act', lambda e: e.activation(out=zT[:, ch, 0:n], in_=sq[ch][:, 0:n], func=AF.Silu,
                                                           bias=cvp[:, ch * 4 + 2:ch * 4 + 3], scale=cvp[:, ch * 4 + 1:ch * 4 + 2]),
                             reads=[('sq', ch), 'cvp'], writes=['zT'])
                    for dch in range(2):
                        for cc_ in range(2):
                            S.op('pe', lambda e: e.matmul(out=pw[dch][:, 0:n], lhsT=wpwb[:, cc_, dch * 128:(dch + 1) * 128],
                                                          rhs=zT[:, cc_, 0:n], start=(cc_ == 0), stop=(cc_ == 1)),
                                 reads=['zT', 'wpwb'], writes=[('pw', dch)], inc=(cc_ == 1))
                        S.op('act', lambda e: e.copy(out=yT[:, 2 + dch, ycol0 + c0:ycol0 + c0 + n], in_=pw[dch][:, 0:n]),
                             reads=[('pw', dch)], writes=['yT'])
            S.barrier()
    dbg_dump('d_QT', QY[:, 0:4, :], ('QT', 0))
    dbg_dump('d_yT', QY[:, 4:8, :], 'yT')
    if stop_after <= 1:
        return finish(nc, S, es, x_out, ctx_out)

    with ExitStack() as pb:
        KTz = [sb('KTz%d' % i, [128, KEYS], BF, st=pb) for i in range(2)]
        KT = KTz[0]
        Vg = sb('Vg', [128, NKC, 192], BF, st=pb)
        with ExitStack() as pb1:
            wkvb = sb('wkvb', [128, 8, 256], BF, st=pb1)
            xt = [sb('xtb%d' % i, [128, D], st=pb1) for i in range(3)]
            xnb = [sb('xnbb%d' % i, [128, D], BF, st=pb1) for i in range(3)]
            hTt = [sb('hTt%d' % i, [128, 8, 128], BF, st=pb1) for i in range(2)]
            ktmp = [sb('ktmp%d' % i, [128, 384], st=pb1) for i in range(2)]
            krb = [sb('krb%d' % i, [128, 128], BF, st=pb1) for i in range(2)]
            cst = [sb('cstb%d' % i, [128, 64], st=pb1) for i in range(3)]
            ptr = [ps('ptrb%d' % i, [128, 8, 128], BF, st=pb1) for i in range(2)]
            pkv = [ps('pkv%d' % i, [128, 512], st=pb1) for i in range(3)]
            pkt = [ps('pkt%d' % i, [128, 128], BF, st=pb1) for i in range(2)]
            for hh in range(2):
                stv = xt[hh][:, :].rearrange("p (k n) -> p k n", n=256)
                S.dma('sp', stv, I['w_in'].rearrange("(k p) n -> p k n", p=128)[:, hh * 4:hh * 4 + 4, 512:768], writes=[('xtb', hh)])
                S.op('dve', lambda e: e.tensor_copy(out=wkvb[:, hh * 4:hh * 4 + 4, :], in_=stv), reads=[('xtb', hh)], writes=['wkvb'])
            S.op('pool', lambda e: e.memset(KTz[0][64:128, :], 0.0), writes=['KTm'])
            S.op('pool', lambda e: e.memset(KTz[1][0:64, :], 0.0), writes=['KTm'])
            S.op('pool', lambda e: e.memset(Vg[:, :, 64:128], 0.0), writes=['Vg'])
            S.op('pool', lambda e: e.memset(Vg[:, :, 64:65], 1.0), writes=['Vg'])
            SB_ = {}; SR_ = {}

            def stB1(c):
                b3 = c % 3
                isctx = c < 2
                src = I['ctx'][c * 128:(c + 1) * 128, :] if isctx else I['x_all'][(c - 2) * 128:(c - 1) * 128, :]
                S.dma('sp', xt[b3][:], src, writes=[('xtb', b3)])
                SB_[c] = ln_stats_a(xt[b3], ('xtb', b3))

            def stB1b(c):
                b3 = c % 3
                ln_mod_a(xt[b3], ('xtb', b3), 128, xnb[b3], ('xnbb', b3), s_=SB_.pop(c))

            def stB2(c):
                b3 = c % 3; b = c % 2
                isctx = c < 2
                ln_mod_b(128, (0, 1), 1 if isctx else 0, lambda kk: hTt[b][:, kk, :], ('hTt', b),
                         xnb[b3], ('xnbb', b3), ptr[b], ('ptrb', b))
                for kk in range(8):
                    S.op('pe', lambda e: e.matmul(out=pkv[b3][:, 0:256], lhsT=hTt[b][:, kk, :], rhs=wkvb[:, kk, :],
                                                  start=(kk == 0), stop=(kk == 7)),
                         reads=hk(('hTt', b)) + ['wkvb'], writes=[('pkv', b3)], inc=(kk == 7))
                S.op('act', lambda e: e.copy(out=Vg[:, c, 0:64], in_=pkv[b3][:, 128:192]), reads=[('pkv', b3)], writes=[('Vg', c)])
                S.op('act', lambda e: e.copy(out=Vg[:, c, 128:192], in_=pkv[b3][:, 192:256]), reads=[('pkv', b3)], writes=[('Vg', c)])
                if not isctx:
                    S.dma('sp', cst[b3][:], I['cs_all'][(c - 2) * 128:(c - 1) * 128, :], writes=[('cstb', b3)])

            def stB3a(c):
                b3 = c % 3; b = c % 2
                SR_[c] = rms_rope(pkv[b3][:, 0:128], ('pkv', b3), 2, qkg[:, 64:128], cst[b3], ('cstb', b3), krb[b][:, :], ('krb', b),
                                  ktmp[b], ('ktmp', b), rope=(c >= 2), part='a')

            def stB3(c):
                b3 = c % 3; b = c % 2
                isctx = c < 2
                rms_rope(pkv[b3][:, 0:128], ('pkv', b3), 2, qkg[:, 64:128], cst[b3], ('cstb', b3), krb[b][:, :], ('krb', b),
                         ktmp[b], ('ktmp', b), rope=not isctx, part='b', st=SR_.pop(c))
                S.op('pe', lambda e: e.transpose(out=pkt[b][:, :], in_=krb[b][:, :], identity=ident_b[:, :]),
                     reads=[('krb', b), 'ident_b'], writes=[('pkt', b)])
                S.op('act', lambda e: e.copy(out=KTz[0][0:64, c * 128:(c + 1) * 128], in_=pkt[b][0:64, :]),
                     reads=[('pkt', b)], writes=[('KT', c)])
                S.op('act', lambda e: e.copy(out=KTz[1][64:128, c * 128:(c + 1) * 128], in_=pkt[b][64:128, :]),
                     reads=[('pkt', b)], writes=[('KT', c)])

            for step in range(NKC + 2):
                if step < NKC:
                    stB1(step)
                if 0 <= step - 2 < NKC:
                    stB3a(step - 2)
                if step < NKC:
                    stB1b(step)
                if 0 <= step - 2 < NKC:
                    stB3(step - 2)
                if 0 <= step - 1 < NKC:
                    stB2(step - 1)
            S.barrier()
        dbg_dump('d_KT', KT[:], ('KT', 0))
        dbg_dump('d_V', Vg[:].rearrange("p a b -> p (a b)"), ('Vg', 0))
        if stop_after <= 2:
            S.barrier()
            return finish(nc, S, es, x_out, ctx_out)

        with ExitStack() as pb2:
            PT = [sb('PT%d' % i, [128, 2, 512], BF, st=pb2) for i in range(4)]
            rr = sb('rr', [128, 512], st=pb2); bcs = [sb('bcs%d' % i, [128, 512], st=pb2) for i in range(2)]
            O = [ps('O%d' % i, [128, 512], st=pb2) for i in range(4)]
            SP = [ps('SP%d' % i, [128, 2, 512], st=pb2) for i in range(2)]
            qblocks = [(i * 512, 512, list(range(NKC))) for i in range(4)] + [(NTOK, NCTX, [0, 1])]
            pti = 0
            for (q0, n, chunks) in qblocks:
                for g in range(2):
                    gp = slice(g * 64, (g + 1) * 64)
                    vsl = slice(0, 65) if g == 0 else slice(64, 192)
                    pend = []
                    steps = [(c, pr) for c in chunks for pr in range(2)]
                    for si, (c, pr) in enumerate(steps):
                        sp_ = SP[pr]
                        for jj in range(2):
                            j = pr * 2 + jj
                            S.op('pe', lambda e: e.matmul(out=sp_[:, jj, 0:n], lhsT=KTz[g][:, c * 128:(c + 1) * 128],
                                                          rhs=QT[:, j, q0:q0 + n], start=True, stop=True),
                                 reads=[('KT', c), ('QT', q0 // 512)], writes=[('SP', pr)], inc=(jj == 1))
                        pb_ = pti % 4; pti += 1
                        S.op('act', lambda e: e.activation(out=PT[pb_][:, :, 0:n], in_=sp_[:, :, 0:n], func=AF.Exp,
                                                           bias=negm[:, 0:1], scale=0.125),
                             reads=[('SP', pr), 'negm'], writes=[('PT', pb_)])
                        if pend:
                            pend.pop(0)()
                        def pv(c=c, pr=pr, pb_=pb_):
                            for jj in range(2):
                                j = pr * 2 + jj
                                S.op('pe', lambda e: e.matmul(out=O[j][0:(65 if g == 0 else 128), 0:n], lhsT=Vg[:, c, vsl],
                                                              rhs=PT[pb_][:, jj, 0:n], start=(c == chunks[0]), stop=(c == chunks[-1])),
                                     reads=[('Vg', c), ('PT', pb_)], writes=[('O', j)], inc=(jj == 1))
                        pend.append(pv)
                    while pend:
                        pend.pop(0)()
                    srow = 64 if g == 0 else 0
                    for j in range(4):
                        bb = j % 2
                        S.op('dve', lambda e: e.reciprocal(out=rr[srow:srow + 1, 0:n], in_=O[j][srow:srow + 1, 0:n]),
                             reads=[('O', j)], writes=['rr'])
                        S.op('pe', lambda e: e.matmul(out=SP[bb][:, 0, 0:n], lhsT=ones_f[srow:srow + 1, :], rhs=rr[srow:srow + 1, 0:n],
                                                      start=True, stop=True), reads=['rr', 'ones_f'], writes=[('SP', bb)])
                        S.op('act', lambda e: e.copy(out=bcs[bb][:, 0:n], in_=SP[bb][:, 0, 0:n]), reads=[('SP', bb)], writes=[('bcs', bb)])
                        S.op('dve', lambda e: e.tensor_tensor(out=QT[gp, j, q0:q0 + n], in0=O[j][gp, 0:n], in1=bcs[bb][gp, 0:n], op=ALU.mult),
                             reads=[('O', j), ('bcs', bb)], writes=[('QT', q0 // 512)])
            S.barrier()
    if dbg:
        dbg_dump('d_QT', QY[:, 0:4, :], ('QT', 0))
    if stop_after <= 3:
        S.barrier()
        return finish(nc, S, es, x_out, ctx_out)

    x_res = sb('x_res', [128, 18, D])
    GT = sb('GT', [32, NTOK + NCTX])
    tilesC = [('own', t, t * 128) for t in range(NTI)] + [('ctx', t, NTOK + t * 128) for t in range(2)]

    LN_ = {}

    def post_ln(tI, pre, prekey):
        lnbc = LN_['t']
        rstd_, nmr_, k = ln_stats(pre, prekey)
        S.op('act', lambda e: e.activation(out=pre[:, :], in_=pre[:, :], func=AF.Identity, bias=nmr_, scale=rstd_),
             reads=[prekey, k], writes=[prekey])
        S.op('dve', lambda e: e.tensor_tensor(out=pre[:, :], in0=pre[:, :], in1=lnbc[:, 0, :], op=ALU.mult),
             reads=[prekey, 'lnbc'], writes=[prekey])
        S.op('pool', lambda e: e.tensor_tensor(out=x_res[:, tI, :], in0=pre[:, :], in1=lnbc[:, 1, :], op=ALU.add),
             reads=[prekey, 'lnbc'], writes=[('xres', tI)])

    with ExitStack() as pc:
        lnbc = sb('lnbc', [128, 2, D], st=pc)
        LN_['t'] = lnbc
        woutb = sb('woutb', [128, 8, D], BF, st=pc)
        wst = [sb('wstc%d' % i, [128, 2, D], st=pc) for i in range(2)]
        xt = [sb('xtc%d' % i, [128, D], st=pc) for i in range(2)]
        pre = [sb('pre%d' % i, [128, D], st=pc) for i in range(2)]
        py = [ps('py%d' % i, [128, 2, 512], st=pc) for i in range(2)]
        S.dma('sp', lnbc[:, 0, :], I['lnv'][:, 0:D].to_broadcast([128, D]), writes=['lnbc'])
        S.dma('sp', lnbc[:, 1, :], I['lnv'][:, D:2 * D].to_broadcast([128, D]), writes=['lnbc'])
        for j in range(4):
            b = j % 2
            for g in range(2):
                r0 = g * 256 + j * 64
                S.dma('sp', wst[b][g * 64:(g + 1) * 64, 0, :], I['w_out'][r0:r0 + 64, :], writes=[('wstc', b)])
            S.dma('sp', wst[b][:, 1, :], I['w_out'][512 + j * 128:512 + (j + 1) * 128, :], writes=[('wstc', b)])
            S.op('dve', lambda e: e.tensor_copy(out=woutb[:, j, :], in_=wst[b][:, 0, :]), reads=[('wstc', b)], writes=['woutb'])
            S.op('pool', lambda e: e.tensor_copy(out=woutb[:, 4 + j, :], in_=wst[b][:, 1, :]), reads=[('wstc', b)], writes=['woutb'])
        for tI, (kind, t, col0) in enumerate(tilesC):
            b = tI % 2
            src = I['x_own'] if kind == 'own' else I['ctx']
            S.dma('sp', xt[b][:], src[t * 128:(t + 1) * 128, :], writes=[('xtc', b)])
            for nh in range(2):
                for kk in range(8):
                    lh = QT[:, kk, col0:col0 + 128] if kk < 4 else yT[:, kk - 4, col0:col0 + 128]
                    S.op('pe', lambda e: e.matmul(out=py[b][:, nh, :], lhsT=lh, rhs=woutb[:, kk, nh * 512:(nh + 1) * 512],
                                                  start=(kk == 0), stop=(kk == 7)),
                         reads=[('QT', col0 // 512), 'yT', 'woutb'], writes=[('py', b)], inc=(kk == 7 and nh == 1))
            gi = 1 if kind == 'ctx' else 0
            S.op('dve', lambda e: e.tensor_tensor(out=pre[b][:, :], in0=py[b][:, :, :].rearrange("p a b -> p (a b)"), in1=gbc[:, gi, :], op=ALU.mult),
                 reads=[('py', b), 'gbc'], writes=[('pre', b)])
            S.op('dve', lambda e: e.scalar_tensor_tensor(out=pre[b][:, :], in0=xt[b][:, :], scalar=ALPHA, in1=pre[b][:, :],
                                                         op0=ALU.mult, op1=ALU.add), reads=[('xtc', b), ('pre', b)], writes=[('pre', b)])
            post_ln(tI, pre[b], ('pre', b))
        S.barrier()
    dbg_dump('d_xres', x_res[:].rearrange("p a b -> p (a b)"), ('xres', 0))
    if stop_after <= 4:
        S.barrier()
        return finish(nc, S, es, x_out, ctx_out)

    hT = QY
    with ExitStack() as pd:
        wrb = sb('wrb', [128, 8, 36], BF, st=pd); wrs = sb('wrs', [128, 8, 36], st=pd)
        brb = sb('brb', [1, 36], BF, st=pd); brs = sb('brs', [1, 36], st=pd)
        ones_b = sb('ones_b', [1, 128], BF, st=pd)
        xnb = [sb('xnbd%d' % i, [128, D], BF, st=pd) for i in range(2)]
        rt = sb('rt', [128, 4, 64], st=pd)
        ptr = [ps('ptrd%d' % i, [128, 8, 128], BF, st=pd) for i in range(2)]
        prr = [ps('prr%d' % i, [128, 64], st=pd) for i in range(2)]
        pgt = [ps('pgt%d' % i, [32, 128], st=pd) for i in range(2)]
        S.dma('sp', wrs[:], I['w_r'].rearrange("(k p) n -> p k n", p=128), writes=['wrs'])
        S.op('dve', lambda e: e.tensor_copy(out=wrb[:], in_=wrs[:]), reads=['wrs'], writes=['wrb'])
        S.dma('sp', brs[:], I['b_r'], writes=['brs'])
        S.op('dve', lambda e: e.tensor_copy(out=brb[:], in_=brs[:]), reads=['brs'], writes=['brb'])
        S.op('dve', lambda e: e.memset(ones_b[:], 1.0), writes=['ones_b'])
        for tI, (kind, t, col0) in enumerate(tilesC):
            b = tI % 2
            w = 1 if kind == 'ctx' else 0
            xr = x_res[:, tI, :]
            ln_mod_T(xr, ('xres', tI), 128, (2, 3), w, lambda kk: hT[:, kk, col0:col0 + 128], ('hT2', tI),
                     xnb[b], ('xnbd', b), ptr[b], ('ptrd', b))
            S.op('pool', lambda e: e.tensor_scalar(out=xr, in0=xr, scalar1=ALPHA, scalar2=None, op0=ALU.mult),
                 reads=[('xres', tI), ('xnbd', b)], writes=[('xres', tI)])
            for kk in range(8):
                S.op('pe', lambda e: e.matmul(out=prr[b][:, 0:36], lhsT=hT[:, kk, col0:col0 + 128], rhs=wrb[:, kk, :],
                                              start=(kk == 0), stop=False), reads=hk(('hT2', tI)) + ['wrb'], writes=[('prr', b)], inc=False)
            S.op('pe', lambda e: e.matmul(out=prr[b][:, 0:36], lhsT=ones_b[0:1, :], rhs=brb[0:1, :], start=False, stop=True),
                 reads=['ones_b', 'brb'], writes=[('prr', b)])
            r = rt[:, tI % 4, :]; rk = ('rt', tI % 4)
            lg = r[:, 0:36]
            S.op('act', lambda e: e.copy(out=lg, in_=prr[b][:, 0:36]), reads=[('prr', b)], writes=[rk])
            S.op('dve', lambda e: e.tensor_reduce(out=r[:, 36:37], in_=r[:, 0:4], axis=AX.X, op=ALU.max), reads=[rk], writes=[rk])
            S.op('dve', lambda e: e.tensor_scalar(out=r[:, 40:44], in0=r[:, 0:4], scalar1=r[:, 36:37], scalar2=None, op0=ALU.is_ge),
                 reads=[rk], writes=[rk])
            S.op('dve', lambda e: e.tensor_scalar(out=r[:, 37:38], in0=r[:, 36:37], scalar1=-1.0, scalar2=None, op0=ALU.mult),
                 reads=[rk], writes=[rk])
            S.op('act', lambda e: e.activation(out=r[:, 44:48], in_=r[:, 0:4], func=AF.Exp, bias=r[:, 37:38], scale=1.0,
                                               accum_out=r[:, 38:39]), reads=[rk], writes=[rk])
            S.op('dve', lambda e: e.tensor_scalar(out=r[:, 40:44], in0=r[:, 40:44], scalar1=-1.0, scalar2=1.0e4, op0=ALU.add, op1=ALU.mult),
                 reads=[rk], writes=[rk])
            le = r[:, 4:36].rearrange("p (g x) -> p g x", x=8)
            S.op('dve', lambda e: e.tensor_tensor(out=le, in0=le, in1=r[:, 40:44].unsqueeze(2).to_broadcast([128, 4, 8]), op=ALU.add),
                 reads=[rk], writes=[rk])
            S.op('dve', lambda e: e.max(out=r[:, 48:56], in_=r[:, 4:36]), reads=[rk], writes=[rk])
            S.op('dve', lambda e: e.tensor_scalar(out=r[:, 39:40], in0=r[:, 48:49], scalar1=-1.0, scalar2=None, op0=ALU.mult),
                 reads=[rk], writes=[rk])
            S.op('act', lambda e: e.activation(out=r[:, 4:36], in_=r[:, 4:36], func=AF.Exp, bias=r[:, 39:40], scale=1.0),
                 reads=[rk], writes=[rk])
            S.op('act', lambda e: e.activation(out=r[:, 56:57], in_=r[:, 49:50], func=AF.Exp, bias=r[:, 39:40], scale=1.0),
                 reads=[rk], writes=[rk])
            S.op('dve', lambda e: e.tensor_scalar(out=r[:, 57:58], in0=r[:, 56:57], scalar1=1.0, scalar2=r[:, 38:39], op0=ALU.add, op1=ALU.mult),
                 reads=[rk], writes=[rk])
            S.op('dve', lambda e: e.reciprocal(out=r[:, 57:58], in_=r[:, 57:58]), reads=[rk], writes=[rk])
            S.op('dve', lambda e: e.scalar_tensor_tensor(out=r[:, 4:36], in0=r[:, 4:36], scalar=r[:, 56:57], in1=r[:, 4:36],
                                                         op0=ALU.is_ge, op1=ALU.mult), reads=[rk], writes=[rk])
            S.op('dve', lambda e: e.tensor_scalar(out=r[:, 4:36], in0=r[:, 4:36], scalar1=r[:, 57:58], scalar2=None, op0=ALU.mult),
                 reads=[rk], writes=[rk])
            S.op('pe', lambda e: e.transpose(out=pgt[b][:, :], in_=r[:, 4:36], identity=ident_f[:, :]),
                 reads=[rk, 'ident_f'], writes=[('pgt', b)])
            S.op('act', lambda e: e.copy(out=GT[:, col0:col0 + 128], in_=pgt[b][:, :]), reads=[('pgt', b)], writes=['GT'])
        S.barrier()
    dbg_dump('d_GT', GT[:], 'GT')
    if stop_after <= 5:
        S.barrier()
        return finish(nc, S, es, x_out, ctx_out)

    allhT2 = [k_ for i in range(18) for k_ in hk(('hT2', i))]
    with ExitStack() as pe_:
        wgb = [sb('wgb%d' % i, [128, 8, 256], BF, st=pe_) for i in range(2)]
        wub = [sb('wub%d' % i, [128, 8, 256], BF, st=pe_) for i in range(2)]
        wdb = [sb('wdb%d' % i, [128, 2, D], BF, st=pe_) for i in range(2)]
        wdc = [sb('wdc%d' % i, [128, 2, D], BF, st=pe_) for i in range(2)]
        stg = [sb('stg%d' % i, [128, 2048], st=pe_) for i in range(2)]
        Gs = [sb('Gs%d' % i, [128, 512], st=pe_) for i in range(2)]
        st_ = [sb('st_s%d' % i, [128, 512], st=pe_) for i in range(2)]
        tt_ = [sb('tt_s%d' % i, [128, 512], st=pe_) for i in range(2)]
        aT = [sb('aT%d' % i, [128, 2, 512], BF, st=pe_) for i in range(2)]
        pg = [ps('pg%d' % i, [128, 512], st=pe_) for i in range(2)]
        pu = [ps('pu%d' % i, [128, 512], st=pe_) for i in range(2)]
        pgb = ps('pgb', [128, 512], st=pe_)
        pyd = [ps('pyd%d' % i, [128, 512], st=pe_) for i in range(3)]
        si = 0; YD = [0]; cnt2 = 0; pend_dn = []
        tblocks = [(i * 512, 512, 'own') for i in range(4)] + [(NTOK, NCTX, 'ctx')]
        for e_ in range(NEXP):
            for fh in range(2):
                wb = (e_ * 2 + fh) % 2
                for which in range(3):
                    s_ = si % 2; si += 1
                    if which < 2:
                        srcw = (I['w_eg'] if which == 0 else I['w_eu'])[e_].rearrange("(k p) f -> p k f", p=128)[:, :, fh * 256:(fh + 1) * 256]
                        S.dma('sp', stg[s_][:, :].rearrange("p (k f) -> p k f", f=256), srcw, writes=[('stg', s_)])
                        dst = (wgb if which == 0 else wub)[wb]
                        S.op('pool' if which == 0 else 'act',
                             (lambda e: e.tensor_copy(out=dst[:], in_=stg[s_][:, :].rearrange("p (k f) -> p k f", f=256))) if which == 0 else
                             (lambda e: e.copy(out=dst[:], in_=stg[s_][:, :].rearrange("p (k f) -> p k f", f=256))),
                             reads=[('stg', s_)], writes=[('wg' if which == 0 else 'wu', wb)])
                    else:
                        srcw = I['w_ed'][e_].rearrange("(c p) n -> p c n", p=128)[:, fh * 2:fh * 2 + 2, :]
                        S.dma('sp', stg[s_][:, :].rearrange("p (c n) -> p c n", n=D), srcw, writes=[('stg', s_)])
                        sv = stg[s_][:, :].rearrange("p (c n) -> p c n", n=D)
                        S.op('pool', lambda e: e.tensor_tensor(out=wdb[wb][:], in0=sv, in1=gbc[:, 2:3, :].to_broadcast([128, 2, D]), op=ALU.mult),
                             reads=[('stg', s_), 'gbc'], writes=[('wd', wb)])
                        S.op('dve', lambda e: e.tensor_tensor(out=wdc[wb][:], in0=sv, in1=gbc[:, 3:4, :].to_broadcast([128, 2, D]), op=ALU.mult),
                             reads=[('stg', s_), 'gbc'], writes=[('wdc', wb)])
                for (c0, n, kind) in tblocks:
                    gb = cnt2 % 2; cnt2 += 1
                    S.op('pe', lambda e: e.matmul(out=pgb[:, 0:n], lhsT=ident_f[0:32, e_:e_ + 1].to_broadcast([32, 128]), rhs=GT[:, c0:c0 + n], start=True, stop=True),
                         reads=['ident_f', 'GT'], writes=['pgb'])
                    S.op('act', lambda e: e.copy(out=Gs[gb][:, 0:n], in_=pgb[:, 0:n]), reads=['pgb'], writes=[('Gs', gb)])
                    for fc in range(2):
                        for kk in range(8):
                            S.op('pe', lambda e: e.matmul(out=pg[fc][:, 0:n], lhsT=wgb[wb][:, kk, fc * 128:(fc + 1) * 128], rhs=hT[:, kk, c0:c0 + n],
                                                          start=(kk == 0), stop=(kk == 7)), reads=allhT2 + [('wg', wb)], writes=[('pg', fc)], inc=(kk == 7))
                        for kk in range(8):
                            S.op('pe', lambda e: e.matmul(out=pu[fc][:, 0:n], lhsT=wub[wb][:, kk, fc * 128:(fc + 1) * 128], rhs=hT[:, kk, c0:c0 + n],
                                                          start=(kk == 0), stop=(kk == 7)), reads=allhT2 + [('wu', wb)], writes=[('pu', fc)], inc=(kk == 7))
                        S.op('act', lambda e: e.activation(out=st_[fc][:, 0:n], in_=pg[fc][:, 0:n], func=AF.Silu), reads=[('pg', fc)], writes=[('st', fc)])
                        S.op('dve', lambda e: e.tensor_tensor(out=tt_[fc][:, 0:n], in0=pu[fc][:, 0:n], in1=Gs[gb][:, 0:n], op=ALU.mult),
                             reads=[('pu', fc), ('Gs', gb)], writes=[('tt', fc)])
                        S.op('pool', lambda e: e.tensor_tensor(out=aT[gb][:, fc, 0:n], in0=st_[fc][:, 0:n], in1=tt_[fc][:, 0:n], op=ALU.mult),
                             reads=[('st', fc), ('tt', fc)], writes=[('aT', gb)])
                    def down(gb=gb, wb=wb, kind=kind, c0=c0, n=n):
                        wdsel = wdc if kind == 'ctx' else wdb
                        for tt in range(n // 128):
                            tI = (c0 + tt * 128) // 128
                            for nh in range(2):
                                y_ = YD[0] % 3; YD[0] += 1
                                for fc in range(2):
                                    S.op('pe', lambda e: e.matmul(out=pyd[y_][:, :], lhsT=aT[gb][:, fc, tt * 128:(tt + 1) * 128],
                                                                  rhs=wdsel[wb][:, fc, nh * 512:(nh + 1) * 512], start=(fc == 0), stop=(fc == 1)),
                                         reads=[('aT', gb), ('wdc' if kind == 'ctx' else 'wd', wb)], writes=[('pyd', y_)], inc=(fc == 1))
                                xs = x_res[:, tI, nh * 512:(nh + 1) * 512]
                                S.op('dve', lambda e: e.tensor_tensor(out=xs, in0=pyd[y_][:, :], in1=xs, op=ALU.add),
                                     reads=[('pyd', y_), ('xres', tI)], writes=[('xres', tI)])
                    if pend_dn:
                        pend_dn.pop(0)()
                    pend_dn.append(down)
        while pend_dn:
            pend_dn.pop(0)()
        S.barrier()

    with ExitStack() as pf_:
        lnbc = sb('lnbc2', [128, 2, D], st=pf_)
        LN_['t'] = lnbc
        S.dma('sp', lnbc[:, 0, :], I['lnv'][:, 2 * D:3 * D].to_broadcast([128, D]), writes=['lnbc'])
        S.dma('sp', lnbc[:, 1, :], I['lnv'][:, 3 * D:4 * D].to_broadcast([128, D]), writes=['lnbc'])
        pre = [sb('pree%d' % i, [128, D], st=pf_) for i in range(2)]
        for tI, (kind, t, col0) in enumerate(tilesC):
            b = tI % 2
            S.op('act', lambda e: e.copy(out=pre[b][:, :], in_=x_res[:, tI, :]), reads=[('xres', tI)], writes=[('pree', b)])
            post_ln(tI, pre[b], ('pree', b))
            dst = x_out[t * 128:(t + 1) * 128, :] if kind == 'own' else ctx_out[t * 128:(t + 1) * 128, :]
            S.dma('sp', dst, x_res[:, tI, :], reads=[('xres', tI)], writes=[('out', tI)])
        S.barrier()
    return finish(nc, S, es, x_out, ctx_out)


def finish(nc, S, es, x_out, ctx_out):
    S.barrier()
    es.close()
    return nc


def _rope_cs(n_tok):
    t = np.arange(n_tok)
    row = (t // GRID_W).astype(np.float32); col = (t % GRID_W).astype(np.float32)
    inv = (10000.0 ** (-np.arange(16, dtype=np.float32) / 16)).astype(np.float32)
    ang = np.stack([row[:, None] * inv, col[:, None] * inv], axis=1).astype(np.float32)
    return np.concatenate([np.cos(ang).reshape(n_tok, 32), np.sin(ang).reshape(n_tok, 32)], axis=1).astype(np.float32)


def _invcnt(n, t0, L):
    t = np.arange(t0, t0 + L)
    out = np.zeros((128, 2 * L), np.float32)
    for ch, (wa, wb) in enumerate(((2, 4), (8, 16))):
        for h, w in enumerate((wa, wb)):
            lo = np.clip(t - w // 2, 0, n); hi = np.clip(t + (w - w // 2), 0, n)
            out[h * 64:(h + 1) * 64, ch * L:(ch + 1) * L] = (1.0 / (hi - lo).astype(np.float32))[None, :]
    return out


_NC_CACHE = {}


def _layer_inputs(l, x, ctx_x, P):
    f = np.float32
    common = {
        'x_all': np.ascontiguousarray(x, f), 'ctx': np.ascontiguousarray(ctx_x, f),
        'cT': np.ascontiguousarray(np.stack([P['c'].reshape(8, 128).T, P['c_ctx'].reshape(8, 128).T], axis=2).reshape(128, 16), f),
        'w_mod': np.ascontiguousarray(P['w_mod'][l], f), 'b_mod': np.ascontiguousarray(P['b_mod'][l][None, :], f),
        'w_in': np.ascontiguousarray(P['w_in'][l], f),
        'qk_gain': np.ascontiguousarray(np.concatenate([P['q_gain'][l], P['k_gain'][l]])[None, :], f),
        'w_pool': np.ascontiguousarray(P['w_pool'][l].reshape(256, 64), f),
        'cvp': np.ascontiguousarray(np.stack([P['b_dw'][l].reshape(2, 128).T, P['cv_ln_g'][l].reshape(2, 128).T,
                                              P['cv_ln_b'][l].reshape(2, 128).T, P['pool_scale'][l].reshape(2, 128).T],
                                             axis=2).reshape(128, 8), f),
        'w_dwT': np.ascontiguousarray(P['w_dw'][l].reshape(31, 2, 128).transpose(2, 1, 0).reshape(128, 62), f),
        'w_pw': np.ascontiguousarray(P['w_cv_pw'][l], f), 'w_out': np.ascontiguousarray(P['w_out'][l], f),
        'lnv': np.ascontiguousarray(np.concatenate([P['ln1_g'][l], P['ln1_b'][l], P['ln2_g'][l], P['ln2_b'][l]])[None, :], f),
        'w_r': np.ascontiguousarray(np.concatenate([P['w_rg'][l], P['w_re'][l]], axis=1), f),
        'b_r': np.ascontiguousarray(np.concatenate([P['b_rg'][l], P['b_re'][l]])[None, :], f),
        'w_eg': np.ascontiguousarray(P['w_e_gate'][l], f), 'w_eu': np.ascontiguousarray(P['w_e_up'][l], f),
        'w_ed': np.ascontiguousarray(P['w_e_down'][l], f),
        'cs_all': _rope_cs(SEQ),
        'sel': np.ascontiguousarray(np.repeat(np.eye(32, dtype=f), 128, axis=1)),
        'selrow': np.ascontiguousarray(np.concatenate([np.repeat(np.eye(64, dtype=f)[:, 0:1], 128, 1),
                                                       np.repeat(np.eye(64, dtype=f)[:, 32:33], 128, 1)], axis=1)),
        'ident_f': np.eye(128, dtype=f), 'ident_b': np.eye(128, dtype=f).astype(ml_dtypes.bfloat16),
    }
    ic_ctx = _invcnt(NCTX, 0, NCTX)
    maps = []
    for r in range(NCORE):
        m = dict(common)
        t0 = r * NTOK
        m['x_own'] = np.ascontiguousarray(x[t0:t0 + NTOK], f)
        xh = np.zeros((32, D), f); hm = np.zeros((128, 32), f)
        if r > 0:
            xh[0:16] = x[t0 - 16:t0]; hm[:, 0:16] = 1.0
        if r < NCORE - 1:
            xh[16:32] = x[t0 + NTOK:t0 + NTOK + 16]; hm[:, 16:32] = 1.0
        m['x_halo'] = xh; m['hmask'] = hm
        m['cs_own'] = np.ascontiguousarray(common['cs_all'][t0:t0 + NTOK])
        m['invcnt'] = np.ascontiguousarray(np.concatenate([_invcnt(SEQ, t0, NTOK), ic_ctx], axis=1))
        maps.append(m)
    return maps


def kernel(**inputs):
    P = {k: np.asarray(v) for k, v in inputs.items()}
    x = P['x'][0]
    ctx_x = P['ctx'][0]
    P['c'] = P['c'].reshape(-1)
    if 'nc' not in _NC_CACHE:
        _NC_CACHE['nc'] = build()
    nc = _NC_CACHE['nc']
    for l in range(DEPTH):
        maps = _layer_inputs(l, x, ctx_x, P)
        res = run_bass_kernel_spmd(nc, maps, core_ids=list(range(NCORE)))
        x = np.concatenate([np.asarray(res.results[r]['x_out']) for r in range(NCORE)], axis=0)
        ctx_x = np.asarray(res.results[0]['ctx_out'])
    return x[None].astype(np.float32)
```

```python
import numpy as np
import ml_dtypes
from contextlib import ExitStack
import concourse.bass as bass
import concourse.mybir as mybir
from concourse.bass_utils import run_bass_kernel_spmd

F32 = mybir.dt.float32
BF = mybir.dt.bfloat16
ALU = mybir.AluOpType
AF = mybir.ActivationFunctionType
AX = mybir.AxisListType

D = 1024
SEQ = 16384
NCORE = 8
NTOK = SEQ // NCORE
NTI = NTOK // 128
NCTX = 256
NKC = (SEQ + NCTX) // 128
KEYS = SEQ + NCTX
HTW = NTOK + NCTX + 32
HAL0 = NTOK + NCTX
NEXP = 32
DEPTH = 2
ALPHA = (2 * DEPTH) ** 0.25
EPS = 1e-6
GRID_W = 64


class Sched:
    def __init__(self, nc, es):
        self.nc = nc
        self.E = {'pe': nc.tensor, 'act': nc.scalar, 'dve': nc.vector, 'pool': nc.gpsimd, 'sp': nc.sync}
        self.sem = {k: es.enter_context(nc.semaphore('c_' + k)) for k in self.E}
        self.cnt = {k: 0 for k in self.E}
        self.seen = {k: {} for k in self.E}
        self.W = {}
        self.R = {}
        self.ND = 32
        self.dsem = [es.enter_context(nc.semaphore('d%d' % i)) for i in range(self.ND)]
        self.dval = [0] * self.ND
        self.di = 0
        self.nwait = 0

    def _wait(self, eng, dep):
        sem, val, key = dep
        if self.seen[eng].get(key, 0) >= val:
            return
        self.seen[eng][key] = val
        self.E[eng].wait_ge(sem, val)
        self.nwait += 1

    def _deps(self, eng, reads, writes):
        for k in reads:
            d = self.W.get(k)
            if d is not None and not (eng == 'pe' and d[2] == 'pe'):
                self._wait(eng, d)
        for k in writes:
            d = self.W.get(k)
            if d is not None and not (eng == 'pe' and d[2] == 'pe'):
                self._wait(eng, d)
            for d in self.R.get(k, {}).values():
                if not (eng == 'pe' and d[2] == 'pe'):
                    self._wait(eng, d)

    def _reg(self, dep, reads, writes):
        for k in writes:
            self.W[k] = dep
            self.R[k] = {}
        for k in reads:
            self.R.setdefault(k, {})[dep[2]] = dep

    def op(self, eng, fn, reads=(), writes=(), inc=True):
        self._deps(eng, reads, writes)
        ins = fn(self.E[eng])
        if inc:
            self.cnt[eng] += 1
            ins.then_inc(self.sem[eng], 1)
            dep = (self.sem[eng], self.cnt[eng], eng)
        else:
            dep = (self.sem[eng], self.cnt[eng] + 1, eng)
        self._reg(dep, reads, writes)

    def dma(self, q, out, in_, reads=(), writes=()):
        i = self.di
        self.di = (self.di + 1) % self.ND
        if self.dval[i] > 0:
            self._wait(q, (self.dsem[i], self.dval[i], ('d', i)))
        self._deps(q, reads, writes)
        self.dval[i] += 16
        self.E[q].dma_start(out=out, in_=in_).then_inc(self.dsem[i], 16)
        dep = (self.dsem[i], self.dval[i], ('d', i))
        self._reg(dep, reads, writes)

    def barrier(self, engs=None):
        engs = engs or list(self.E)
        for e in engs:
            for f in self.E:
                if f != e and self.cnt[f] > 0:
                    self._wait(e, (self.sem[f], self.cnt[f], f))
            for i in range(self.ND):
                if self.dval[i] > 0:
                    self._wait(e, (self.dsem[i], self.dval[i], ('d', i)))


INPUTS = [
    ('x_all', [SEQ, D], F32), ('x_own', [NTOK, D], F32), ('x_halo', [32, D], F32), ('ctx', [NCTX, D], F32),
    ('cT', [128, 16], F32), ('w_mod', [D, 6 * D], F32), ('b_mod', [1, 6 * D], F32), ('w_in', [D, 1536], F32),
    ('qk_gain', [1, 128], F32), ('w_pool', [256, 64], F32), ('cvp', [128, 8], F32), ('w_dwT', [128, 62], F32),
    ('w_pw', [256, 256], F32), ('w_out', [D, D], F32), ('lnv', [1, 4 * D], F32), ('w_r', [D, 36], F32),
    ('b_r', [1, 36], F32), ('w_eg', [NEXP, D, 512], F32), ('w_eu', [NEXP, D, 512], F32),
    ('w_ed', [NEXP, 512, D], F32), ('cs_all', [SEQ, 64], F32), ('cs_own', [NTOK, 64], F32),
    ('invcnt', [128, 2 * (NTOK + NCTX)], F32), ('hmask', [128, 32], F32), ('sel', [32, NEXP * 128], F32),
    ('selrow', [64, 256], F32), ('ident_f', [128, 128], F32), ('ident_b', [128, 128], BF),
]


def build(stop_after=99, dbg=False):
    nc = bass.Bass("TRN2", target_bir_lowering=False)
    I = {n: nc.dram_tensor(n, list(s), dt, kind="ExternalInput").ap() for n, s, dt in INPUTS}
    x_out = nc.dram_tensor('x_out', [NTOK, D], F32, kind="ExternalOutput").ap()
    ctx_out = nc.dram_tensor('ctx_out', [NCTX, D], F32, kind="ExternalOutput").ap()
    DBG = {}
    if dbg:
        for n, s, dt in [('d_modT', [128, 64], F32), ('d_gbc', [128, 4096], F32), ('d_KT', [128, KEYS], BF),
                         ('d_V', [128, NKC * 192], BF), ('d_QT', [128, 4, NTOK + NCTX], BF),
                         ('d_yT', [128, 4, NTOK + NCTX], BF), ('d_hT', [128, 8 * HTW], BF),
                         ('d_xres', [128, 18 * D], F32), ('d_GT', [32, NTOK + NCTX], F32)]:
            DBG[n] = nc.dram_tensor(n, list(s), dt, kind="ExternalOutput").ap()

    es = ExitStack()
    S = Sched(nc, es)

    def sb(name, shape, dt=F32, st=None):
        return (st or es).enter_context(nc.sbuf_tensor('s_' + name, list(shape), dt))

    def ps(name, shape, dt=F32, st=None):
        return (st or es).enter_context(nc.psum_tensor('p_' + name, list(shape), dt))

    def dbg_dump(name, ap, key):
        if dbg:
            S.barrier()
            S.dma('sp', DBG[name], ap, reads=[key], writes=[('dbgout', name)])
            S.barrier()

    ident_f = sb('ident_f', [128, 128]); ident_b = sb('ident_b', [128, 128], BF)
    ones_f = sb('ones_f', [128, 128]); epst = sb('epst', [128, 1])
    modT = sb('modT', [128, 64])
    gbc = sb('gbc', [128, 4, D])
    qkg = sb('qkg', [128, 128]); cvp = sb('cvp', [128, 8]); wdwT = sb('wdwT', [128, 62])
    negm = sb('negm', [128, 1]); hmask = sb('hmask', [128, 32])
    QY = sb('QY', [128, 8, NTOK + NCTX], BF)
    QT = QY[:, 0:4, :]
    yT = QY[:, 4:8, :]
    sm = sb('sm', [128, 8, 64])
    smi = [0]

    def smslot():
        smi[0] = (smi[0] + 1) % 8
        return smi[0]

    S.dma('sp', ident_f[:], I['ident_f'], writes=['ident_f'])
    S.dma('sp', ident_b[:], I['ident_b'], writes=['ident_b'])
    S.dma('sp', qkg[:], I['qk_gain'].to_broadcast([128, 128]), writes=['qkg'])
    S.dma('sp', cvp[:], I['cvp'], writes=['cvp'])
    S.dma('sp', wdwT[:], I['w_dwT'], writes=['wdwT'])
    S.dma('sp', hmask[:], I['hmask'], writes=['hmask'])
    S.op('dve', lambda e: e.memset(ones_f[:], 1.0), writes=['ones_f'])
    S.op('dve', lambda e: e.memset(epst[:], EPS), writes=['epst'])
    sl = smslot()
    S.op('dve', lambda e: e.tensor_tensor(out=sm[:, sl, 0:64], in0=qkg[:, 0:64], in1=qkg[:, 64:128], op=ALU.mult),
         reads=['qkg'], writes=[('sm', sl)])
    S.op('dve', lambda e: e.tensor_reduce(out=negm[:], in_=sm[:, sl, 0:64], axis=AX.X, op=ALU.max,
                                          apply_absolute_value=True), reads=[('sm', sl)], writes=['negm'])
    S.op('dve', lambda e: e.tensor_scalar(out=negm[:], in0=negm[:], scalar1=-8.0, scalar2=None, op0=ALU.mult),
         reads=['negm'], writes=['negm'])

    def ln_stats(src, srckey):
        return ln_stats_b(ln_stats_a(src, srckey))

    def ln_stats_a(src, srckey):
        s = smslot()
        k = ('sm', s)
        S.op('dve', lambda e: e.bn_stats(out=sm[:, s, 0:6], in_=src[:, 0:512]), reads=[srckey], writes=[k])
        S.op('dve', lambda e: e.bn_stats(out=sm[:, s, 6:12], in_=src[:, 512:1024]), reads=[srckey, k], writes=[k])
        S.op('dve', lambda e: e.bn_aggr(out=sm[:, s, 12:14], in_=sm[:, s, 0:12]), reads=[k], writes=[k])
        S.op('act', lambda e: e.activation(out=sm[:, s, 14:15], in_=sm[:, s, 13:14], func=AF.Sqrt,
                                           bias=epst[:, 0:1], scale=1.0), reads=[k, 'epst'], writes=[k])
        return s

    def ln_stats_b(s):
        k = ('sm', s)
        S.op('dve', lambda e: e.reciprocal(out=sm[:, s, 14:15], in_=sm[:, s, 14:15]), reads=[k], writes=[k])
        S.op('dve', lambda e: e.tensor_scalar(out=sm[:, s, 15:16], in0=sm[:, s, 12:13], scalar1=sm[:, s, 14:15],
                                              scalar2=-1.0, op0=ALU.mult, op1=ALU.mult), reads=[k], writes=[k])
        return sm[:, s, 14:15], sm[:, s, 15:16], k

    with ExitStack() as p0:
        cc = sb('cc', [128, 8, 64], st=p0); cTt = sb('cTt', [128, 16], st=p0)
        bmod = sb('bmod', [1, 6 * D], st=p0)
        wst = [sb('wst%d' % i, [128, 8, 512], st=p0) for i in range(2)]
        mrow = [sb('mrow%d' % i, [64, 512], st=p0) for i in range(2)]
        selrow = sb('selrow', [64, 256], st=p0)
        pm = [ps('pm%d' % i, [128, 512], st=p0) for i in range(2)]
        pt = [ps('ptm%d' % i, [128, 512], st=p0) for i in range(2)]
        S.dma('sp', cTt[:], I['cT'], writes=['cTt'])
        S.dma('sp', bmod[:], I['b_mod'], writes=['bmod'])
        S.dma('sp', selrow[:], I['selrow'], writes=['selrow'])
        S.op('dve', lambda e: e.memset(cc[:], 0.0), writes=['cc'])
        cTv = cTt[:].rearrange("p (k w) -> p k w", w=2)
        for w in range(2):
            S.op('act', lambda e: e.activation(out=cc[:, :, 32 * w:32 * w + 1], in_=cTv[:, :, w:w + 1], func=AF.Silu),
                 reads=['cTt', 'cc'], writes=['cc'])
        wmv = I['w_mod'].rearrange("(k p) n -> p k n", p=128)
        for nb in range(12):
            b = nb % 2
            S.dma('sp', wst[b][:], wmv[:, :, nb * 512:(nb + 1) * 512], writes=[('wst', b)])
            for k in range(8):
                S.op('pe', lambda e: e.matmul(out=pm[b][0:64, :], lhsT=cc[:, k, :], rhs=wst[b][:, k, :],
                                              start=(k == 0), stop=False),
                     reads=['cc', ('wst', b)], writes=[('pm', b)], inc=False)
            S.op('pe', lambda e: e.matmul(out=pm[b][0:64, :], lhsT=ones_f[0:1, 0:64],
                                          rhs=bmod[0:1, nb * 512:(nb + 1) * 512], start=False, stop=True),
                 reads=['ones_f', 'bmod'], writes=[('pm', b)])
            S.op('act', lambda e: e.copy(out=mrow[b][:], in_=pm[b][0:64, :]), reads=[('pm', b)], writes=[('mrow', b)])
            kind6 = nb // 2
            if kind6 in (2, 5):
                for w in range(2):
                    S.op('pe', lambda e: e.matmul(out=pt[w][:, :], lhsT=selrow[:, w * 128:(w + 1) * 128],
                                                  rhs=mrow[b][:], start=True, stop=True),
                         reads=['selrow', ('mrow', b)], writes=[('pt', w)])
                    gi = (0 if kind6 == 2 else 2) + w
                    S.op('act', lambda e: e.copy(out=gbc[:, gi, (nb % 2) * 512:(nb % 2) * 512 + 512], in_=pt[w][:, :]),
                         reads=[('pt', w)], writes=['gbc'])
            else:
                kind = {0: 0, 1: 1, 3: 2, 4: 3}[kind6]
                for j in range(4):
                    k = (nb % 2) * 4 + j
                    S.op('pe', lambda e: e.transpose(out=pt[0][:, j * 64:(j + 1) * 64],
                                                     in_=mrow[b][:, j * 128:(j + 1) * 128], identity=ident_f[0:64, 0:64]),
                         reads=[('mrow', b), 'ident_f'], writes=[('pt', 0)], inc=(j == 3))
                ptv = pt[0][:, 0:256].rearrange("p (j c) -> p j c", c=64)
                mv = modT[:, kind * 16 + (nb % 2) * 8: kind * 16 + (nb % 2) * 8 + 8].rearrange("p (j w) -> p j w", w=2)
                for w in range(2):
                    S.op('dve', lambda e: e.tensor_scalar(out=mv[:, :, w:w + 1], in0=ptv[:, :, 32 * w:32 * w + 1],
                                                          scalar1=(1.0 if kind in (1, 3) else 0.0), scalar2=None,
                                                          op0=ALU.add), reads=[('pt', 0)], writes=['modT'])
        dbg_dump('d_modT', modT[:], 'modT')
        dbg_dump('d_gbc', gbc[:].rearrange("p a b -> p (a b)"), 'gbc')
        S.barrier()
    if stop_after <= 0:
        return finish(nc, S, es, x_out, ctx_out)

    def SC(kind, k, w):
        i = kind * 16 + k * 2 + w
        return modT[:, i:i + 1]

    def ln_mod_T(src, srckey, ntok, kinds, w, dst_ap_fn, dstkey, xnb, xnbkey, ptr, ptrkey):
        ln_mod_a(src, srckey, ntok, xnb, xnbkey)
        ln_mod_b(ntok, kinds, w, dst_ap_fn, dstkey, xnb, xnbkey, ptr, ptrkey)

    def ln_mod_a(src, srckey, ntok, xnb, xnbkey, s_=None):
        rstd, nmr, k = ln_stats(src, srckey) if s_ is None else ln_stats_b(s_)
        S.op('act', lambda e: e.activation(out=xnb[0:ntok, :], in_=src[0:ntok, :], func=AF.Identity, bias=nmr[0:ntok],
                                           scale=rstd[0:ntok]), reads=[srckey, k], writes=[xnbkey])

    def ln_mod_b(ntok, kinds, w, dst_ap_fn, dstkey, xnb, xnbkey, ptr, ptrkey):
        for kk in range(8):
            S.op('pe', lambda e: e.transpose(out=ptr[:, kk, 0:ntok], in_=xnb[0:ntok, kk * 128:(kk + 1) * 128],
                                             identity=ident_b[0:ntok, 0:ntok]),
                 reads=[xnbkey, 'ident_b'], writes=[ptrkey], inc=(kk == 7))
        for kk in range(8):
            if kk % 2 == 0:
                S.op('dve', lambda e: e.tensor_scalar(out=dst_ap_fn(kk), in0=ptr[:, kk, 0:ntok],
                                                      scalar1=SC(kinds[1], kk, w), scalar2=SC(kinds[0], kk, w),
                                                      op0=ALU.mult, op1=ALU.add),
                     reads=[ptrkey, 'modT'], writes=[dstkey])
            else:
                S.op('act', lambda e: e.activation(out=dst_ap_fn(kk), in_=ptr[:, kk, 0:ntok], func=AF.Identity,
                                                   bias=SC(kinds[0], kk, w), scale=SC(kinds[1], kk, w)),
                     reads=[ptrkey, 'modT'], writes=[dstkey])

    def hk(key):
        return [key]

    def rms_rope(src, srckey, nh, gain_ap, cs, cskey, dst, dstkey, tmp, tmpkey, rope, part=None, st=None):
        if part != 'b':
            s = smslot()
        else:
            s = st
        k = ('sm', s)
        W_ = nh * 64
        if part == 'b':
            src = tmp[:, 2 * W_:3 * W_]
            srckey = tmpkey
        else:
            _rms_a(src, srckey, nh, tmp, tmpkey, s)
            src = tmp[:, 2 * W_:3 * W_]
            srckey = tmpkey
            if part == 'a':
                return s
        _rms_b(src, srckey, nh, gain_ap, cs, cskey, dst, dstkey, tmp, tmpkey, rope, s)

    def _rms_a(src, srckey, nh, tmp, tmpkey, s):
        k = ('sm', s)
        W_ = nh * 64
        src_ps = src
        src = tmp[:, 2 * W_:3 * W_]
        S.op('act', lambda e: e.copy(out=src, in_=src_ps), reads=[srckey], writes=[tmpkey])
        srckey = tmpkey
        S.op('dve', lambda e: e.tensor_tensor(out=tmp[:, 0:W_], in0=src, in1=src, op=ALU.mult),
             reads=[srckey], writes=[tmpkey])
        S.op('dve', lambda e: e.tensor_reduce(out=sm[:, s, 0:nh], in_=tmp[:, 0:W_].rearrange("p (h d) -> p h d", d=64),
                                              axis=AX.X, op=ALU.add), reads=[tmpkey], writes=[k])
        S.op('act', lambda e: e.activation(out=sm[:, s, 0:nh], in_=sm[:, s, 0:nh], func=AF.Sqrt, bias=epst[:, 0:1],
                                           scale=1.0 / 64), reads=[k, 'epst'], writes=[k])

    def _rms_b(src, srckey, nh, gain_ap, cs, cskey, dst, dstkey, tmp, tmpkey, rope, s):
        k = ('sm', s)
        W_ = nh * 64
        S.op('dve', lambda e: e.reciprocal(out=sm[:, s, 0:nh], in_=sm[:, s, 0:nh]), reads=[k], writes=[k])
        t3 = tmp[:, 0:W_].rearrange("p (h d) -> p h d", d=64)
        S.op('dve', lambda e: e.tensor_tensor(out=t3, in0=src.rearrange("p (h d) -> p h d", d=64),
                                              in1=sm[:, s, 0:nh].unsqueeze(2).to_broadcast([128, nh, 64]), op=ALU.mult),
             reads=[srckey, k], writes=[tmpkey])
        gdst = t3 if rope else dst.rearrange("p (h d) -> p h d", d=64)
        S.op('dve', lambda e: e.tensor_tensor(out=gdst, in0=t3, in1=gain_ap.unsqueeze(1).to_broadcast([128, nh, 64]),
                                              op=ALU.mult), reads=[tmpkey, 'qkg'], writes=[tmpkey if rope else dstkey])
        if not rope:
            return
        t5 = tmp[:, 0:W_].rearrange("p (h a b f) -> p h a b f", a=2, b=2, f=16)
        d5 = dst.rearrange("p (h a b f) -> p h a b f", a=2, b=2, f=16)
        u5 = tmp[:, W_:2 * W_].rearrange("p (h a b f) -> p h a b f", a=2, b=2, f=16)
        cosb = cs[:, 0:32].rearrange("p (a f) -> p a f", f=16).unsqueeze(1).to_broadcast([128, nh, 2, 16])
        sinb = cs[:, 32:64].rearrange("p (a f) -> p a f", f=16).unsqueeze(1).to_broadcast([128, nh, 2, 16])
        t1, t2 = t5[:, :, :, 0, :], t5[:, :, :, 1, :]
        ua, ub = u5[:, :, :, 0, :], u5[:, :, :, 1, :]
        rk = [tmpkey, cskey]
        S.op('dve', lambda e: e.tensor_tensor(out=ua, in0=t1, in1=cosb, op=ALU.mult), reads=rk, writes=[tmpkey])
        S.op('dve', lambda e: e.tensor_tensor(out=ub, in0=t2, in1=sinb, op=ALU.mult), reads=rk, writes=[tmpkey])
        S.op('dve', lambda e: e.tensor_tensor(out=d5[:, :, :, 0, :], in0=ua, in1=ub, op=ALU.subtract),
             reads=[tmpkey], writes=[dstkey])
        S.op('dve', lambda e: e.tensor_tensor(out=ua, in0=t2, in1=cosb, op=ALU.mult), reads=rk, writes=[tmpkey])
        S.op('dve', lambda e: e.tensor_tensor(out=ub, in0=t1, in1=sinb, op=ALU.mult), reads=rk, writes=[tmpkey])
        S.op('dve', lambda e: e.tensor_tensor(out=d5[:, :, :, 1, :], in0=ua, in1=ub, op=ALU.add),
             reads=[tmpkey], writes=[dstkey])

    LP = 16 + NTOK + 16
    LC = 16 + NCTX + 16
    with ExitStack() as pa:
        hT = sb('hT', [128, 8, HTW], BF, st=pa)
        uT = sb('uT', [128, 2, LP], st=pa); cuT = sb('cuT', [128, 2, LP], st=pa)
        uTc = sb('uTc', [128, 2, LC], st=pa); cuTc = sb('cuTc', [128, 2, LC], st=pa)
        with ExitStack() as pa1:
            winb = sb('winb', [128, 8, 1536], BF, st=pa1)
            xt = [sb('xt%d' % i, [128, D], st=pa1) for i in range(3)]
            xnb = [sb('xnb%d' % i, [128, D], BF, st=pa1) for i in range(2)]
            wst = [sb('wsta%d' % i, [128, 8, 512], st=pa1) for i in range(1)]
            qtmp = [sb('qtmp%d' % i, [128, 1536], st=pa1) for i in range(2)]
            qrb = [sb('qrb%d' % i, [128, 512], BF, st=pa1) for i in range(2)]
            cst = [sb('cst%d' % i, [128, 64], st=pa1) for i in range(2)]
            sig = [sb('sig%d' % i, [128, 512], st=pa1) for i in range(2)]
            ptr = [ps('ptr%d' % i, [128, 8, 128], BF, st=pa1) for i in range(2)]
            pq = [ps('pq%d' % i, [128, 512], st=pa1) for i in range(2)]
            pqt = [ps('pqt%d' % i, [128, 4, 128], BF, st=pa1) for i in range(2)]
            pf = [ps('pf%d' % i, [128, 512], st=pa1) for i in range(2)]
            wiv = I['w_in'].rearrange("(k p) n -> p k n", p=128)
            for nb in range(3):
                b = 0
                S.dma('sp', wst[b][:], wiv[:, :, nb * 512:(nb + 1) * 512], writes=[('wsta', b)])
                S.op('pool' if nb == 1 else 'dve', lambda e: e.tensor_copy(out=winb[:, :, nb * 512:(nb + 1) * 512], in_=wst[b][:]),
                     reads=[('wsta', b)], writes=['winb'])
            tiles = [('own', t) for t in range(NTI)] + [('ctx', t) for t in range(2)] + [('halo', 0)]
            for ti, (kind, t) in enumerate(tiles):
                b3 = ti % 3; b = ti % 2
                ntok = 32 if kind == 'halo' else 128
                src = {'own': I['x_own'], 'ctx': I['ctx'], 'halo': I['x_halo']}[kind]
                col0 = {'own': t * 128, 'ctx': NTOK + t * 128, 'halo': HAL0}[kind]
                w = 1 if kind == 'ctx' else 0
                S.dma('sp', xt[b3][0:ntok, :], src[t * 128:t * 128 + ntok, :], writes=[('xt', b3)])
                ln_mod_T(xt[b3], ('xt', b3), ntok, (0, 1), w,
                         lambda kk: hT[:, kk, col0:col0 + ntok], ('hT', ti), xnb[b], ('xnb', b), ptr[b], ('ptr', b))
                if kind == 'halo':
                    continue
                for kk in range(8):
                    S.op('pe', lambda e: e.matmul(out=pq[b][:, :], lhsT=hT[:, kk, col0:col0 + 128], rhs=winb[:, kk, 0:512],
                                                  start=(kk == 0), stop=(kk == 7)),
                         reads=hk(('hT', ti)) + ['winb'], writes=[('pq', b)], inc=(kk == 7))
                if kind == 'own':
                    S.dma('sp', cst[b][:], I['cs_own'][t * 128:(t + 1) * 128, :], writes=[('cst', b)])
                rms_rope(pq[b][:, :], ('pq', b), 8, qkg[:, 0:64], cst[b], ('cst', b), qrb[b][:, :], ('qrb', b),
                         qtmp[b], ('qtmp', b), rope=(kind == 'own'))
                for h_ in range(8):
                    g_, j_ = h_ // 4, h_ % 4
                    S.op('pe', lambda e: e.transpose(out=pqt[b][g_ * 64:(g_ + 1) * 64, j_, :], in_=qrb[b][:, h_ * 64:(h_ + 1) * 64],
                                                     identity=ident_b[:, :]),
                         reads=[('qrb', b), 'ident_b'], writes=[('pqt', b)], inc=(h_ == 7))
                S.op('act', lambda e: e.copy(out=QT[:, :, col0:col0 + 128], in_=pqt[b][:, :, :]),
                     reads=[('pqt', b)], writes=[('QT', col0 // 512)])
            blocks = [(i * 512, 512, 'own') for i in range(4)] + [(NTOK, 256, 'ctx'), (HAL0, 32, 'halo')]
            S.barrier(['pe'])
            allhT = []
            pi = 0
            for (c0, n, kind) in blocks:
                def dsts(tn, ch):
                    if kind == 'own':
                        return tn[:, ch, 16 + c0:16 + c0 + n]
                    if kind == 'ctx':
                        return tn[:, ch, 16:16 + NCTX]
                    return tn[:, ch, :].rearrange("p (a b) -> p a b", a=2)[:, :, 0:16] if False else None
                for ch in range(2):
                    pu_, pa_, pg_ = None, None, None
                    res = {}
                    for which, cc0 in (('u', 768), ('g', 1280), ('a', 1024)):
                        b = pi % 2; pi += 1
                        for kk in range(8):
                            S.op('pe', lambda e: e.matmul(out=pf[b][:, 0:n], lhsT=winb[:, kk, cc0 + ch * 128:cc0 + ch * 128 + 128],
                                                          rhs=hT[:, kk, c0:c0 + n], start=(kk == 0), stop=(kk == 7)),
                                 reads=allhT + ['winb'], writes=[('pf', b)], inc=(kk == 7))
                        if kind == 'halo':
                            def hal(tn):
                                return [(tn[:, ch, 0:16], 0), (tn[:, ch, 16 + NTOK:32 + NTOK], 16)]
                        if which == 'u':
                            if kind == 'halo':
                                for (dap, o) in hal(uT):
                                    S.op('dve', lambda e: e.tensor_tensor(out=dap, in0=pf[b][:, o:o + 16], in1=hmask[:, o:o + 16],
                                                                          op=ALU.mult), reads=[('pf', b), 'hmask'], writes=['uT'])
                            else:
                                S.op('act', lambda e: e.copy(out=dsts(uT if kind == 'own' else uTc, ch), in_=pf[b][:, 0:n]),
                                     reads=[('pf', b)], writes=['uT'])
                        elif which == 'g':
                            sb_ = b
                            S.op('act', lambda e: e.activation(out=sig[sb_][:, 0:n], in_=pf[b][:, 0:n], func=AF.Sigmoid),
                                 reads=[('pf', b)], writes=[('sig', sb_)])
                        else:
                            if kind == 'halo':
                                S.op('dve', lambda e: e.tensor_tensor(out=sig[sb_][:, 0:32], in0=sig[sb_][:, 0:32], in1=hmask[:, :],
                                                                      op=ALU.mult), reads=[('sig', sb_), 'hmask'], writes=[('sig', sb_)])
                                for (dap, o) in hal(cuT):
                                    S.op('dve', lambda e: e.tensor_tensor(out=dap, in0=pf[b][:, o:o + 16], in1=sig[sb_][:, o:o + 16],
                                                                          op=ALU.mult), reads=[('pf', b), ('sig', sb_)], writes=['cuT'])
                            else:
                                S.op('dve', lambda e: e.tensor_tensor(out=dsts(cuT if kind == 'own' else cuTc, ch), in0=pf[b][:, 0:n],
                                                                      in1=sig[sb_][:, 0:n], op=ALU.mult),
                                     reads=[('pf', b), ('sig', sb_)], writes=['cuT'])
            for tn, kname in ((uTc, 'uT'), (cuTc, 'cuT')):
                S.op('pool', lambda e: e.memset(tn[:, :, 0:16], 0.0), writes=[kname])
                S.op('pool', lambda e: e.memset(tn[:, :, 16 + NCTX:32 + NCTX], 0.0), writes=[kname])
            S.barrier()
        dbg_dump('d_hT', hT[:].rearrange("p a b -> p (a b)"), ('hT', 0))

        with ExitStack() as pa2:
            pl = [sb('pl%d' % i, [128, LP], st=pa2) for i in range(2)]
            pooled = sb('pooled', [128, NTOK], BF, st=pa2)
            icn = sb('icn', [128, NTOK], st=pa2)
            wpbd = sb('wpbd', [128, 2, 128], BF, st=pa2); wpst = sb('wpst', [128, 2, 128], st=pa2)
            wpwb = sb('wpwb', [128, 2, 256], BF, st=pa2); wpws = sb('wpws', [128, 2, 256], st=pa2)
            acc = sb('acc', [128, 2, NTOK], st=pa2)
            sq = [sb('sq%d' % i, [128, 512], st=pa2) for i in range(2)]
            mean = sb('mean', [128, 512], st=pa2); rstd = sb('rstdc', [128, 512], st=pa2)
            zT = sb('zT', [128, 2, 512], BF, st=pa2)
            pp = [ps('pp%d' % i, [128, 512], st=pa2) for i in range(2)]
            ps1 = ps('ps1', [128, 512], st=pa2); ps2 = ps('ps2', [128, 512], st=pa2)
            pw = [ps('pw%d' % i, [128, 512], st=pa2) for i in range(2)]
            S.op('dve', lambda e: e.memset(wpst[:], 0.0), writes=['wpst'])
            for g in range(4):
                h = (g % 2) * 64
                S.dma('sp', wpst[h:h + 64, g // 2, h:h + 64], I['w_pool'][g * 64:(g + 1) * 64, :], reads=[], writes=['wpst'])
            S.op('dve', lambda e: e.tensor_copy(out=wpbd[:], in_=wpst[:]), reads=['wpst'], writes=['wpbd'])
            S.dma('sp', wpws[:], I['w_pw'].rearrange("(k p) n -> p k n", p=128), writes=['wpws'])
            S.op('dve', lambda e: e.tensor_copy(out=wpwb[:], in_=wpws[:]), reads=['wpws'], writes=['wpwb'])

            for (kind, U_, CU_, L, ycol0, ic0) in (('own', uT, cuT, NTOK, 0, 0), ('ctx', uTc, cuTc, NCTX, NTOK, 2 * NTOK)):
                LL = L + 32
                nblk = [(i * 512, 512) for i in range(L // 512)] if L >= 512 else [(0, L)]
                for ch in range(2):
                    U = U_[:, ch, :]
                    A, B = pl[0], pl[1]
                    S.dma('sp', icn[:, 0:L], I['invcnt'][:, ic0 + ch * L: ic0 + (ch + 1) * L], writes=['icn'])
                    S.op('dve', lambda e: e.tensor_tensor(out=A[:, 1:LL], in0=U[:, 0:LL - 1], in1=U[:, 1:LL], op=ALU.add),
                         reads=['uT'], writes=['plA'])
                    S.op('dve', lambda e: e.tensor_tensor(out=B[:, 2:LL - 1], in0=A[:, 1:LL - 2], in1=A[:, 3:LL], op=ALU.add),
                         reads=['plA'], writes=['plB'])
                    if ch == 0:
                        lo, hi = A, B
                    else:
                        S.op('dve', lambda e: e.tensor_tensor(out=A[:, 4:LL - 3], in0=B[:, 2:LL - 5], in1=B[:, 6:LL - 1], op=ALU.add),
                             reads=['plB'], writes=['plA'])
                        S.op('dve', lambda e: e.tensor_tensor(out=B[:, 8:LL - 7], in0=A[:, 4:LL - 11], in1=A[:, 12:LL - 3], op=ALU.add),
                             reads=['plA'], writes=['plB'])
                        lo, hi = A, B
                    for (h0, srcw, kk_) in ((0, lo, 'plA'), (64, hi, 'plB')):
                        S.op('dve', lambda e: e.tensor_tensor(out=srcw[h0:h0 + 64, 16:16 + L], in0=srcw[h0:h0 + 64, 16:16 + L],
                                                              in1=icn[h0:h0 + 64, 0:L], op=ALU.mult),
                             reads=[kk_, 'icn'], writes=[kk_])
                        S.op('dve', lambda e: e.tensor_tensor(out=pooled[h0:h0 + 64, 0:L], in0=srcw[h0:h0 + 64, 16:16 + L],
                                                              in1=U[h0:h0 + 64, 16:16 + L], op=ALU.subtract),
                             reads=[kk_, 'uT'], writes=['pooled'])
                    for bi, (c0, n) in enumerate(nblk):
                        b = bi % 2
                        S.op('pe', lambda e: e.matmul(out=pp[b][:, 0:n], lhsT=wpbd[:, ch, :], rhs=pooled[:, c0:c0 + n],
                                                      start=True, stop=True), reads=['wpbd', 'pooled'], writes=[('pp', b)])
                        S.op('act', lambda e: e.activation(out=yT[:, ch, ycol0 + c0:ycol0 + c0 + n], in_=pp[b][:, 0:n],
                                                           func=AF.Identity, bias=0.0, scale=cvp[:, ch * 4 + 3:ch * 4 + 4]),
                             reads=[('pp', b), 'cvp'], writes=['yT'])
                for ch in range(2):
                    CU = CU_[:, ch, :]
                    a_ = acc[:, ch, 0:L]
                    S.op('dve', lambda e: e.tensor_scalar(out=a_, in0=CU[:, 1:1 + L], scalar1=wdwT[:, ch * 31:ch * 31 + 1],
                                                          scalar2=cvp[:, ch * 4:ch * 4 + 1], op0=ALU.mult, op1=ALU.add),
                         reads=['cuT', 'wdwT', 'cvp'], writes=['acc'])
                    for j in range(1, 31):
                        S.op('dve', lambda e: e.scalar_tensor_tensor(out=a_, in0=CU[:, 1 + j:1 + j + L],
                                                                     scalar=wdwT[:, ch * 31 + j:ch * 31 + j + 1], in1=a_,
                                                                     op0=ALU.mult, op1=ALU.add),
                             reads=['cuT', 'acc'], writes=['acc'])
                for bi, (c0, n) in enumerate(nblk):
                    for ch in range(2):
                        S.op('pe', lambda e: e.matmul(out=ps1[:, 0:n], lhsT=ones_f[:, :], rhs=acc[:, ch, c0:c0 + n],
                                                      start=(ch == 0), stop=(ch == 1)), reads=['acc', 'ones_f'], writes=['ps1'], inc=(ch == 1))
                    for ch in range(2):
                        S.op('act', lambda e: e.activation(out=sq[ch][:, 0:n], in_=acc[:, ch, c0:c0 + n], func=AF.Square),
                             reads=['acc'], writes=[('sq', ch)])
                        S.op('pe', lambda e: e.matmul(out=ps2[:, 0:n], lhsT=ones_f[:, :], rhs=sq[ch][:, 0:n],
                                                      start=(ch == 0), stop=(ch == 1)), reads=[('sq', ch), 'ones_f'], writes=['ps2'], inc=(ch == 1))
                    S.op('act', lambda e: e.activation(out=mean[:, 0:n], in_=ps1[:, 0:n], func=AF.Copy, scale=1.0 / 256),
                         reads=['ps1'], writes=['mean'])
                    S.op('dve', lambda e: e.tensor_tensor(out=rstd[:, 0:n], in0=mean[:, 0:n], in1=mean[:, 0:n], op=ALU.mult),
                         reads=['mean'], writes=['rstd'])
                    S.op('dve', lambda e: e.scalar_tensor_tensor(out=rstd[:, 0:n], in0=ps2[:, 0:n], scalar=1.0 / 256, in1=rstd[:, 0:n],
                                                                 op0=ALU.mult, op1=ALU.subtract), reads=['ps2', 'rstd'], writes=['rstd'])
                    S.op('act', lambda e: e.activation(out=rstd[:, 0:n], in_=rstd[:, 0:n], func=AF.Sqrt, bias=epst[:, 0:1], scale=1.0),
                         reads=['rstd', 'epst'], writes=['rstd'])
                    S.op('dve', lambda e: e.reciprocal(out=rstd[:, 0:n], in_=rstd[:, 0:n]), reads=['rstd'], writes=['rstd'])
                    for ch in range(2):
                        S.op('dve', lambda e: e.tensor_tensor(out=sq[ch][:, 0:n], in0=acc[:, ch, c0:c0 + n], in1=mean[:, 0:n], op=ALU.subtract),
                             reads=['acc', 'mean'], writes=[('sq', ch)])
                        S.op('dve', lambda e: e.tensor_tensor(out=sq[ch][:, 0:n], in0=sq[ch][:, 0:n], in1=rstd[:, 0:n], op=ALU.mult),
                             reads=['rstd', ('sq', ch)], writes=[('sq', ch)])
                        S.op('act', lambda e: e.activation(out=zT[:, ch, 0:n], in_=sq[ch][:, 0:n], func=AF.Silu,
                                                           bias=cvp[:, ch * 4 + 2:ch * 4 + 3], scale=cvp[:, ch * 4 + 1:ch * 4 + 2]),
                             reads=[('sq', ch), 'cvp'], writes=['zT'])
                    for dch in range(2):
                        for cc_ in range(2):
                            S.op('pe', lambda e: e.matmul(out=pw[dch][:, 0:n], lhsT=wpwb[:, cc_, dch * 128:(dch + 1) * 128],
                                                          rhs=zT[:, cc_, 0:n], start=(cc_ == 0), stop=(cc_ == 1)),
                                 reads=['zT', 'wpwb'], writes=[('pw', dch)], inc=(cc_ == 1))
                        S.op('act', lambda e: e.copy(out=yT[:, 2 + dch, ycol0 + c0:ycol0 + c0 + n], in_=pw[dch][:, 0:n]),
                             reads=[('pw', dch)], writes=['yT'])
            S.barrier()
    dbg_dump('d_QT', QY[:, 0:4, :], ('QT', 0))
    dbg_dump('d_yT', QY[:, 4:8, :], 'yT')
    if stop_after <= 1:
        return finish(nc, S, es, x_out, ctx_out)

    with ExitStack() as pb:
        KTz = [sb('KTz%d' % i, [128, KEYS], BF, st=pb) for i in range(2)]
        KT = KTz[0]
        Vg = sb('Vg', [128, NKC, 192], BF, st=pb)
        with ExitStack() as pb1:
            wkvb = sb('wkvb', [128, 8, 256], BF, st=pb1)
            xt = [sb('xtb%d' % i, [128, D], st=pb1) for i in range(3)]
            xnb = [sb('xnbb%d' % i, [128, D], BF, st=pb1) for i in range(3)]
            hTt = [sb('hTt%d' % i, [128, 8, 128], BF, st=pb1) for i in range(2)]
            ktmp = [sb('ktmp%d' % i, [128, 384], st=pb1) for i in range(2)]
            krb = [sb('krb%d' % i, [128, 128], BF, st=pb1) for i in range(2)]
            cst = [sb('cstb%d' % i, [128, 64], st=pb1) for i in range(3)]
            ptr = [ps('ptrb%d' % i, [128, 8, 128], BF, st=pb1) for i in range(2)]
            pkv = [ps('pkv%d' % i, [128, 512], st=pb1) for i in range(3)]
            pkt = [ps('pkt%d' % i, [128, 128], BF, st=pb1) for i in range(2)]
            for hh in range(2):
                stv = xt[hh][:, :].rearrange("p (k n) -> p k n", n=256)
                S.dma('sp', stv, I['w_in'].rearrange("(k p) n -> p k n", p=128)[:, hh * 4:hh * 4 + 4, 512:768], writes=[('xtb', hh)])
                S.op('dve', lambda e: e.tensor_copy(out=wkvb[:, hh * 4:hh * 4 + 4, :], in_=stv), reads=[('xtb', hh)], writes=['wkvb'])
            S.op('pool', lambda e: e.memset(KTz[0][64:128, :], 0.0), writes=['KTm'])
            S.op('pool', lambda e: e.memset(KTz[1][0:64, :], 0.0), writes=['KTm'])
            S.op('pool', lambda e: e.memset(Vg[:, :, 64:128], 0.0), writes=['Vg'])
            S.op('pool', lambda e: e.memset(Vg[:, :, 64:65], 1.0), writes=['Vg'])
            SB_ = {}; SR_ = {}

            def stB1(c):
                b3 = c % 3
                isctx = c < 2
                src = I['ctx'][c * 128:(c + 1) * 128, :] if isctx else I['x_all'][(c - 2) * 128:(c - 1) * 128, :]
                S.dma('sp', xt[b3][:], src, writes=[('xtb', b3)])
                SB_[c] = ln_stats_a(xt[b3], ('xtb', b3))

            def stB1b(c):
                b3 = c % 3
                ln_mod_a(xt[b3], ('xtb', b3), 128, xnb[b3], ('xnbb', b3), s_=SB_.pop(c))

            def stB2(c):
                b3 = c % 3; b = c % 2
                isctx = c < 2
                ln_mod_b(128, (0, 1), 1 if isctx else 0, lambda kk: hTt[b][:, kk, :], ('hTt', b),
                         xnb[b3], ('xnbb', b3), ptr[b], ('ptrb', b))
                for kk in range(8):
                    S.op('pe', lambda e: e.matmul(out=pkv[b3][:, 0:256], lhsT=hTt[b][:, kk, :], rhs=wkvb[:, kk, :],
                                                  start=(kk == 0), stop=(kk == 7)),
                         reads=hk(('hTt', b)) + ['wkvb'], writes=[('pkv', b3)], inc=(kk == 7))
                S.op('act', lambda e: e.copy(out=Vg[:, c, 0:64], in_=pkv[b3][:, 128:192]), reads=[('pkv', b3)], writes=[('Vg', c)])
                S.op('act', lambda e: e.copy(out=Vg[:, c, 128:192], in_=pkv[b3][:, 192:256]), reads=[('pkv', b3)], writes=[('Vg', c)])
                if not isctx:
                    S.dma('sp', cst[b3][:], I['cs_all'][(c - 2) * 128:(c - 1) * 128, :], writes=[('cstb', b3)])

            def stB3a(c):
                b3 = c % 3; b = c % 2
                SR_[c] = rms_rope(pkv[b3][:, 0:128], ('pkv', b3), 2, qkg[:, 64:128], cst[b3], ('cstb', b3), krb[b][:, :], ('krb', b),
                                  ktmp[b], ('ktmp', b), rope=(c >= 2), part='a')

            def stB3(c):
                b3 = c % 3; b = c % 2
                isctx = c < 2
                rms_rope(pkv[b3][:, 0:128], ('pkv', b3), 2, qkg[:, 64:128], cst[b3], ('cstb', b3), krb[b][:, :], ('krb', b),
                         ktmp[b], ('ktmp', b), rope=not isctx, part='b', st=SR_.pop(c))
                S.op('pe', lambda e: e.transpose(out=pkt[b][:, :], in_=krb[b][:, :], identity=ident_b[:, :]),
                     reads=[('krb', b), 'ident_b'], writes=[('pkt', b)])
                S.op('act', lambda e: e.copy(out=KTz[0][0:64, c * 128:(c + 1) * 128], in_=pkt[b][0:64, :]),
                     reads=[('pkt', b)], writes=[('KT', c)])
                S.op('act', lambda e: e.copy(out=KTz[1][64:128, c * 128:(c + 1) * 128], in_=pkt[b][64:128, :]),
                     reads=[('pkt', b)], writes=[('KT', c)])

            for step in range(NKC + 2):
                if step < NKC:
                    stB1(step)
                if 0 <= step - 2 < NKC:
                    stB3a(step - 2)
                if step < NKC:
                    stB1b(step)
                if 0 <= step - 1 < NKC:
                    stB2(step - 1)
                if 0 <= step - 2 < NKC:
                    stB3(step - 2)
            S.barrier()
        dbg_dump('d_KT', KT[:], ('KT', 0))
        dbg_dump('d_V', Vg[:].rearrange("p a b -> p (a b)"), ('Vg', 0))
        if stop_after <= 2:
            S.barrier()
            return finish(nc, S, es, x_out, ctx_out)

        with ExitStack() as pb2:
            PT = [sb('PT%d' % i, [128, 2, 512], BF, st=pb2) for i in range(4)]
            rr = sb('rr', [128, 512], st=pb2); bcs = [sb('bcs%d' % i, [128, 512], st=pb2) for i in range(2)]
            O = [ps('O%d' % i, [128, 512], st=pb2) for i in range(4)]
            SP = [ps('SP%d' % i, [128, 2, 512], st=pb2) for i in range(2)]
            qblocks = [(i * 512, 512, list(range(NKC))) for i in range(4)] + [(NTOK, NCTX, [0, 1])]
            pti = 0
            for (q0, n, chunks) in qblocks:
                for g in range(2):
                    gp = slice(g * 64, (g + 1) * 64)
                    vsl = slice(0, 65) if g == 0 else slice(64, 192)
                    pend = []
                    steps = [(c, pr) for c in chunks for pr in range(2)]
                    for si, (c, pr) in enumerate(steps):
                        sp_ = SP[pr]
                        for jj in range(2):
                            j = pr * 2 + jj
                            S.op('pe', lambda e: e.matmul(out=sp_[:, jj, 0:n], lhsT=KTz[g][:, c * 128:(c + 1) * 128],
                                                          rhs=QT[:, j, q0:q0 + n], start=True, stop=True),
                                 reads=[('KT', c), ('QT', q0 // 512)], writes=[('SP', pr)], inc=(jj == 1))
                        pb_ = pti % 4; pti += 1
                        S.op('act', lambda e: e.activation(out=PT[pb_][:, :, 0:n], in_=sp_[:, :, 0:n], func=AF.Exp,
                                                           bias=negm[:, 0:1], scale=0.125),
                             reads=[('SP', pr), 'negm'], writes=[('PT', pb_)])
                        if pend:
                            pend.pop(0)()
                        def pv(c=c, pr=pr, pb_=pb_):
                            for jj in range(2):
                                j = pr * 2 + jj
                                S.op('pe', lambda e: e.matmul(out=O[j][0:(65 if g == 0 else 128), 0:n], lhsT=Vg[:, c, vsl],
                                                              rhs=PT[pb_][:, jj, 0:n], start=(c == chunks[0]), stop=(c == chunks[-1])),
                                     reads=[('Vg', c), ('PT', pb_)], writes=[('O', j)], inc=(jj == 1))
                        pend.append(pv)
                    while pend:
                        pend.pop(0)()
                    srow = 64 if g == 0 else 0
                    for j in range(4):
                        bb = j % 2
                        S.op('dve', lambda e: e.reciprocal(out=rr[srow:srow + 1, 0:n], in_=O[j][srow:srow + 1, 0:n]),
                             reads=[('O', j)], writes=['rr'])
                        S.op('pe', lambda e: e.matmul(out=SP[bb][:, 0, 0:n], lhsT=ones_f[srow:srow + 1, :], rhs=rr[srow:srow + 1, 0:n],
                                                      start=True, stop=True), reads=['rr', 'ones_f'], writes=[('SP', bb)])
                        S.op('act', lambda e: e.copy(out=bcs[bb][:, 0:n], in_=SP[bb][:, 0, 0:n]), reads=[('SP', bb)], writes=[('bcs', bb)])
                        S.op('dve', lambda e: e.tensor_tensor(out=QT[gp, j, q0:q0 + n], in0=O[j][gp, 0:n], in1=bcs[bb][gp, 0:n], op=ALU.mult),
                             reads=[('O', j), ('bcs', bb)], writes=[('QT', q0 // 512)])
            S.barrier()
    if dbg:
        dbg_dump('d_QT', QY[:, 0:4, :], ('QT', 0))
    if stop_after <= 3:
        S.barrier()
        return finish(nc, S, es, x_out, ctx_out)

    x_res = sb('x_res', [128, 18, D])
    GT = sb('GT', [32, NTOK + NCTX])
    tilesC = [('own', t, t * 128) for t in range(NTI)] + [('ctx', t, NTOK + t * 128) for t in range(2)]

    LN_ = {}

    def post_ln(tI, pre, prekey):
        lnbc = LN_['t']
        rstd_, nmr_, k = ln_stats(pre, prekey)
        S.op('act', lambda e: e.activation(out=pre[:, :], in_=pre[:, :], func=AF.Identity, bias=nmr_, scale=rstd_),
             reads=[prekey, k], writes=[prekey])
        S.op('dve', lambda e: e.tensor_tensor(out=pre[:, :], in0=pre[:, :], in1=lnbc[:, 0, :], op=ALU.mult),
             reads=[prekey, 'lnbc'], writes=[prekey])
        S.op('pool', lambda e: e.tensor_tensor(out=x_res[:, tI, :], in0=pre[:, :], in1=lnbc[:, 1, :], op=ALU.add),
             reads=[prekey, 'lnbc'], writes=[('xres', tI)])

    with ExitStack() as pc:
        lnbc = sb('lnbc', [128, 2, D], st=pc)
        LN_['t'] = lnbc
        woutb = sb('woutb', [128, 8, D], BF, st=pc)
        wst = [sb('wstc%d' % i, [128, 2, D], st=pc) for i in range(2)]
        xt = [sb('xtc%d' % i, [128, D], st=pc) for i in range(2)]
        pre = [sb('pre%d' % i, [128, D], st=pc) for i in range(2)]
        py = [ps('py%d' % i, [128, 2, 512], st=pc) for i in range(2)]
        S.dma('sp', lnbc[:, 0, :], I['lnv'][:, 0:D].to_broadcast([128, D]), writes=['lnbc'])
        S.dma('sp', lnbc[:, 1, :], I['lnv'][:, D:2 * D].to_broadcast([128, D]), writes=['lnbc'])
        for j in range(4):
            b = j % 2
            for g in range(2):
                r0 = g * 256 + j * 64
                S.dma('sp', wst[b][g * 64:(g + 1) * 64, 0, :], I['w_out'][r0:r0 + 64, :], writes=[('wstc', b)])
            S.dma('sp', wst[b][:, 1, :], I['w_out'][512 + j * 128:512 + (j + 1) * 128, :], writes=[('wstc', b)])
            S.op('dve', lambda e: e.tensor_copy(out=woutb[:, j, :], in_=wst[b][:, 0, :]), reads=[('wstc', b)], writes=['woutb'])
            S.op('pool', lambda e: e.tensor_copy(out=woutb[:, 4 + j, :], in_=wst[b][:, 1, :]), reads=[('wstc', b)], writes=['woutb'])
        for tI, (kind, t, col0) in enumerate(tilesC):
            b = tI % 2
            src = I['x_own'] if kind == 'own' else I['ctx']
            S.dma('sp', xt[b][:], src[t * 128:(t + 1) * 128, :], writes=[('xtc', b)])
            for nh in range(2):
                for kk in range(8):
                    lh = QT[:, kk, col0:col0 + 128] if kk < 4 else yT[:, kk - 4, col0:col0 + 128]
                    S.op('pe', lambda e: e.matmul(out=py[b][:, nh, :], lhsT=lh, rhs=woutb[:, kk, nh * 512:(nh + 1) * 512],
                                                  start=(kk == 0), stop=(kk == 7)),
                         reads=[('QT', col0 // 512), 'yT', 'woutb'], writes=[('py', b)], inc=(kk == 7 and nh == 1))
            gi = 1 if kind == 'ctx' else 0
            S.op('dve', lambda e: e.tensor_tensor(out=pre[b][:, :], in0=py[b][:, :, :].rearrange("p a b -> p (a b)"), in1=gbc[:, gi, :], op=ALU.mult),
                 reads=[('py', b), 'gbc'], writes=[('pre', b)])
            S.op('dve', lambda e: e.scalar_tensor_tensor(out=pre[b][:, :], in0=xt[b][:, :], scalar=ALPHA, in1=pre[b][:, :],
                                                         op0=ALU.mult, op1=ALU.add), reads=[('xtc', b), ('pre', b)], writes=[('pre', b)])
            post_ln(tI, pre[b], ('pre', b))
        S.barrier()
    dbg_dump('d_xres', x_res[:].rearrange("p a b -> p (a b)"), ('xres', 0))
    if stop_after <= 4:
        S.barrier()
        return finish(nc, S, es, x_out, ctx_out)

    hT = QY
    with ExitStack() as pd:
        wrb = sb('wrb', [128, 8, 36], BF, st=pd); wrs = sb('wrs', [128, 8, 36], st=pd)
        brb = sb('brb', [1, 36], BF, st=pd); brs = sb('brs', [1, 36], st=pd)
        ones_b = sb('ones_b', [1, 128], BF, st=pd)
        xnb = [sb('xnbd%d' % i, [128, D], BF, st=pd) for i in range(2)]
        rt = sb('rt', [128, 4, 64], st=pd)
        ptr = [ps('ptrd%d' % i, [128, 8, 128], BF, st=pd) for i in range(2)]
        prr = [ps('prr%d' % i, [128, 64], st=pd) for i in range(2)]
        pgt = [ps('pgt%d' % i, [32, 128], st=pd) for i in range(2)]
        S.dma('sp', wrs[:], I['w_r'].rearrange("(k p) n -> p k n", p=128), writes=['wrs'])
        S.op('dve', lambda e: e.tensor_copy(out=wrb[:], in_=wrs[:]), reads=['wrs'], writes=['wrb'])
        S.dma('sp', brs[:], I['b_r'], writes=['brs'])
        S.op('dve', lambda e: e.tensor_copy(out=brb[:], in_=brs[:]), reads=['brs'], writes=['brb'])
        S.op('dve', lambda e: e.memset(ones_b[:], 1.0), writes=['ones_b'])
        for tI, (kind, t, col0) in enumerate(tilesC):
            b = tI % 2
            w = 1 if kind == 'ctx' else 0
            xr = x_res[:, tI, :]
            ln_mod_T(xr, ('xres', tI), 128, (2, 3), w, lambda kk: hT[:, kk, col0:col0 + 128], ('hT2', tI),
                     xnb[b], ('xnbd', b), ptr[b], ('ptrd', b))
            S.op('pool', lambda e: e.tensor_scalar(out=xr, in0=xr, scalar1=ALPHA, scalar2=None, op0=ALU.mult),
                 reads=[('xres', tI), ('xnbd', b)], writes=[('xres', tI)])
            for kk in range(8):
                S.op('pe', lambda e: e.matmul(out=prr[b][:, 0:36], lhsT=hT[:, kk, col0:col0 + 128], rhs=wrb[:, kk, :],
                                              start=(kk == 0), stop=False), reads=hk(('hT2', tI)) + ['wrb'], writes=[('prr', b)], inc=False)
            S.op('pe', lambda e: e.matmul(out=prr[b][:, 0:36], lhsT=ones_b[0:1, :], rhs=brb[0:1, :], start=False, stop=True),
                 reads=['ones_b', 'brb'], writes=[('prr', b)])
            r = rt[:, tI % 4, :]; rk = ('rt', tI % 4)
            lg = r[:, 0:36]
            S.op('act', lambda e: e.copy(out=lg, in_=prr[b][:, 0:36]), reads=[('prr', b)], writes=[rk])
            S.op('dve', lambda e: e.tensor_reduce(out=r[:, 36:37], in_=r[:, 0:4], axis=AX.X, op=ALU.max), reads=[rk], writes=[rk])
            S.op('dve', lambda e: e.tensor_scalar(out=r[:, 40:44], in0=r[:, 0:4], scalar1=r[:, 36:37], scalar2=None, op0=ALU.is_ge),
                 reads=[rk], writes=[rk])
            S.op('dve', lambda e: e.tensor_scalar(out=r[:, 37:38], in0=r[:, 36:37], scalar1=-1.0, scalar2=None, op0=ALU.mult),
                 reads=[rk], writes=[rk])
            S.op('act', lambda e: e.activation(out=r[:, 44:48], in_=r[:, 0:4], func=AF.Exp, bias=r[:, 37:38], scale=1.0,
                                               accum_out=r[:, 38:39]), reads=[rk], writes=[rk])
            S.op('dve', lambda e: e.tensor_scalar(out=r[:, 40:44], in0=r[:, 40:44], scalar1=-1.0, scalar2=1.0e4, op0=ALU.add, op1=ALU.mult),
                 reads=[rk], writes=[rk])
            le = r[:, 4:36].rearrange("p (g x) -> p g x", x=8)
            S.op('dve', lambda e: e.tensor_tensor(out=le, in0=le, in1=r[:, 40:44].unsqueeze(2).to_broadcast([128, 4, 8]), op=ALU.add),
                 reads=[rk], writes=[rk])
            S.op('dve', lambda e: e.max(out=r[:, 48:56], in_=r[:, 4:36]), reads=[rk], writes=[rk])
            S.op('dve', lambda e: e.tensor_scalar(out=r[:, 39:40], in0=r[:, 48:49], scalar1=-1.0, scalar2=None, op0=ALU.mult),
                 reads=[rk], writes=[rk])
            S.op('act', lambda e: e.activation(out=r[:, 4:36], in_=r[:, 4:36], func=AF.Exp, bias=r[:, 39:40], scale=1.0),
                 reads=[rk], writes=[rk])
            S.op('act', lambda e: e.activation(out=r[:, 56:57], in_=r[:, 49:50], func=AF.Exp, bias=r[:, 39:40], scale=1.0),
                 reads=[rk], writes=[rk])
            S.op('dve', lambda e: e.tensor_scalar(out=r[:, 57:58], in0=r[:, 56:57], scalar1=1.0, scalar2=r[:, 38:39], op0=ALU.add, op1=ALU.mult),
                 reads=[rk], writes=[rk])
            S.op('dve', lambda e: e.reciprocal(out=r[:, 57:58], in_=r[:, 57:58]), reads=[rk], writes=[rk])
            S.op('dve', lambda e: e.scalar_tensor_tensor(out=r[:, 4:36], in0=r[:, 4:36], scalar=r[:, 56:57], in1=r[:, 4:36],
                                                         op0=ALU.is_ge, op1=ALU.mult), reads=[rk], writes=[rk])
            S.op('dve', lambda e: e.tensor_scalar(out=r[:, 4:36], in0=r[:, 4:36], scalar1=r[:, 57:58], scalar2=None, op0=ALU.mult),
                 reads=[rk], writes=[rk])
            S.op('pe', lambda e: e.transpose(out=pgt[b][:, :], in_=r[:, 4:36], identity=ident_f[:, :]),
                 reads=[rk, 'ident_f'], writes=[('pgt', b)])
            S.op('act', lambda e: e.copy(out=GT[:, col0:col0 + 128], in_=pgt[b][:, :]), reads=[('pgt', b)], writes=['GT'])
        S.barrier()
    dbg_dump('d_GT', GT[:], 'GT')
    if stop_after <= 5:
        S.barrier()
        return finish(nc, S, es, x_out, ctx_out)

    allhT2 = []
    with ExitStack() as pe_:
        wgb = [sb('wgb%d' % i, [128, 8, 256], BF, st=pe_) for i in range(2)]
        wub = [sb('wub%d' % i, [128, 8, 256], BF, st=pe_) for i in range(2)]
        wdb = [sb('wdb%d' % i, [128, 2, D], BF, st=pe_) for i in range(2)]
        wdc = [sb('wdc%d' % i, [128, 2, D], BF, st=pe_) for i in range(2)]
        stg = [sb('stg%d' % i, [128, 2048], st=pe_) for i in range(2)]
        Gs = [sb('Gs%d' % i, [128, 512], st=pe_) for i in range(2)]
        st_ = [sb('st_s%d' % i, [128, 512], st=pe_) for i in range(2)]
        tt_ = [sb('tt_s%d' % i, [128, 512], st=pe_) for i in range(2)]
        aT = [sb('aT%d' % i, [128, 2, 512], BF, st=pe_) for i in range(2)]
        pg = [ps('pg%d' % i, [128, 512], st=pe_) for i in range(2)]
        pu = [ps('pu%d' % i, [128, 512], st=pe_) for i in range(2)]
        pgb = ps('pgb', [128, 512], st=pe_)
        pyd = [ps('pyd%d' % i, [128, 512], st=pe_) for i in range(3)]
        si = 0; YD = [0]; cnt2 = 0; pend_dn = []
        tblocks = [(i * 512, 512, 'own') for i in range(4)] + [(NTOK, NCTX, 'ctx')]
        for e_ in range(NEXP):
            for fh in range(2):
                wb = (e_ * 2 + fh) % 2
                for which in range(3):
                    s_ = si % 2; si += 1
                    if which < 2:
                        srcw = (I['w_eg'] if which == 0 else I['w_eu'])[e_].rearrange("(k p) f -> p k f", p=128)[:, :, fh * 256:(fh + 1) * 256]
                        S.dma('sp', stg[s_][:, :].rearrange("p (k f) -> p k f", f=256), srcw, writes=[('stg', s_)])
                        dst = (wgb if which == 0 else wub)[wb]
                        S.op('pool' if which == 0 else 'act',
                             (lambda e: e.tensor_copy(out=dst[:], in_=stg[s_][:, :].rearrange("p (k f) -> p k f", f=256))) if which == 0 else
                             (lambda e: e.copy(out=dst[:], in_=stg[s_][:, :].rearrange("p (k f) -> p k f", f=256))),
                             reads=[('stg', s_)], writes=[('wg' if which == 0 else 'wu', wb)])
                    else:
                        srcw = I['w_ed'][e_].rearrange("(c p) n -> p c n", p=128)[:, fh * 2:fh * 2 + 2, :]
                        S.dma('sp', stg[s_][:, :].rearrange("p (c n) -> p c n", n=D), srcw, writes=[('stg', s_)])
                        sv = stg[s_][:, :].rearrange("p (c n) -> p c n", n=D)
                        S.op('pool', lambda e: e.tensor_tensor(out=wdb[wb][:], in0=sv, in1=gbc[:, 2:3, :].to_broadcast([128, 2, D]), op=ALU.mult),
                             reads=[('stg', s_), 'gbc'], writes=[('wd', wb)])
                        S.op('dve', lambda e: e.tensor_tensor(out=wdc[wb][:], in0=sv, in1=gbc[:, 3:4, :].to_broadcast([128, 2, D]), op=ALU.mult),
                             reads=[('stg', s_), 'gbc'], writes=[('wdc', wb)])
                for (c0, n, kind) in tblocks:
                    gb = cnt2 % 2; cnt2 += 1
                    S.op('pe', lambda e: e.matmul(out=pgb[:, 0:n], lhsT=ident_f[0:32, e_:e_ + 1].to_broadcast([32, 128]), rhs=GT[:, c0:c0 + n], start=True, stop=True),
                         reads=['ident_f', 'GT'], writes=['pgb'])
                    S.op('act', lambda e: e.copy(out=Gs[gb][:, 0:n], in_=pgb[:, 0:n]), reads=['pgb'], writes=[('Gs', gb)])
                    for fc in range(2):
                        for kk in range(8):
                            S.op('pe', lambda e: e.matmul(out=pg[fc][:, 0:n], lhsT=wgb[wb][:, kk, fc * 128:(fc + 1) * 128], rhs=hT[:, kk, c0:c0 + n],
                                                          start=(kk == 0), stop=(kk == 7)), reads=allhT2 + [('wg', wb)], writes=[('pg', fc)], inc=(kk == 7))
                        for kk in range(8):
                            S.op('pe', lambda e: e.matmul(out=pu[fc][:, 0:n], lhsT=wub[wb][:, kk, fc * 128:(fc + 1) * 128], rhs=hT[:, kk, c0:c0 + n],
                                                          start=(kk == 0), stop=(kk == 7)), reads=allhT2 + [('wu', wb)], writes=[('pu', fc)], inc=(kk == 7))
                        S.op('act', lambda e: e.activation(out=st_[fc][:, 0:n], in_=pg[fc][:, 0:n], func=AF.Silu), reads=[('pg', fc)], writes=[('st', fc)])
                        S.op('dve', lambda e: e.tensor_tensor(out=tt_[fc][:, 0:n], in0=pu[fc][:, 0:n], in1=Gs[gb][:, 0:n], op=ALU.mult),
                             reads=[('pu', fc), ('Gs', gb)], writes=[('tt', fc)])
                        S.op('pool', lambda e: e.tensor_tensor(out=aT[gb][:, fc, 0:n], in0=st_[fc][:, 0:n], in1=tt_[fc][:, 0:n], op=ALU.mult),
                             reads=[('st', fc), ('tt', fc)], writes=[('aT', gb)])
                    def down(gb=gb, wb=wb, kind=kind, c0=c0, n=n):
                        wdsel = wdc if kind == 'ctx' else wdb
                        for tt in range(n // 128):
                            tI = (c0 + tt * 128) // 128
                            for nh in range(2):
                                y_ = YD[0] % 3; YD[0] += 1
                                for fc in range(2):
                                    S.op('pe', lambda e: e.matmul(out=pyd[y_][:, :], lhsT=aT[gb][:, fc, tt * 128:(tt + 1) * 128],
                                                                  rhs=wdsel[wb][:, fc, nh * 512:(nh + 1) * 512], start=(fc == 0), stop=(fc == 1)),
                                         reads=[('aT', gb), ('wdc' if kind == 'ctx' else 'wd', wb)], writes=[('pyd', y_)], inc=(fc == 1))
                                xs = x_res[:, tI, nh * 512:(nh + 1) * 512]
                                S.op('dve', lambda e: e.tensor_tensor(out=xs, in0=pyd[y_][:, :], in1=xs, op=ALU.add),
                                     reads=[('pyd', y_), ('xres', tI)], writes=[('xres', tI)])
                    if pend_dn:
                        pend_dn.pop(0)()
                    pend_dn.append(down)
        while pend_dn:
            pend_dn.pop(0)()
        S.barrier()

    with ExitStack() as pf_:
        lnbc = sb('lnbc2', [128, 2, D], st=pf_)
        LN_['t'] = lnbc
        S.dma('sp', lnbc[:, 0, :], I['lnv'][:, 2 * D:3 * D].to_broadcast([128, D]), writes=['lnbc'])
        S.dma('sp', lnbc[:, 1, :], I['lnv'][:, 3 * D:4 * D].to_broadcast([128, D]), writes=['lnbc'])
        pre = [sb('pree%d' % i, [128, D], st=pf_) for i in range(2)]
        for tI, (kind, t, col0) in enumerate(tilesC):
            b = tI % 2
            S.op('act', lambda e: e.copy(out=pre[b][:, :], in_=x_res[:, tI, :]), reads=[('xres', tI)], writes=[('pree', b)])
            post_ln(tI, pre[b], ('pree', b))
            dst = x_out[t * 128:(t + 1) * 128, :] if kind == 'own' else ctx_out[t * 128:(t + 1) * 128, :]
            S.dma('sp', dst, x_res[:, tI, :], reads=[('xres', tI)], writes=[('out', tI)])
        S.barrier()
    return finish(nc, S, es, x_out, ctx_out)


def finish(nc, S, es, x_out, ctx_out):
    S.barrier()
    es.close()
    return nc


def _rope_cs(n_tok):
    t = np.arange(n_tok)
    row = (t // GRID_W).astype(np.float32); col = (t % GRID_W).astype(np.float32)
    inv = (10000.0 ** (-np.arange(16, dtype=np.float32) / 16)).astype(np.float32)
    ang = np.stack([row[:, None] * inv, col[:, None] * inv], axis=1).astype(np.float32)
    return np.concatenate([np.cos(ang).reshape(n_tok, 32), np.sin(ang).reshape(n_tok, 32)], axis=1).astype(np.float32)


def _invcnt(n, t0, L):
    t = np.arange(t0, t0 + L)
    out = np.zeros((128, 2 * L), np.float32)
    for ch, (wa, wb) in enumerate(((2, 4), (8, 16))):
        for h, w in enumerate((wa, wb)):
            lo = np.clip(t - w // 2, 0, n); hi = np.clip(t + (w - w // 2), 0, n)
            out[h * 64:(h + 1) * 64, ch * L:(ch + 1) * L] = (1.0 / (hi - lo).astype(np.float32))[None, :]
    return out


_NC_CACHE = {}


def _layer_inputs(l, x, ctx_x, P):
    f = np.float32
    common = {
        'x_all': np.ascontiguousarray(x, f), 'ctx': np.ascontiguousarray(ctx_x, f),
        'cT': np.ascontiguousarray(np.stack([P['c'].reshape(8, 128).T, P['c_ctx'].reshape(8, 128).T], axis=2).reshape(128, 16), f),
        'w_mod': np.ascontiguousarray(P['w_mod'][l], f), 'b_mod': np.ascontiguousarray(P['b_mod'][l][None, :], f),
        'w_in': np.ascontiguousarray(P['w_in'][l], f),
        'qk_gain': np.ascontiguousarray(np.concatenate([P['q_gain'][l], P['k_gain'][l]])[None, :], f),
        'w_pool': np.ascontiguousarray(P['w_pool'][l].reshape(256, 64), f),
        'cvp': np.ascontiguousarray(np.stack([P['b_dw'][l].reshape(2, 128).T, P['cv_ln_g'][l].reshape(2, 128).T,
                                              P['cv_ln_b'][l].reshape(2, 128).T, P['pool_scale'][l].reshape(2, 128).T],
                                             axis=2).reshape(128, 8), f),
        'w_dwT': np.ascontiguousarray(P['w_dw'][l].reshape(31, 2, 128).transpose(2, 1, 0).reshape(128, 62), f),
        'w_pw': np.ascontiguousarray(P['w_cv_pw'][l], f), 'w_out': np.ascontiguousarray(P['w_out'][l], f),
        'lnv': np.ascontiguousarray(np.concatenate([P['ln1_g'][l], P['ln1_b'][l], P['ln2_g'][l], P['ln2_b'][l]])[None, :], f),
        'w_r': np.ascontiguousarray(np.concatenate([P['w_rg'][l], P['w_re'][l]], axis=1), f),
        'b_r': np.ascontiguousarray(np.concatenate([P['b_rg'][l], P['b_re'][l]])[None, :], f),
        'w_eg': np.ascontiguousarray(P['w_e_gate'][l], f), 'w_eu': np.ascontiguousarray(P['w_e_up'][l], f),
        'w_ed': np.ascontiguousarray(P['w_e_down'][l], f),
        'cs_all': _rope_cs(SEQ),
        'sel': np.ascontiguousarray(np.repeat(np.eye(32, dtype=f), 128, axis=1)),
        'selrow': np.ascontiguousarray(np.concatenate([np.repeat(np.eye(64, dtype=f)[:, 0:1], 128, 1),
                                                       np.repeat(np.eye(64, dtype=f)[:, 32:33], 128, 1)], axis=1)),
        'ident_f': np.eye(128, dtype=f), 'ident_b': np.eye(128, dtype=f).astype(ml_dtypes.bfloat16),
    }
    ic_ctx = _invcnt(NCTX, 0, NCTX)
    maps = []
    for r in range(NCORE):
        m = dict(common)
        t0 = r * NTOK
        m['x_own'] = np.ascontiguousarray(x[t0:t0 + NTOK], f)
        xh = np.zeros((32, D), f); hm = np.zeros((128, 32), f)
        if r > 0:
            xh[0:16] = x[t0 - 16:t0]; hm[:, 0:16] = 1.0
        if r < NCORE - 1:
            xh[16:32] = x[t0 + NTOK:t0 + NTOK + 16]; hm[:, 16:32] = 1.0
        m['x_halo'] = xh; m['hmask'] = hm
        m['cs_own'] = np.ascontiguousarray(common['cs_all'][t0:t0 + NTOK])
        m['invcnt'] = np.ascontiguousarray(np.concatenate([_invcnt(SEQ, t0, NTOK), ic_ctx], axis=1))
        maps.append(m)
    return maps


def kernel(**inputs):
    P = {k: np.asarray(v) for k, v in inputs.items()}
    x = P['x'][0]
    ctx_x = P['ctx'][0]
    P['c'] = P['c'].reshape(-1)
    if 'nc' not in _NC_CACHE:
        _NC_CACHE['nc'] = build()
    nc = _NC_CACHE['nc']
    for l in range(DEPTH):
        maps = _layer_inputs(l, x, ctx_x, P)
        res = run_bass_kernel_spmd(nc, maps, core_ids=list(range(NCORE)))
        x = np.concatenate([np.asarray(res.results[r]['x_out']) for r in range(NCORE)], axis=0)
        ctx_x = np.asarray(res.results[0]['ctx_out'])
    return x[None].astype(np.float32)
```
